# Optimizing a Trainium2 kernel written in Bass

```python
import jax, jax.numpy as jnp
from jax import lax
import numpy as np

D_MODEL = 1024
BATCH = 8
SEQ = 4096
DEPTH = 2

MIX_WIDTH = D_MODEL
POOL_WIDTH = MIX_WIDTH // 2
POOL_GROUPS = 4
POOL_GROUP_DIM = POOL_WIDTH // POOL_GROUPS
POOL_WINDOWS = (2, 4, 8, 16)
CONV_WIDTH = MIX_WIDTH - POOL_WIDTH
CONV_HEADS = 8
CONV_HEAD_DIM = CONV_WIDTH // CONV_HEADS
CONV_KERNEL = 31
IN_PROJ_WIDTH = POOL_WIDTH + 2 * CONV_WIDTH
N_GROUPS = 4
EXPERTS_PER_GROUP = 8
N_EXPERTS = N_GROUPS * EXPERTS_PER_GROUP
TOP_K = 2
EXPERT_HIDDEN = D_MODEL // 4
N_MOD = 6
RMS_EPS = 1e-6
LN_EPS = 1e-5

kernel_name = "hybrid_pool_conformer_hmoe_adaln"


def rmsnorm(x, g):
    xf = x.astype(jnp.float32)
    y = xf * lax.rsqrt(jnp.mean(jnp.square(xf), axis=-1, keepdims=True) + RMS_EPS)
    return (y * g.astype(jnp.float32)).astype(x.dtype)


def pool_mixer(u, pool_w, pool_scale):
    b, s, _ = u.shape
    uf = u.astype(jnp.float32).reshape(b, s, POOL_GROUPS, POOL_GROUP_DIM)
    cs = lax.cumsum(uf, axis=1)
    pos = jnp.arange(s)
    outs = []
    for g, w in enumerate(POOL_WINDOWS):
        cg = cs[:, :, g]
        lag = jnp.pad(cg[:, :s - w], ((0, 0), (w, 0), (0, 0)))
        count = jnp.minimum(pos + 1, w).astype(jnp.float32)[None, :, None]
        outs.append((cg - lag) / count - uf[:, :, g])
    pooled = jnp.stack(outs, axis=2).astype(u.dtype)
    y = jnp.einsum('bsgc,gcd->bsgd', pooled, pool_w).reshape(b, s, POOL_WIDTH)
    return y * pool_scale


def conv_module(a, gate, conv_w, conv_b, ln_g, ln_b):
    v = a * jax.nn.sigmoid(gate)
    y = lax.conv_general_dilated(
        v, conv_w[:, None, :], window_strides=(1,), padding=[(CONV_KERNEL - 1, 0)],
        dimension_numbers=('NWC', 'WIO', 'NWC'), feature_group_count=CONV_WIDTH) + conv_b
    b, s, _ = y.shape
    yf = y.astype(jnp.float32).reshape(b, s, CONV_HEADS, CONV_HEAD_DIM)
    mu = jnp.mean(yf, axis=-1, keepdims=True)
    var = jnp.mean(jnp.square(yf - mu), axis=-1, keepdims=True)
    yn = ((yf - mu) * lax.rsqrt(var + LN_EPS)).reshape(b, s, CONV_WIDTH)
    yn = yn * ln_g.astype(jnp.float32) + ln_b.astype(jnp.float32)
    return jax.nn.silu(yn).astype(a.dtype)


def hier_moe(h, rg_w, rg_b, re_w, re_b, w_gate, w_up, w_down):
    b, s, d = h.shape
    t = h.reshape(b * s, d)
    n = t.shape[0]
    g_logits = (t @ rg_w).astype(jnp.float32) + rg_b.astype(jnp.float32)
    g_prob = jax.nn.softmax(g_logits, axis=-1)
    g_sel = jnp.argmax(g_logits, axis=-1)
    p_group = jnp.take_along_axis(g_prob, g_sel[:, None], axis=-1)
    e_logits = ((t @ re_w).astype(jnp.float32) + re_b.astype(jnp.float32)).reshape(n, N_GROUPS, EXPERTS_PER_GROUP)
    e_logits = jnp.take_along_axis(e_logits, g_sel[:, None, None], axis=1)[:, 0]
    top_logit, top_idx = lax.top_k(e_logits, TOP_K)
    weights = p_group * jax.nn.softmax(top_logit, axis=-1)
    eid = g_sel[:, None] * EXPERTS_PER_GROUP + top_idx
    gates = jnp.zeros((n, N_EXPERTS), jnp.float32).at[jnp.arange(n)[:, None], eid].add(weights).astype(h.dtype)
    out = jnp.zeros_like(t)
    for e in range(N_EXPERTS):
        hid = jax.nn.silu(t @ w_gate[e]) * (t @ w_up[e])
        out = out + gates[:, e:e + 1] * (hid @ w_down[e])
    return out.reshape(b, s, d)


def setup_inputs(seed: int = 0) -> dict:
    key = jax.random.key(seed)
    ks = jax.random.split(key, 24)
    f32 = jnp.float32
    nrm = lambda k, shape, scale: (jax.random.normal(k, shape, f32) * scale).astype(f32)
    L, D = DEPTH, D_MODEL
    return {
        "x": nrm(ks[0], (BATCH, SEQ, D), 1.0),
        "c": nrm(ks[1], (BATCH, D), 1.0),
        "ada_w": nrm(ks[2], (L, D, N_MOD * D), 0.5 * D ** -0.5),
        "ada_b": nrm(ks[3], (L, N_MOD * D), 0.02),
        "norm1_g": 1.0 + nrm(ks[4], (L, D), 0.02),
        "w_in": nrm(ks[5], (L, D, IN_PROJ_WIDTH), D ** -0.5),
        "pool_w": nrm(ks[6], (L, POOL_GROUPS, POOL_GROUP_DIM, POOL_GROUP_DIM), POOL_GROUP_DIM ** -0.5),
        "pool_scale": 1.0 + nrm(ks[7], (L, POOL_WIDTH), 0.1),
        "conv_w": nrm(ks[8], (L, CONV_KERNEL, CONV_WIDTH), CONV_KERNEL ** -0.5),
        "conv_b": nrm(ks[9], (L, CONV_WIDTH), 0.02),
        "conv_ln_g": 1.0 + nrm(ks[10], (L, CONV_WIDTH), 0.02),
        "conv_ln_b": nrm(ks[11], (L, CONV_WIDTH), 0.02),
        "w_out": nrm(ks[12], (L, MIX_WIDTH, D), MIX_WIDTH ** -0.5),
        "norm2_g": 1.0 + nrm(ks[13], (L, D), 0.02),
        "router_group_w": nrm(ks[14], (L, D, N_GROUPS), D ** -0.5),
        "router_group_b": nrm(ks[15], (L, N_GROUPS), 0.01),
        "router_expert_w": nrm(ks[16], (L, D, N_EXPERTS), D ** -0.5),
        "router_expert_b": nrm(ks[17], (L, N_EXPERTS), 0.01),
        "expert_w_gate": nrm(ks[18], (L, N_EXPERTS, D, EXPERT_HIDDEN), D ** -0.5),
        "expert_w_up": nrm(ks[19], (L, N_EXPERTS, D, EXPERT_HIDDEN), D ** -0.5),
        "expert_w_down": nrm(ks[20], (L, N_EXPERTS, EXPERT_HIDDEN, D), EXPERT_HIDDEN ** -0.5),
        "final_g": 1.0 + nrm(ks[21], (D,), 0.02),
    }


def reference(x, c, ada_w, ada_b, norm1_g, w_in, pool_w, pool_scale, conv_w, conv_b,
              conv_ln_g, conv_ln_b, w_out, norm2_g, router_group_w, router_group_b,
              router_expert_w, router_expert_b, expert_w_gate, expert_w_up, expert_w_down,
              final_g):
    cond = jax.nn.silu(c)
    for l in range(DEPTH):
        mod = (cond @ ada_w[l] + ada_b[l])[:, None, :]
        sh1, sc1, g1, sh2, sc2, g2 = jnp.split(mod, N_MOD, axis=-1)
        h = rmsnorm(x, norm1_g[l]) * (1.0 + sc1) + sh1
        z = h @ w_in[l]
        u_pool = z[..., :POOL_WIDTH]
        glu_a = z[..., POOL_WIDTH:POOL_WIDTH + CONV_WIDTH]
        glu_g = z[..., POOL_WIDTH + CONV_WIDTH:]
        y_pool = pool_mixer(u_pool, pool_w[l], pool_scale[l])
        y_conv = conv_module(glu_a, glu_g, conv_w[l], conv_b[l], conv_ln_g[l], conv_ln_b[l])
        y = jnp.concatenate([y_pool, y_conv], axis=-1) @ w_out[l]
        x = x + g1 * y
        h = rmsnorm(x, norm2_g[l]) * (1.0 + sc2) + sh2
        x = x + g2 * hier_moe(h, router_group_w[l], router_group_b[l], router_expert_w[l],
                              router_expert_b[l], expert_w_gate[l], expert_w_up[l], expert_w_down[l])
    return rmsnorm(x, final_g)
```

```python
import numpy as np
import concourse.bass as bass
import concourse.mybir as mybir
from concourse.bass_utils import run_bass_kernel_spmd

F32 = mybir.dt.float32
BF16 = mybir.dt.bfloat16
I32 = mybir.dt.int32
AF = mybir.ActivationFunctionType
ALU = mybir.AluOpType
AX = mybir.AxisListType

D = 1024
DEPTH = 2
NE = 32
HID = 256
RMS_EPS = 1e-6
LN_EPS = 1e-5
POOL_WINDOWS = (2, 4, 8, 16)
CK = 31
N_CORES = 8


class Prog:
    ENGS = ("pe", "act", "dve", "pool", "sp")

    def __init__(self, nc, n_dma_sems=6):
        self.nc = nc
        self.ops = []
        self.n_dma_sems = n_dma_sems

    def op(self, eng, fn, r=(), w=()):
        self.ops.append(dict(eng=eng, fn=fn, r=tuple(r), w=tuple(w), dma=False))

    def dma(self, q, fn, r=(), w=()):
        self.ops.append(dict(eng=q, fn=fn, r=tuple(r), w=tuple(w), dma=True))

    def barrier(self):
        self.ops.append(dict(eng=None, fn=None, r=(), w=(), dma=False, barrier=True))

    def emit(self, sems):
        nc = self.nc
        engobj = dict(pe=nc.tensor, act=nc.scalar, dve=nc.vector, pool=nc.gpsimd, sp=nc.sync)
        ops = self.ops
        n = len(ops)
        last_w = {}
        readers = {}
        clock = {e: dict(pe=-1, act=-1, dve=-1, pool=-1, sp=-1, dma=set()) for e in self.ENGS}
        opclock = [None] * n
        dma_hist = {e: [] for e in self.ENGS}
        waits = [None] * n
        sig = [False] * n
        bar_deps = set()
        last_on = {}
        for i, o in enumerate(ops):
            e = o["eng"]
            if e is None:
                bar_deps = set(last_on.values())
                for q in self.ENGS:
                    bar_deps |= set(dma_hist[q][-self.n_dma_sems:])
                waits[i] = []
                opclock[i] = None
                continue
            deps = set(bar_deps)
            for r in o["r"]:
                if r in last_w:
                    deps.add(last_w[r])
            for w in o["w"]:
                if w in last_w:
                    deps.add(last_w[w])
                for j in readers.get(w, ()):
                    deps.add(j)
            if o["dma"]:
                h = dma_hist[e]
                if len(h) >= self.n_dma_sems:
                    deps.add(h[-self.n_dma_sems])
                h.append(i)
            deps.discard(i)
            need = []
            ck = clock[e]
            for j in sorted(deps):
                pj = ops[j]
                if pj["dma"]:
                    if j in ck["dma"]:
                        continue
                else:
                    if pj["eng"] == e and e == "pe" and not o["dma"]:
                        continue
                    if ck[pj["eng"]] >= j:
                        continue
                need.append(j)
                sig[j] = True
                oc = opclock[j]
                for k in ("pe", "act", "dve", "pool", "sp"):
                    if oc[k] > ck[k]:
                        ck[k] = oc[k]
                ck["dma"] |= oc["dma"]
            waits[i] = need
            oc = dict(pe=ck["pe"], act=ck["act"], dve=ck["dve"], pool=ck["pool"], sp=ck["sp"], dma=set(ck["dma"]))
            if o["dma"]:
                oc["dma"].add(i)
            else:
                oc[e] = i
            opclock[i] = oc
            if not o["dma"]:
                last_on[e] = i
            for r in o["r"]:
                readers.setdefault(r, []).append(i)
            for w in o["w"]:
                last_w[w] = i
                readers[w] = []
        cnt = {e: 0 for e in self.ENGS}
        dcnt = {}
        dma_n = {e: 0 for e in self.ENGS}
        ev = [None] * n
        for i, o in enumerate(ops):
            e = o["eng"]
            if e is None:
                continue
            E = engobj[e]
            for j in waits[i]:
                s, v = ev[j]
                E.wait_ge(s, v)
            inst = o["fn"](E)
            if o["dma"]:
                k = dma_n[e] % self.n_dma_sems
                dma_n[e] += 1
                s = sems["dma_" + e][k]
                dcnt[(e, k)] = dcnt.get((e, k), 0) + 16
                inst.then_inc(s, 16)
                ev[i] = (s, dcnt[(e, k)])
            elif sig[i]:
                cnt[e] += 1
                inst.then_inc(sems[e], 1)
                ev[i] = (sems[e], cnt[e])
        return ev


def build(S, depth=DEPTH, debug=False):
    DEPTH_ = depth
    NB = S // 128
    NT = S // 512
    TM = 2 * S // 128 + NE
    nc = bass.Bass("TRN2", target_bir_lowering=False)
    P = Prog(nc)

    def dram_in(name, shape, dt=F32):
        return nc.dram_tensor(name, list(shape), dt, kind="ExternalInput")

    x_d = dram_in("x", [S, D])
    c_d = dram_in("c", [D])
    ada_w = dram_in("ada_w", [DEPTH, D, 6 * D])
    ada_b = dram_in("ada_b", [DEPTH, 6 * D])
    norm1_g = dram_in("norm1_g", [DEPTH, D])
    w_in = dram_in("w_in", [DEPTH, D, 1536])
    pool_w = dram_in("pool_w", [DEPTH, 4, 128, 128])
    pvec = dram_in("pvec", [DEPTH, 35, 512])
    w_out = dram_in("w_out", [DEPTH, D, D])
    norm2_g = dram_in("norm2_g", [DEPTH, D])
    rw = dram_in("rw", [DEPTH, D, 36])
    rb = dram_in("rb", [DEPTH, 36])
    wg_d = [dram_in(f"wg{l}", [NE * 128, 8 * HID]) for l in range(DEPTH)]
    wu_d = [dram_in(f"wu{l}", [NE * 128, 8 * HID]) for l in range(DEPTH)]
    wd_d = [dram_in(f"wd{l}", [NE * 128, 2 * D]) for l in range(DEPTH)]
    final_g = dram_in("final_g", [D])
    out_d = nc.dram_tensor("out", [S, D], F32, kind="ExternalOutput")
    sk = "ExternalOutput" if debug else "Internal"
    xs1 = nc.dram_tensor("xs1", [S, D], F32, kind=sk)
    xs2 = nc.dram_tensor("xs2", [S, D], F32, kind=sk)
    h2s = nc.dram_tensor("h2s", [S, D], BF16, kind=sk)
    hsort = nc.dram_tensor("hsort", [TM * 128, D], BF16, kind=sk)
    ysort = nc.dram_tensor("ysort", [TM * 128, D], F32, kind=sk)
    dbg = {}
    if debug:
        for nm, shp, dt in (("d_mod", [128, 6 * D], F32), ("d_hT", [128, 8 * 512], BF16), ("d_ycat", [128, 8 * 512], BF16),
                            ("d_lg", [128, 4 * 36], F32), ("d_pos1", [128, NB], I32), ("d_pos2", [128, NB], I32),
                            ("d_w1", [128, NB], F32), ("d_w2", [128, NB], F32), ("d_te", [128, TM], I32), ("d_pvT", [128, 4 * 35], F32)):
            dbg[nm] = nc.dram_tensor(nm, shp, dt, kind="ExternalOutput")

    import contextlib
    es = contextlib.ExitStack()
    with es:
        def sb(name, shape, dt=F32):
            return es.enter_context(nc.sbuf_tensor(name, list(shape), dt))

        def psum(name, shape, dt=F32):
            return es.enter_context(nc.psum_tensor(name, list(shape), dt))

        sems = {}
        for e in Prog.ENGS:
            sems[e] = es.enter_context(nc.semaphore("s_" + e))
            sems["dma_" + e] = [es.enter_context(nc.semaphore(f"d_{e}{k}")) for k in range(P.n_dma_sems)]

        identf = sb("identf", [128, 128])
        identb = sb("identb", [128, 128], BF16)
        onesf = sb("onesf", [128, 128])
        onesb = sb("onesb", [128, 128], BF16)
        trib = sb("trib", [128, 128], BF16)
        trif = sb("trif", [128, 128])
        bavg = sb("bavg", [128, 128], BF16)
        bavgf = sb("bavgf", [128, 128])
        inv16 = sb("inv16", [128, 16])
        iota_i = sb("iota_i", [128, 128], I32)
        iota_f = sb("iota_f", [128, 128])

        P.op("pool", lambda E: E.memset(identf[:], 0.0), w=["identf"])
        P.op("pool", lambda E: E.affine_select(out=identf[:], in_=identf[:], pattern=[[-1, 128]], compare_op=ALU.not_equal,
                                               fill=1.0, base=0, channel_multiplier=1), r=["identf"], w=["identf"])
        P.op("pool", lambda E: E.tensor_copy(out=identb[:], in_=identf[:]), r=["identf"], w=["identb"])
        P.op("pool", lambda E: E.memset(onesf[:], 1.0), w=["onesf"])
        P.op("pool", lambda E: E.memset(onesb[:], 1.0), w=["onesb"])
        P.op("pool", lambda E: E.memset(trif[:], 1.0), w=["trif"])
        P.op("pool", lambda E: E.affine_select(out=trif[:], in_=trif[:], pattern=[[1, 128]], compare_op=ALU.is_gt,
                                               fill=0.0, base=0, channel_multiplier=-1), r=["trif"], w=["trif"])
        P.op("pool", lambda E: E.tensor_copy(out=trib[:], in_=trif[:]), r=["trif"], w=["trib"])
        P.op("pool", lambda E: E.memset(bavgf[:], 0.0), w=["bavgf"])
        P.op("pool", lambda E: E.memset(bavgf[0:64, 0:64], 1.0 / 64), r=["bavgf"], w=["bavgf"])
        P.op("pool", lambda E: E.memset(bavgf[64:128, 64:128], 1.0 / 64), r=["bavgf"], w=["bavgf"])
        P.op("pool", lambda E: E.tensor_copy(out=bavg[:], in_=bavgf[:]), r=["bavgf"], w=["bavg"])
        P.op("pool", lambda E: E.iota(iota_i[:], pattern=[[1, 128]], base=0, channel_multiplier=0), w=["iota_i"])
        P.op("pool", lambda E: E.tensor_copy(out=iota_f[:], in_=iota_i[:]), r=["iota_i"], w=["iota_f"])
        epsr = sb("epsr", [128, 1])
        epsl = sb("epsl", [128, 1])
        P.op("pool", lambda E: E.memset(epsr[:], D * RMS_EPS), w=["epsr"])
        P.op("pool", lambda E: E.memset(epsl[:], LN_EPS), w=["epsl"])
        pidx_i = sb("pidx_i", [128, 1], I32)
        pidx_f = sb("pidx_f", [128, 1])
        P.op("pool", lambda E: E.iota(pidx_i[:], pattern=[[0, 1]], base=0, channel_multiplier=1), w=["pidx_i"])
        P.op("pool", lambda E: E.tensor_copy(out=pidx_f[:], in_=pidx_i[:]), r=["pidx_i"], w=["pidx_f"])
        P.op("dve", lambda E: E.tensor_scalar(out=inv16[:], in0=iota_f[:, 0:16], scalar1=1.0, scalar2=None, op0=ALU.add),
             r=["iota_f"], w=["inv16"])
        P.op("dve", lambda E: E.reciprocal(out=inv16[:], in_=inv16[:]), r=["inv16"], w=["inv16"])

        NFB = 5
        psf = [psum(f"psf{i}", [128, 512]) for i in range(NFB)]
        psb = [psum(f"psb{i}", [128, 1024], BF16) for i in range(2)]
        psr = psum("psr", [128, 512])
        bank_ctr = [0, 0]

        def fbank():
            i = bank_ctr[0] % NFB
            bank_ctr[0] += 1
            return psf[i], f"psf{i}"

        def bbank():
            i = bank_ctr[1] % 2
            bank_ctr[1] += 1
            return psb[i], f"psb{i}"

        c_sb = sb("c_sb", [128, 8])
        cond = sb("cond", [128, 8])
        condbc = sb("condbc", [128, 8, 128])
        P.dma("sp", lambda E: E.dma_start(out=c_sb[:], in_=c_d.ap().rearrange("(c p) -> p c", p=128), allow_slow_non_contiguous=True),
              w=["c_sb"])
        P.op("act", lambda E: E.activation(out=cond[:], in_=c_sb[:], func=AF.Silu), r=["c_sb"], w=["cond"])
        for c in range(8):
            P.op("act", lambda E, c=c: E.activation(out=condbc[:, c, :], in_=onesf[:], func=AF.Copy, scale=cond[:, c:c + 1]),
                 r=["cond", "onesf"], w=["condbc"])

        mod = sb("mod", [128, 6 * D])
        oh1 = sb("oh1", [128, NB, NE], BF16)
        oh2 = sb("oh2", [128, NB, NE], BF16)
        Aall = sb("Aall", [128, NB, NE], BF16)
        w1 = sb("w1", [128, NB])
        w2 = sb("w2", [128, NB])
        pos1 = sb("pos1", [128, NB], I32)
        pos2 = sb("pos2", [128, NB], I32)
        te_i = sb("te_i", [128, TM], I32)

        B1, A1, G1, B2, A2, G2 = (mod[:, k * D:(k + 1) * D] for k in range(6))

        def layer(l):
            x_src = x_d if l == 0 else xs2
            last = (l == DEPTH_ - 1)
            with contextlib.ExitStack() as sc:
                P.barrier()
                def sbl(name, shape, dt=F32, sc=sc):
                    return sc.enter_context(nc.sbuf_tensor(f"{name}_{l}", list(shape), dt))
                aw = [sbl(f"aw{k}", [128, 8, 512]) for k in range(2)]
                ab = [sbl(f"ab{k}", [128, 512]) for k in range(2)]
                gbc = sbl("gbc", [128, D])
                for nn in range(12):
                    k = nn % 2
                    P.dma("sp", lambda E, nn=nn, k=k: E.dma_start(
                        out=aw[k][:], in_=ada_w[l, :, nn * 512:(nn + 1) * 512].rearrange("(c p) n -> p c n", p=128)),
                        w=[f"aw{k}"])
                    P.dma("sp", lambda E, nn=nn, k=k: E.dma_start(
                        out=ab[k][:], in_=ada_b[l, nn * 512:(nn + 1) * 512].partition_broadcast(128)), w=[f"ab{k}"])
                    pb, pbn = fbank()
                    for c in range(8):
                        P.op("pe", lambda E, c=c, pb=pb, k=k: E.matmul(pb[:], lhsT=condbc[:, c, :], rhs=aw[k][:, c, :],
                                                                      start=(c == 0), stop=(c == 7)),
                             r=["condbc", f"aw{k}"], w=[pbn])
                    P.op("dve", lambda E, nn=nn, pb=pb, k=k: E.tensor_tensor(out=mod[:, nn * 512:(nn + 1) * 512], in0=pb[:], in1=ab[k][:],
                                                                             op=ALU.add), r=[f"ab{k}"], w=[pbn, "mod"])
                for (gsrc, Aap) in ((norm1_g, A1), (norm2_g, A2)):
                    P.dma("sp", lambda E, gsrc=gsrc: E.dma_start(out=gbc[:], in_=gsrc[l, :].partition_broadcast(128)), w=["gbc"])
                    P.op("act", lambda E: E.mul(out=gbc[:], in_=gbc[:], mul=32.0), r=["gbc"], w=["gbc"])
                    P.op("dve", lambda E, Aap=Aap: E.scalar_tensor_tensor(out=Aap, in0=Aap, scalar=1.0, in1=gbc[:], op0=ALU.add,
                                                                          op1=ALU.mult), r=["gbc", "mod"], w=["mod"])
            with contextlib.ExitStack() as sc:
                P.barrier()
                def sbl(name, shape, dt=F32, sc=sc):
                    return sc.enter_context(nc.sbuf_tensor(f"{name}_{l}", list(shape), dt))
                win = sbl("win", [128, 8, 1536], BF16)
                wout = sbl("wout", [128, 8, D], BF16)
                pw = sbl("pw", [128, 4, 128], BF16)
                wr = sbl("wr", [128, 8, 36])
                rbias = sbl("rbias", [128, 4, 36])
                pv = sbl("pv", [35, 512])
                pvT = sbl("pvT", [128, 4, 35])
                diag = sbl("diag", [128, CK, 4, 128], BF16)
                xt = [sbl(f"xt{k}", [128, 4, D]) for k in range(1)]
                ss = sbl("ss", [128, 8])
                rr = sbl("rr", [128, 8])
                tmpA = [sbl(f"tmpA{k}", [128, D]) for k in range(2)]
                hb = sbl("hb", [128, 4, D], BF16)
                hT = sbl("hT", [128, 8, 512], BF16)
                u = sbl("u", [128, 4, 528])
                sA = sbl("sA", [128, 528])
                sB = sbl("sB", [128, 528])
                pooled = sbl("pooled", [128, 4, 512], BF16)
                ycat = sbl("ycat", [128, 8, 512], BF16)
                vbuf = sbl("vbuf", [128, 4, 544], BF16)
                sig = [sbl(f"sig{k}", [128, 512]) for k in range(1)]
                ybf = [sbl(f"ybf{k}", [128, 512], BF16) for k in range(1)]
                dd = [sbl(f"dd{k}", [128, 512]) for k in range(1)]
                sq = [sbl(f"sq{k}", [128, 512], BF16) for k in range(1)]
                rs = [sbl(f"rs{k}", [128, 512]) for k in range(1)]
                yn = dd
                tmpO = [sbl(f"tmpO{k}", [128, 512]) for k in range(2)]
                h2b = hb
                h2T = [sbl(f"h2T{k}", [128, 8, 128]) for k in range(1)]
                lg = sbl("lg", [128, 4, 36])
                gmax = sbl("gmax", [128, 4])
                gd = sbl("gd", [128, 4, 4])
                gsum = sbl("gsum", [128, 4])
                pgrp = sbl("pgrp", [128, 4])
                ohg = sbl("ohg", [128, 4, 4])
                em = sbl("em", [128, 4, NE])
                em2 = sbl("em2", [128, 4, NE])
                m1 = sbl("m1", [128, 4])
                m2 = sbl("m2", [128, 4])
                d12 = sbl("d12", [128, 4])

                for c in range(8):
                    P.dma("pool", lambda E, c=c: E.dma_start(out=win[:, c, :], in_=w_in[l, c * 128:(c + 1) * 128, :]), w=["win"])
                for c in range(8):
                    P.dma("pool", lambda E, c=c: E.dma_start(out=wout[:, c, :], in_=w_out[l, c * 128:(c + 1) * 128, :]), w=["wout"])
                P.dma("pool", lambda E: E.dma_start(out=pw[:], in_=pool_w[l].rearrange("g c d -> c g d")), w=["pw"])
                P.dma("sp", lambda E: E.dma_start(out=wr[:], in_=rw[l].rearrange("(c p) g -> p c g", p=128)), w=["wr"])
                for j in range(4):
                    P.dma("sp", lambda E, j=j: E.dma_start(out=rbias[:, j, :], in_=rb[l, :].partition_broadcast(128)), w=["rbias"])
                P.dma("sp", lambda E: E.dma_start(out=pv[:], in_=pvec[l]), w=["pv"])
                for c in range(4):
                    pb, pbn = fbank()
                    P.op("pe", lambda E, c=c, pb=pb: E.transpose(out=pb[:, 0:35], in_=pv[:, c * 128:(c + 1) * 128], identity=identf[0:35, 0:35]),
                         r=["pv", "identf"], w=[pbn])
                    P.op("act", lambda E, c=c, pb=pb: E.activation(out=pvT[:, c, :], in_=pb[:, 0:35], func=AF.Copy), w=[pbn, "pvT"])
                for k in range(CK):
                    for c in range(4):
                        eng = "dve" if (k * 4 + c) % 2 == 0 else "pool"
                        P.op(eng, lambda E, k=k, c=c: E.tensor_scalar(out=diag[:, k, c, :], in0=identf[:], scalar1=pvT[:, c, k:k + 1],
                                                                      scalar2=None, op0=ALU.mult),
                             r=["identf", "pvT"], w=[f"diag{k}_{c}"])
                P.op("pool", lambda E: E.memset(u[:], 0.0), w=["u0", "u1", "u2", "u3"])
                P.op("pool", lambda E: E.memset(vbuf[:], 0.0), w=["v0", "v1", "v2", "v3"])

                def mtile(i):
                    X = xt[0]
                    Xn = "xt0"
                    rows = slice(i * 512, (i + 1) * 512)
                    P.dma("sp", lambda E, X=X, rows=rows: E.dma_start(out=X[:], in_=x_src[rows, :].rearrange("(j p) d -> p j d", p=128)),
                          r=[f"xs2_{4 * i + q}" for q in range(4)], w=[Xn])
                    P.op("dve", lambda E: E.memset(ss[:], 0.0), w=["ss"])
                    for j in range(4):
                        P.op("act", lambda E, j=j, X=X: E.activation(out=hb[:, j, :], in_=X[:, j, :], func=AF.Square, accum_out=ss[:, j:j + 1]),
                             r=[Xn], w=[f"hb{j}", "ss"])
                    P.op("act", lambda E: E.activation(out=rr[:, 0:4], in_=ss[:, 0:4], func=AF.Sqrt, bias=epsr[:, 0:1]), r=["ss", "epsr"], w=["rr"])
                    P.op("dve", lambda E: E.reciprocal(out=rr[:, 0:4], in_=rr[:, 0:4]), r=["rr"], w=["rr"])
                    for j in range(4):
                        T = tmpA[j % 2]
                        Tn = f"tmpA{j % 2}"
                        P.op("dve", lambda E, j=j, X=X, T=T: E.scalar_tensor_tensor(out=T[:], in0=X[:, j, :], scalar=rr[:, j:j + 1], in1=A1,
                                                                                    op0=ALU.mult, op1=ALU.mult),
                             r=[Xn, "rr", "mod"], w=[Tn])
                        P.op("pool", lambda E, j=j, T=T: E.tensor_tensor(out=hb[:, j, :], in0=T[:], in1=B1, op=ALU.add),
                             r=[Tn, "mod"], w=[f"hb{j}"])
                    for j in range(4):
                        pb, pbn = bbank()
                        for c in range(8):
                            P.op("pe", lambda E, j=j, c=c, pb=pb: E.transpose(out=pb[:, c * 128:(c + 1) * 128], in_=hb[:, j, c * 128:(c + 1) * 128],
                                                                             identity=identb[:]),
                                 r=[f"hb{j}", "identb"], w=[pbn])
                        eng = "act" if j % 2 == 0 else "dve"
                        if eng == "act":
                            P.op("act", lambda E, j=j, pb=pb: E.activation(out=hT[:, :, j * 128:(j + 1) * 128],
                                                                           in_=pb[:].rearrange("p (c t) -> p c t", c=8), func=AF.Copy),
                                 w=[pbn, f"hT{j}"])
                        else:
                            P.op("dve", lambda E, j=j, pb=pb: E.tensor_copy(out=hT[:, :, j * 128:(j + 1) * 128],
                                                                            in_=pb[:].rearrange("p (c t) -> p c t", c=8)),
                                 w=[pbn, f"hT{j}"])
                    hTr = ["hT0", "hT1", "hT2", "hT3"]
                    if debug and l == 0 and i == 0:
                        P.dma("sp", lambda E: E.dma_start(out=dbg["d_hT"].ap(), in_=hT[:].rearrange("p c t -> p (c t)")), r=hTr, w=["dbg1"])
                        P.dma("sp", lambda E: E.dma_start(out=dbg["d_mod"].ap(), in_=mod[:]), r=["mod"], w=["dbg2"])
                        P.dma("sp", lambda E: E.dma_start(out=dbg["d_pvT"].ap(), in_=pvT[:].rearrange("p c t -> p (c t)")), r=["pvT"], w=["dbg2b"])

                    def win_mm(oc):
                        pb, pbn = fbank()
                        for c in range(8):
                            P.op("pe", lambda E, c=c, pb=pb, oc=oc: E.matmul(pb[:], lhsT=win[:, c, oc * 128:(oc + 1) * 128], rhs=hT[:, c, :],
                                                                           start=(c == 0), stop=(c == 7)),
                                 r=["win"] + hTr, w=[pbn])
                        return pb, pbn

                    def pool_chunk(g):
                        wdw = POOL_WINDOWS[g]
                        pb, pbn = win_mm(g)
                        un = f"u{g}"
                        P.op("act", lambda E, g=g, pb=pb: E.activation(out=u[:, g, 16:528], in_=pb[:], func=AF.Copy), w=[pbn, un])
                        src = None
                        step = 1
                        bufs = [sA, sB]
                        bi = 0
                        cur = None
                        lo = 0
                        while step < wdw:
                            dst = bufs[bi]
                            dn = "sA" if bi == 0 else "sB"
                            nlo = lo + step
                            if cur is None:
                                P.op("pool", lambda E, g=g, dst=dst, nlo=nlo, step=step: E.tensor_tensor(
                                    out=dst[:, nlo:528], in0=u[:, g, nlo:528], in1=u[:, g, nlo - step:528 - step], op=ALU.add),
                                    r=[un], w=[dn])
                            else:
                                cs, cn = cur
                                P.op("pool", lambda E, dst=dst, cs=cs, nlo=nlo, step=step: E.tensor_tensor(
                                    out=dst[:, nlo:528], in0=cs[:, nlo:528], in1=cs[:, nlo - step:528 - step], op=ALU.add),
                                    r=[cn], w=[dn])
                            cur = (dst, dn)
                            lo = nlo
                            step *= 2
                            bi ^= 1
                        cs, cn = cur
                        P.op("dve", lambda E, g=g, cs=cs, wdw=wdw: E.scalar_tensor_tensor(
                            out=pooled[:, g, :], in0=cs[:, 16:528], scalar=1.0 / wdw, in1=u[:, g, 16:528], op0=ALU.mult, op1=ALU.subtract),
                            r=[cn, un], w=[f"pooled{g}"])
                        if i == 0:
                            nfix = wdw - 1
                            P.op("pool", lambda E, cs=cs, nfix=nfix: E.tensor_tensor(out=cs[:, 16:16 + nfix], in0=cs[:, 16:16 + nfix],
                                                                                    in1=inv16[:, 0:nfix], op=ALU.mult),
                                 r=[cn, "inv16", f"pooled{g}"], w=[cn])
                            P.op("pool", lambda E, g=g, cs=cs, nfix=nfix: E.tensor_tensor(out=pooled[:, g, 0:nfix], in0=cs[:, 16:16 + nfix],
                                                                                         in1=u[:, g, 16:16 + nfix], op=ALU.subtract),
                                 r=[cn, un], w=[f"pooled{g}"])
                        P.op("pool", lambda E, g=g: E.tensor_copy(out=u[:, g, 0:16], in_=u[:, g, 512:528]), r=[un], w=[un])
                        pb2, pb2n = fbank()
                        P.op("pe", lambda E, g=g, pb2=pb2: E.matmul(pb2[:], lhsT=pw[:, g, :], rhs=pooled[:, g, :], start=True, stop=True),
                             r=["pw", f"pooled{g}"], w=[pb2n])
                        P.op("act", lambda E, g=g, pb2=pb2: E.activation(out=ycat[:, g, :], in_=pb2[:], func=AF.Copy, scale=pvT[:, g, 34:35]),
                             r=["pvT"], w=[pb2n, f"ycat{g}"])
                    for _g in range(4):
                        pool_chunk(_g)
                    def conv_chunk(c):
                        k2 = 0
                        vn = f"v{c}"
                        pa, pan = win_mm(4 + c)
                        pg, pgn = win_mm(8 + c)
                        P.op("act", lambda E, pg=pg, k2=k2: E.activation(out=sig[k2][:], in_=pg[:], func=AF.Sigmoid), w=[pgn, f"sig{k2}"])
                        P.op("dve", lambda E, c=c, pa=pa, k2=k2: E.tensor_tensor(out=vbuf[:, c, 32:544], in0=pa[:], in1=sig[k2][:], op=ALU.mult),
                             r=[f"sig{k2}"], w=[pan, vn])
                        py, pyn = fbank()
                        for k in range(CK):
                            P.op("pe", lambda E, c=c, k=k, py=py: E.matmul(py[:], lhsT=diag[:, k, c, :], rhs=vbuf[:, c, 2 + k:2 + k + 512],
                                                                         start=(k == 0), stop=(k == CK - 1)),
                                 r=[f"diag{k}_{c}", vn], w=[pyn])
                        P.op("pool", lambda E, c=c: E.tensor_copy(out=vbuf[:, c, 2:32], in_=vbuf[:, c, 514:544]), r=[vn], w=[vn])
                        P.op("act", lambda E, c=c, py=py, k2=k2: E.activation(out=ybf[k2][:], in_=py[:], func=AF.Identity, bias=pvT[:, c, 31:32]),
                             r=["pvT"], w=[pyn, f"ybf{k2}"])
                        pm, pmn = fbank()
                        P.op("pe", lambda E, pm=pm, k2=k2: E.matmul(pm[:], lhsT=bavg[:], rhs=ybf[k2][:], start=True, stop=True),
                             r=["bavg", f"ybf{k2}"], w=[pmn])
                        P.op("dve", lambda E, pm=pm, k2=k2: E.tensor_tensor(out=dd[k2][:], in0=ybf[k2][:], in1=pm[:], op=ALU.subtract),
                             r=[f"ybf{k2}"], w=[pmn, f"dd{k2}"])
                        P.op("act", lambda E, k2=k2: E.activation(out=sq[k2][:], in_=dd[k2][:], func=AF.Square), r=[f"dd{k2}"], w=[f"sq{k2}"])
                        pvv, pvn = fbank()
                        P.op("pe", lambda E, pvv=pvv, k2=k2: E.matmul(pvv[:], lhsT=bavg[:], rhs=sq[k2][:], start=True, stop=True),
                             r=["bavg", f"sq{k2}"], w=[pvn])
                        P.op("act", lambda E, pvv=pvv, k2=k2: E.activation(out=rs[k2][:], in_=pvv[:], func=AF.Sqrt, bias=epsl[:, 0:1]),
                             r=["epsl"], w=[pvn, f"rs{k2}"])
                        P.op("dve", lambda E, k2=k2: E.reciprocal(out=rs[k2][:], in_=rs[k2][:]), r=[f"rs{k2}"], w=[f"rs{k2}"])
                        P.op("pool", lambda E, k2=k2: E.tensor_tensor(out=yn[k2][:], in0=dd[k2][:], in1=rs[k2][:], op=ALU.mult),
                             r=[f"dd{k2}", f"rs{k2}"], w=[f"dd{k2}"])
                        P.op("act", lambda E, c=c, k2=k2: E.activation(out=ycat[:, 4 + c, :], in_=yn[k2][:], func=AF.Silu, bias=pvT[:, c, 33:34],
                                                                       scale=pvT[:, c, 32:33]),
                             r=[f"dd{k2}", "pvT"], w=[f"ycat{4 + c}"])
                    for _c in range(4):
                        conv_chunk(_c)
                    ycr = [f"ycat{k}" for k in range(8)]
                    if debug and l == 0 and i == 0:
                        P.dma("sp", lambda E: E.dma_start(out=dbg["d_ycat"].ap(), in_=ycat[:].rearrange("p c t -> p (c t)")), r=ycr, w=["dbg3"])
                    for j in range(4):
                        for dh in range(2):
                            po, pon = fbank()
                            for k in range(8):
                                P.op("pe", lambda E, j=j, dh=dh, k=k, po=po: E.matmul(po[:], lhsT=ycat[:, k, j * 128:(j + 1) * 128],
                                                                                  rhs=wout[:, k, dh * 512:(dh + 1) * 512], start=(k == 0), stop=(k == 7)),
                                     r=ycr + ["wout"], w=[pon])
                            kk = (j * 2 + dh) % 2
                            P.op("dve", lambda E, dh=dh, po=po, kk=kk: E.tensor_tensor(out=tmpO[kk][:], in0=po[:], in1=G1[:, dh * 512:(dh + 1) * 512],
                                                                                      op=ALU.mult), r=["mod"], w=[pon, f"tmpO{kk}"])
                            P.op("pool", lambda E, j=j, dh=dh, X=X, kk=kk: E.tensor_tensor(out=X[:, j, dh * 512:(dh + 1) * 512], in0=tmpO[kk][:],
                                                                                         in1=X[:, j, dh * 512:(dh + 1) * 512], op=ALU.add),
                                 r=[f"tmpO{kk}", Xn], w=[Xn])
                    P.dma("sp", lambda E, X=X, rows=rows: E.dma_start(out=xs1[rows, :].rearrange("(j p) d -> p j d", p=128), in_=X[:]),
                          r=[Xn], w=[f"xs1_{i}"])
                    for j in range(4):
                        P.op("act", lambda E, j=j, X=X: E.activation(out=hb[:, j, :], in_=X[:, j, :], func=AF.Square, accum_out=ss[:, 4 + j:5 + j]),
                             r=[Xn], w=[f"hb{j}", "ss"])
                    P.op("act", lambda E: E.activation(out=rr[:, 4:8], in_=ss[:, 4:8], func=AF.Sqrt, bias=epsr[:, 0:1]), r=["ss", "epsr"], w=["rr"])
                    P.op("dve", lambda E: E.reciprocal(out=rr[:, 4:8], in_=rr[:, 4:8]), r=["rr"], w=["rr"])
                    pr, prn = psr, "psr"
                    for j in range(4):
                        T = tmpA[j % 2]
                        Tn = f"tmpA{j % 2}"
                        H = T
                        Hn = Tn
                        P.op("dve", lambda E, j=j, X=X, T=T: E.scalar_tensor_tensor(out=T[:], in0=X[:, j, :], scalar=rr[:, 4 + j:5 + j], in1=A2,
                                                                                    op0=ALU.mult, op1=ALU.mult),
                             r=[Xn, "rr", "mod"], w=[Tn])
                        P.op("pool", lambda E, T=T, H=H: E.tensor_tensor(out=H[:], in0=T[:], in1=B2, op=ALU.add), r=[Tn, "mod"], w=[Hn])
                        P.op("act", lambda E, j=j, H=H: E.activation(out=h2b[:, j, :], in_=H[:], func=AF.Copy), r=[Hn], w=[f"hb{j}"])
                        HT = h2T[0]
                        HTn = "h2T0"
                        for half in range(2):
                            pt, ptn = fbank()
                            for cc in range(4):
                                c = half * 4 + cc
                                P.op("pe", lambda E, c=c, cc=cc, H=H, pt=pt: E.transpose(out=pt[:, cc * 128:(cc + 1) * 128],
                                                                                       in_=H[:, c * 128:(c + 1) * 128], identity=identf[:]),
                                     r=[Hn, "identf"], w=[ptn])
                            eng = "act" if half == 0 else "dve"
                            if eng == "act":
                                P.op("act", lambda E, half=half, HT=HT, pt=pt: E.activation(out=HT[:, half * 4:(half + 1) * 4, :],
                                                                                        in_=pt[:].rearrange("p (c t) -> p c t", c=4), func=AF.Copy),
                                     w=[ptn, HTn])
                            else:
                                P.op("dve", lambda E, half=half, HT=HT, pt=pt: E.tensor_copy(out=HT[:, half * 4:(half + 1) * 4, :],
                                                                                         in_=pt[:].rearrange("p (c t) -> p c t", c=4)),
                                     w=[ptn, HTn])
                        for c in range(8):
                            P.op("pe", lambda E, j=j, c=c, HT=HT, pr=pr: E.matmul(pr[:, j * 36:(j + 1) * 36], lhsT=HT[:, c, :], rhs=wr[:, c, :],
                                                                              start=(c == 0), stop=(c == 7)),
                                 r=[HTn, "wr"], w=[prn])
                    P.dma("sp", lambda E, rows=rows: E.dma_start(out=h2s[rows, :].rearrange("(j p) d -> p j d", p=128), in_=h2b[:]), r=["hb0", "hb1", "hb2", "hb3"], w=[f"h2s_{i}"])
                    bs = slice(i * 4, (i + 1) * 4)
                    P.op("dve", lambda E, pr=pr: E.tensor_tensor(out=lg[:], in0=pr[:, 0:144].rearrange("p (j e) -> p j e", j=4), in1=rbias[:],
                                                                 op=ALU.add), r=["rbias"], w=[prn, "lg"])
                    if debug and l == 0 and i == 0:
                        P.dma("sp", lambda E: E.dma_start(out=dbg["d_lg"].ap(), in_=lg[:].rearrange("p c t -> p (c t)")), r=["lg"], w=["dbg4"])
                    gl = lg[:, :, 0:4]
                    el = lg[:, :, 4:36]
                    P.op("dve", lambda E: E.tensor_reduce(out=gmax[:], in_=gl, axis=AX.X, op=ALU.max), r=["lg"], w=["gmax"])
                    P.op("dve", lambda E: E.tensor_tensor(out=gd[:], in0=gl, in1=gmax[:].unsqueeze(2).to_broadcast([128, 4, 4]), op=ALU.subtract),
                         r=["lg", "gmax"], w=["gd"])
                    P.op("dve", lambda E: E.tensor_tensor(out=ohg[:], in0=gl, in1=gmax[:].unsqueeze(2).to_broadcast([128, 4, 4]), op=ALU.is_equal),
                         r=["lg", "gmax"], w=["ohg"])
                    P.op("act", lambda E: E.activation(out=gd[:], in_=gd[:], func=AF.Exp), r=["gd"], w=["gd"])
                    P.op("dve", lambda E: E.tensor_reduce(out=gsum[:], in_=gd[:], axis=AX.X, op=ALU.add), r=["gd"], w=["gsum"])
                    P.op("dve", lambda E: E.reciprocal(out=pgrp[:], in_=gsum[:]), r=["gsum"], w=["pgrp"])
                    P.op("dve", lambda E: E.tensor_scalar(out=ohg[:], in0=ohg[:], scalar1=-1.0, scalar2=1e30, op0=ALU.add, op1=ALU.mult),
                         r=["ohg"], w=["ohg"])
                    P.op("dve", lambda E: E.tensor_tensor(out=em[:].rearrange("p j (g e) -> p j g e", g=4),
                                                          in0=el.rearrange("p j (g e) -> p j g e", g=4),
                                                          in1=ohg[:].unsqueeze(3).to_broadcast([128, 4, 4, 8]), op=ALU.add),
                         r=["lg", "ohg"], w=["em"])
                    P.op("dve", lambda E: E.tensor_reduce(out=m1[:], in_=em[:], axis=AX.X, op=ALU.max), r=["em"], w=["m1"])
                    P.op("dve", lambda E, bs=bs: E.tensor_tensor(out=oh1[:, bs, :], in0=em[:], in1=m1[:].unsqueeze(2).to_broadcast([128, 4, NE]),
                                                                op=ALU.is_equal), r=["em", "m1"], w=["oh1"])
                    P.op("dve", lambda E, bs=bs: E.scalar_tensor_tensor(out=em2[:], in0=oh1[:, bs, :], scalar=-1e30, in1=em[:], op0=ALU.mult,
                                                                       op1=ALU.add), r=["oh1", "em"], w=["em2"])
                    P.op("dve", lambda E: E.tensor_reduce(out=m2[:], in_=em2[:], axis=AX.X, op=ALU.max), r=["em2"], w=["m2"])
                    P.op("dve", lambda E, bs=bs: E.tensor_tensor(out=oh2[:, bs, :], in0=em2[:], in1=m2[:].unsqueeze(2).to_broadcast([128, 4, NE]),
                                                                op=ALU.is_equal), r=["em2", "m2"], w=["oh2"])
                    P.op("dve", lambda E: E.tensor_tensor(out=d12[:], in0=m1[:], in1=m2[:], op=ALU.subtract), r=["m1", "m2"], w=["d12"])
                    P.op("act", lambda E: E.activation(out=d12[:], in_=d12[:], func=AF.Sigmoid), r=["d12"], w=["d12"])
                    P.op("dve", lambda E, bs=bs: E.tensor_tensor(out=w1[:, bs], in0=d12[:], in1=pgrp[:], op=ALU.mult), r=["d12", "pgrp"], w=["w1"])
                    P.op("dve", lambda E, bs=bs: E.tensor_tensor(out=w2[:, bs], in0=pgrp[:], in1=w1[:, bs], op=ALU.subtract),
                         r=["pgrp", "w1"], w=["w2"])
                    P.op("dve", lambda E, bs=bs: E.tensor_tensor(out=Aall[:, bs, :], in0=oh1[:, bs, :], in1=oh2[:, bs, :], op=ALU.add),
                         r=["oh1", "oh2"], w=["Aall"])
                for _i in range(NT):
                    mtile(_i)

            with contextlib.ExitStack() as sc:
                P.barrier()
                def sbl(name, shape, dt=F32, sc=sc):
                    return sc.enter_context(nc.sbuf_tensor(f"{name}_{l}", list(shape), dt))
                NC_ = NB * NE
                within = sbl("within", [128, NB, NE])
                tot = [sbl(f"tot{k}", [128, NB, NE]) for k in range(2)]
                tot0 = sbl("totz", [128, NB, NE])
                cmpb = sbl("cmpb", [128, NE, NB])
                ntile = [sbl(f"ntile{k}", [128, NE]) for k in range(2)]
                nt0 = sbl("ntz", [128, NE])
                basee = sbl("basee", [128, NE])
                tend = sbl("tend", [128, NE])
                posall = sbl("posall", [128, NB, NE])
                ptmp = sbl("ptmp", [128, NB, NE])
                posf = sbl("posf", [128, NB])
                thr = sbl("thr", [128, NB])
                cmpt = sbl("cmpt", [128, TM, NE])
                tef = sbl("tef", [128, TM])
                Af = Aall[:].rearrange("p b e -> p (b e)")
                for h0 in range(0, NC_, 512):
                    wd_ = min(512, NC_ - h0)
                    pbw, pbwn = fbank()
                    P.op("pe", lambda E, h0=h0, wd_=wd_, pbw=pbw: E.matmul(pbw[:, 0:wd_], lhsT=trib[:], rhs=Af[:, h0:h0 + wd_], start=True, stop=True),
                         r=["trib", "Aall"], w=[pbwn])
                    P.op("act", lambda E, h0=h0, wd_=wd_, pbw=pbw: E.activation(out=within[:].rearrange("p b e -> p (b e)")[:, h0:h0 + wd_],
                                                                           in_=pbw[:, 0:wd_], func=AF.Copy), w=[pbwn, "within"])
                    pbt, pbtn = fbank()
                    P.op("pe", lambda E, h0=h0, wd_=wd_, pbt=pbt: E.matmul(pbt[:, 0:wd_], lhsT=onesb[:], rhs=Af[:, h0:h0 + wd_], start=True, stop=True),
                         r=["onesb", "Aall"], w=[pbtn])
                    P.op("dve", lambda E, h0=h0, wd_=wd_, pbt=pbt: E.tensor_copy(out=tot0[:].rearrange("p b e -> p (b e)")[:, h0:h0 + wd_],
                                                                            in_=pbt[:, 0:wd_]), w=[pbtn, "tot0"])
                cur, curn = tot0, "tot0"
                step = 1
                bi = 0
                while step < NB:
                    dst, dn = tot[bi], f"tot{bi}"
                    P.op("dve", lambda E, dst=dst, cur=cur, step=step: E.tensor_copy(out=dst[:, 0:step, :], in_=cur[:, 0:step, :]), r=[curn], w=[dn])
                    P.op("dve", lambda E, dst=dst, cur=cur, step=step: E.tensor_tensor(out=dst[:, step:NB, :], in0=cur[:, step:NB, :],
                                                                                   in1=cur[:, 0:NB - step, :], op=ALU.add), r=[curn], w=[dn])
                    cur, curn = dst, dn
                    step *= 2
                    bi ^= 1
                incl, incln = cur, curn
                P.op("dve", lambda E: E.tensor_tensor(out=posall[:], in0=within[:], in1=incl[:], op=ALU.add), r=["within", incln], w=["posall"])
                P.op("dve", lambda E: E.tensor_tensor(out=posall[:], in0=posall[:], in1=tot0[:], op=ALU.subtract), r=["posall", "tot0"], w=["posall"])
                cnt = incl[:, NB - 1, :]
                P.op("dve", lambda E: E.tensor_scalar(out=thr[:], in0=iota_f[:, 0:NB], scalar1=128.0, scalar2=None, op0=ALU.mult),
                     r=["iota_f"], w=["thr"])
                P.op("dve", lambda E: E.tensor_tensor(out=cmpb[:], in0=cnt.unsqueeze(2).to_broadcast([128, NE, NB]),
                                                      in1=thr[:].unsqueeze(1).to_broadcast([128, NE, NB]), op=ALU.is_gt),
                     r=[incln, "thr"], w=["cmpb"])
                P.op("dve", lambda E: E.tensor_reduce(out=nt0[:], in_=cmpb[:], axis=AX.X, op=ALU.add), r=["cmpb"], w=["nt0"])
                cur, curn = nt0, "nt0"
                step = 1
                bi = 0
                while step < NE:
                    dst, dn = ntile[bi], f"ntile{bi}"
                    P.op("dve", lambda E, dst=dst, cur=cur, step=step: E.tensor_copy(out=dst[:, 0:step], in_=cur[:, 0:step]), r=[curn], w=[dn])
                    P.op("dve", lambda E, dst=dst, cur=cur, step=step: E.tensor_tensor(out=dst[:, step:NE], in0=cur[:, step:NE],
                                                                                   in1=cur[:, 0:NE - step], op=ALU.add), r=[curn], w=[dn])
                    cur, curn = dst, dn
                    step *= 2
                    bi ^= 1
                P.op("dve", lambda E, cur=cur: E.tensor_copy(out=tend[:], in_=cur[:]), r=[curn], w=["tend"])
                P.op("dve", lambda E: E.tensor_tensor(out=basee[:], in0=tend[:], in1=nt0[:], op=ALU.subtract), r=["tend", "nt0"], w=["basee"])
                P.op("dve", lambda E: E.tensor_scalar(out=basee[:], in0=basee[:], scalar1=128.0, scalar2=None, op0=ALU.mult), r=["basee"], w=["basee"])
                P.op("dve", lambda E: E.tensor_tensor(out=posall[:], in0=posall[:], in1=basee[:].unsqueeze(1).to_broadcast([128, NB, NE]), op=ALU.add),
                     r=["posall", "basee"], w=["posall"])
                for (ohk, posk, nm) in ((oh1, pos1, "pos1"), (oh2, pos2, "pos2")):
                    P.op("dve", lambda E, ohk=ohk: E.tensor_tensor(out=ptmp[:], in0=ohk[:], in1=posall[:], op=ALU.mult),
                         r=["oh1", "oh2", "posall"], w=["ptmp"])
                    P.op("dve", lambda E: E.tensor_reduce(out=posf[:], in_=ptmp[:], axis=AX.X, op=ALU.add), r=["ptmp"], w=["posf"])
                    P.op("dve", lambda E, posk=posk: E.tensor_copy(out=posk[:], in_=posf[:]), r=["posf"], w=[nm])
                for j0 in range(0, TM, 128):
                    jn = min(128, TM - j0)
                    P.op("dve", lambda E, j0=j0, jn=jn: E.tensor_scalar(out=tef[:, j0:j0 + jn], in0=iota_f[:, 0:jn], scalar1=float(j0), scalar2=None,
                                                                       op0=ALU.add), r=["iota_f"], w=["tef"])
                P.op("dve", lambda E: E.tensor_tensor(out=cmpt[:], in0=tend[:].unsqueeze(1).to_broadcast([128, TM, NE]),
                                                      in1=tef[:].unsqueeze(2).to_broadcast([128, TM, NE]), op=ALU.is_le),
                     r=["tend", "tef"], w=["cmpt"])
                P.op("dve", lambda E: E.tensor_reduce(out=tef[:], in_=cmpt[:], axis=AX.X, op=ALU.add), r=["cmpt"], w=["tef"])
                P.op("dve", lambda E: E.tensor_scalar(out=tef[:], in0=tef[:], scalar1=float(NE - 1), scalar2=None, op0=ALU.min), r=["tef"], w=["tef"])
                P.op("dve", lambda E: E.tensor_scalar(out=tef[:], in0=tef[:], scalar1=128.0, scalar2=pidx_f[:, 0:1], op0=ALU.mult, op1=ALU.add),
                     r=["tef", "pidx_f"], w=["tef"])
                P.op("dve", lambda E: E.tensor_copy(out=te_i[:], in_=tef[:]), r=["tef"], w=["te_i"])

            if debug and l == 0:
                for nm, t_, rn in (("d_pos1", pos1, "pos1"), ("d_pos2", pos2, "pos2"), ("d_w1", w1, "w1"), ("d_w2", w2, "w2"), ("d_te", te_i, "te_i")):
                    P.dma("sp", lambda E, nm=nm, t_=t_: E.dma_start(out=dbg[nm].ap(), in_=t_[:]), r=[rn], w=["dbg_" + nm])
            with contextlib.ExitStack() as sc:
                P.barrier()
                def sbl(name, shape, dt=F32, sc=sc):
                    return sc.enter_context(nc.sbuf_tensor(f"{name}_{l}", list(shape), dt))
                hrow = [sbl(f"hrow{k}", [128, D], BF16) for k in range(3)]
                def sblock(b):
                    k = b % 3
                    P.dma("sp", lambda E, b=b, k=k: E.dma_start(out=hrow[k][:], in_=h2s[b * 128:(b + 1) * 128, :]), r=[f"h2s_{b // 4}"], w=[f"hrow{k}"])
                    for (posk, nm) in ((pos1, "pos1"), (pos2, "pos2")):
                        P.dma("pool", lambda E, b=b, k=k, posk=posk: E.indirect_dma_start(
                            out=hsort[:, :], out_offset=bass.IndirectOffsetOnAxis(ap=posk[:, b:b + 1], axis=0),
                            in_=hrow[k][:], in_offset=None), r=[f"hrow{k}", nm], w=[f"hsortw{b}"])
                for _b in range(NB):
                    sblock(_b)
            with contextlib.ExitStack() as sc:
                P.barrier()
                def sbl(name, shape, dt=F32, sc=sc):
                    return sc.enter_context(nc.sbuf_tensor(f"{name}_{l}", list(shape), dt))
                NW = 3
                wgb = [sbl(f"wgb{k}", [128, 8, HID], BF16) for k in range(NW)]
                wub = [sbl(f"wub{k}", [128, 8, HID], BF16) for k in range(NW)]
                wdb = [sbl(f"wdb{k}", [128, 2, D], BF16) for k in range(NW)]
                stt = [sbl(f"stt{k}", [128, D], BF16) for k in range(2)]
                hsT = [sbl(f"hsT{k}", [128, 8, 128], BF16) for k in range(2)]
                sgl = [sbl(f"sgl{k}", [128, 256]) for k in range(2)]
                hid = [sbl(f"hid{k}", [128, 2, 128], BF16) for k in range(2)]
                yo = [sbl(f"yo{k}", [128, D]) for k in range(2)]
                allsc = [f"hsortw{b}" for b in range(NB)]
                def etile(j):
                    k = j % NW
                    k2 = j % 2

                    def ldw(E, j=j, k=k):
                        return E.indirect_dma_start(out=wgb[k][:].rearrange("p c h -> p (c h)"), out_offset=None, in_=wg_d[l][:, :],
                                                    in_offset=bass.IndirectOffsetOnAxis(ap=te_i[:, j:j + 1], axis=0))

                    def ldu(E, j=j, k=k):
                        return E.indirect_dma_start(out=wub[k][:].rearrange("p c h -> p (c h)"), out_offset=None, in_=wu_d[l][:, :],
                                                    in_offset=bass.IndirectOffsetOnAxis(ap=te_i[:, j:j + 1], axis=0))

                    def ldd(E, j=j, k=k):
                        return E.indirect_dma_start(out=wdb[k][:].rearrange("p c h -> p (c h)"), out_offset=None, in_=wd_d[l][:, :],
                                                    in_offset=bass.IndirectOffsetOnAxis(ap=te_i[:, j:j + 1], axis=0))
                    P.dma("pool", ldw, r=["te_i"], w=[f"wgb{k}"])
                    P.dma("pool", ldu, r=["te_i"], w=[f"wub{k}"])
                    P.dma("pool", ldd, r=["te_i"], w=[f"wdb{k}"])
                    P.dma("sp", lambda E, j=j, k2=k2: E.dma_start(out=stt[k2][:], in_=hsort[j * 128:(j + 1) * 128, :]), r=allsc, w=[f"stt{k2}"])
                    pb, pbn = bbank()
                    for c in range(8):
                        P.op("pe", lambda E, c=c, pb=pb, k2=k2: E.transpose(out=pb[:, c * 128:(c + 1) * 128], in_=stt[k2][:, c * 128:(c + 1) * 128],
                                                                          identity=identb[:]), r=[f"stt{k2}", "identb"], w=[pbn])
                    P.op("act", lambda E, pb=pb, k2=k2: E.activation(out=hsT[k2][:], in_=pb[:].rearrange("p (c t) -> p c t", c=8), func=AF.Copy),
                         w=[pbn, f"hsT{k2}"])
                    pgu, pgun = fbank()
                    for q in range(4):
                        W = wgb[k] if q < 2 else wub[k]
                        Wn = (f"wgb{k}" if q < 2 else f"wub{k}")
                        hc = q % 2
                        for c in range(8):
                            P.op("pe", lambda E, q=q, c=c, W=W, hc=hc, pgu=pgu, k2=k2: E.matmul(
                                pgu[:, q * 128:(q + 1) * 128], lhsT=W[:, c, hc * 128:(hc + 1) * 128], rhs=hsT[k2][:, c, :],
                                start=(c == 0), stop=(c == 7)), r=[Wn, f"hsT{k2}"], w=[pgun])
                    P.op("act", lambda E, pgu=pgu, k2=k2: E.activation(out=sgl[k2][:], in_=pgu[:, 0:256], func=AF.Silu), w=[pgun, f"sgl{k2}"])
                    P.op("dve", lambda E, pgu=pgu, k2=k2: E.tensor_tensor(out=hid[k2][:].rearrange("p c t -> p (c t)"), in0=pgu[:, 256:512],
                                                                         in1=sgl[k2][:], op=ALU.mult), r=[f"sgl{k2}"], w=[pgun, f"hid{k2}"])
                    for dh in range(2):
                        pd, pdn = fbank()
                        for kc in range(2):
                            P.op("pe", lambda E, dh=dh, kc=kc, pd=pd, k=k, k2=k2: E.matmul(pd[:], lhsT=hid[k2][:, kc, :],
                                                                                      rhs=wdb[k][:, kc, dh * 512:(dh + 1) * 512],
                                                                                      start=(kc == 0), stop=(kc == 1)),
                                 r=[f"hid{k2}", f"wdb{k}"], w=[pdn])
                        if dh == 0:
                            P.op("act", lambda E, pd=pd, k2=k2: E.activation(out=yo[k2][:, 0:512], in_=pd[:], func=AF.Copy), w=[pdn, f"yo{k2}"])
                        else:
                            P.op("dve", lambda E, pd=pd, k2=k2: E.tensor_copy(out=yo[k2][:, 512:1024], in_=pd[:]), w=[pdn, f"yo{k2}"])
                    P.dma("sp", lambda E, j=j, k2=k2: E.dma_start(out=ysort[j * 128:(j + 1) * 128, :], in_=yo[k2][:]), r=[f"yo{k2}"], w=[f"ysortw{j}"])
                for _j in range(TM):
                    etile(_j)

            with contextlib.ExitStack() as sc:
                P.barrier()
                def sbl(name, shape, dt=F32, sc=sc):
                    return sc.enter_context(nc.sbuf_tensor(f"{name}_{l}", list(shape), dt))
                r1 = [sbl(f"r1{k}", [128, D]) for k in range(2)]
                r2 = [sbl(f"r2{k}", [128, D]) for k in range(2)]
                xc = [sbl(f"xc{k}", [128, D]) for k in range(2)]
                fgb = sbl("fgb", [128, D])
                ssf = sbl("ssf", [128, NB])
                rrf = sbl("rrf", [128, NB])
                junk2 = sbl("junk2", [128, D], BF16)
                ally = [f"ysortw{j}" for j in range(TM)]
                if last:
                    P.dma("sp", lambda E: E.dma_start(out=fgb[:], in_=final_g.ap().partition_broadcast(128)), w=["fgb"])
                    P.op("act", lambda E: E.mul(out=fgb[:], in_=fgb[:], mul=32.0), r=["fgb"], w=["fgb"])
                    P.op("dve", lambda E: E.memset(ssf[:], 0.0), w=["ssf"])
                def cblock(b):
                    k = b % 2
                    rows = slice(b * 128, (b + 1) * 128)
                    P.dma("pool", lambda E, b=b, k=k: E.indirect_dma_start(out=r1[k][:], out_offset=None, in_=ysort[:, :],
                                                                          in_offset=bass.IndirectOffsetOnAxis(ap=pos1[:, b:b + 1], axis=0)),
                          r=ally + ["pos1"], w=[f"r1{k}"])
                    P.dma("pool", lambda E, b=b, k=k: E.indirect_dma_start(out=r2[k][:], out_offset=None, in_=ysort[:, :],
                                                                          in_offset=bass.IndirectOffsetOnAxis(ap=pos2[:, b:b + 1], axis=0)),
                          r=ally + ["pos2"], w=[f"r2{k}"])
                    P.dma("sp", lambda E, k=k, rows=rows: E.dma_start(out=xc[k][:], in_=xs1[rows, :]), r=[f"xs1_{b // 4}"], w=[f"xc{k}"])
                    P.op("act", lambda E, b=b, k=k: E.activation(out=r1[k][:], in_=r1[k][:], func=AF.Copy, scale=w1[:, b:b + 1]),
                         r=[f"r1{k}", "w1"], w=[f"r1{k}"])
                    P.op("dve", lambda E, b=b, k=k: E.scalar_tensor_tensor(out=r2[k][:], in0=r2[k][:], scalar=w2[:, b:b + 1], in1=r1[k][:],
                                                                          op0=ALU.mult, op1=ALU.add), r=[f"r2{k}", f"r1{k}", "w2"], w=[f"r2{k}"])
                    P.op("pool", lambda E, k=k: E.tensor_tensor(out=r2[k][:], in0=r2[k][:], in1=G2, op=ALU.mult), r=[f"r2{k}", "mod"], w=[f"r2{k}"])
                    P.op("dve", lambda E, k=k: E.tensor_tensor(out=xc[k][:], in0=xc[k][:], in1=r2[k][:], op=ALU.add), r=[f"xc{k}", f"r2{k}"],
                         w=[f"xc{k}"])
                    if not last:
                        P.dma("sp", lambda E, k=k, rows=rows: E.dma_start(out=xs2[rows, :], in_=xc[k][:]), r=[f"xc{k}"], w=[f"xs2_{b}"])
                    else:
                        P.op("act", lambda E, b=b, k=k: E.activation(out=junk2[:], in_=xc[k][:], func=AF.Square, accum_out=ssf[:, b:b + 1]),
                             r=[f"xc{k}"], w=["junk2", "ssf"])
                        P.op("act", lambda E, b=b: E.activation(out=rrf[:, b:b + 1], in_=ssf[:, b:b + 1], func=AF.Sqrt, bias=epsr[:, 0:1]),
                             r=["ssf", "epsr"], w=["rrf"])
                        P.op("dve", lambda E, b=b: E.reciprocal(out=rrf[:, b:b + 1], in_=rrf[:, b:b + 1]), r=["rrf"], w=["rrf"])
                        P.op("dve", lambda E, b=b, k=k: E.scalar_tensor_tensor(out=xc[k][:], in0=xc[k][:], scalar=rrf[:, b:b + 1], in1=fgb[:],
                                                                              op0=ALU.mult, op1=ALU.mult), r=[f"xc{k}", "rrf", "fgb"], w=[f"xc{k}"])
                        P.dma("sp", lambda E, k=k, rows=rows: E.dma_start(out=out_d[rows, :], in_=xc[k][:]), r=[f"xc{k}"], w=[f"out_{b}"])
                for _b in range(NB):
                    cblock(_b)
        for _l in range(DEPTH_):
            layer(_l)
        P.op("sp", lambda E: E.nop(), r=[f"out_{b}" for b in range(NB)] + [k for k in ("dbg1", "dbg2", "dbg2b", "dbg3", "dbg4", "dbg_d_pos1",
             "dbg_d_pos2", "dbg_d_w1", "dbg_d_w2", "dbg_d_te")], w=["done"])
        P.emit(sems)
    return nc


_CACHE = {}


def _prep(inputs, S):
    f = lambda a: np.ascontiguousarray(np.asarray(a, dtype=np.float32))
    pvec = np.concatenate([f(inputs["conv_w"]), f(inputs["conv_b"])[:, None, :], f(inputs["conv_ln_g"])[:, None, :],
                           f(inputs["conv_ln_b"])[:, None, :], f(inputs["pool_scale"])[:, None, :]], axis=1)
    rw = np.concatenate([f(inputs["router_group_w"]), f(inputs["router_expert_w"])], axis=2)
    rb = np.concatenate([f(inputs["router_group_b"]), f(inputs["router_expert_b"])], axis=1)
    shared = dict(ada_w=f(inputs["ada_w"]), ada_b=f(inputs["ada_b"]), norm1_g=f(inputs["norm1_g"]), w_in=f(inputs["w_in"]),
                  pool_w=f(inputs["pool_w"]), pvec=np.ascontiguousarray(pvec), w_out=f(inputs["w_out"]), norm2_g=f(inputs["norm2_g"]),
                  rw=np.ascontiguousarray(rw), rb=np.ascontiguousarray(rb), final_g=f(inputs["final_g"]))
    for nm, key, nch in (("wg", "expert_w_gate", 8), ("wu", "expert_w_up", 8), ("wd", "expert_w_down", 2)):
        w = f(inputs[key])
        L, E_, K, F_ = w.shape
        wr_ = w.reshape(L, E_, nch, 128, F_).transpose(0, 1, 3, 2, 4).reshape(L, E_ * 128, nch * F_)
        for l in range(L):
            shared[f"{nm}{l}"] = np.ascontiguousarray(wr_[l])
    x = f(inputs["x"])
    c = f(inputs["c"])
    in_maps = []
    for b in range(x.shape[0]):
        m = dict(shared)
        m["x"] = np.ascontiguousarray(x[b])
        m["c"] = np.ascontiguousarray(c[b])
        in_maps.append(m)
    return in_maps


def kernel(**inputs):
    x = np.asarray(inputs["x"])
    B, S, _ = x.shape
    assert B == N_CORES
    if S not in _CACHE:
        _CACHE[S] = build(S)
    nc = _CACHE[S]
    in_maps = _prep(inputs, S)
    res = run_bass_kernel_spmd(nc, in_maps, core_ids=list(range(B)))
    return np.stack([np.asarray(r["out"], dtype=np.float32) for r in res.results], axis=0)
```

```python
import numpy as np
import concourse.bass as bass
import concourse.mybir as mybir
from concourse.bass_utils import run_bass_kernel_spmd

F32 = mybir.dt.float32
BF16 = mybir.dt.bfloat16
I32 = mybir.dt.int32
AF = mybir.ActivationFunctionType
ALU = mybir.AluOpType
AX = mybir.AxisListType

D = 1024
DEPTH = 2
NE = 32
HID = 256
RMS_EPS = 1e-6
LN_EPS = 1e-5
POOL_WINDOWS = (2, 4, 8, 16)
CK = 31
N_CORES = 8


class Prog:
    ENGS = ("pe", "act", "dve", "pool", "sp")

    def __init__(self, nc, n_dma_sems=6):
        self.nc = nc
        self.ops = []
        self.n_dma_sems = n_dma_sems

    def op(self, eng, fn, r=(), w=()):
        self.ops.append(dict(eng=eng, fn=fn, r=tuple(r), w=tuple(w), dma=False))

    def dma(self, q, fn, r=(), w=()):
        self.ops.append(dict(eng=q, fn=fn, r=tuple(r), w=tuple(w), dma=True))

    def barrier(self):
        self.ops.append(dict(eng=None, fn=None, r=(), w=(), dma=False, barrier=True))

    def emit(self, sems):
        nc = self.nc
        engobj = dict(pe=nc.tensor, act=nc.scalar, dve=nc.vector, pool=nc.gpsimd, sp=nc.sync)
        ops = self.ops
        n = len(ops)
        last_w = {}
        readers = {}
        clock = {e: dict(pe=-1, act=-1, dve=-1, pool=-1, sp=-1, dma=set()) for e in self.ENGS}
        opclock = [None] * n
        dma_hist = {e: [] for e in self.ENGS}
        waits = [None] * n
        sig = [False] * n
        bar_deps = set()
        last_on = {}
        for i, o in enumerate(ops):
            e = o["eng"]
            if e is None:
                bar_deps = set(last_on.values())
                for q in self.ENGS:
                    bar_deps |= set(dma_hist[q][-self.n_dma_sems:])
                waits[i] = []
                opclock[i] = None
                continue
            deps = set(bar_deps)
            for r in o["r"]:
                if r in last_w:
                    deps.add(last_w[r])
            for w in o["w"]:
                if w in last_w:
                    deps.add(last_w[w])
                for j in readers.get(w, ()):
                    deps.add(j)
            if o["dma"]:
                h = dma_hist[e]
                if len(h) >= self.n_dma_sems:
                    deps.add(h[-self.n_dma_sems])
                h.append(i)
            deps.discard(i)
            need = []
            ck = clock[e]
            for j in sorted(deps):
                pj = ops[j]
                if pj["dma"]:
                    if j in ck["dma"]:
                        continue
                else:
                    if pj["eng"] == e and e == "pe" and not o["dma"]:
                        continue
                    if ck[pj["eng"]] >= j:
                        continue
                need.append(j)
                sig[j] = True
                oc = opclock[j]
                for k in ("pe", "act", "dve", "pool", "sp"):
                    if oc[k] > ck[k]:
                        ck[k] = oc[k]
                ck["dma"] |= oc["dma"]
            waits[i] = need
            oc = dict(pe=ck["pe"], act=ck["act"], dve=ck["dve"], pool=ck["pool"], sp=ck["sp"], dma=set(ck["dma"]))
            if o["dma"]:
                oc["dma"].add(i)
            else:
                oc[e] = i
            opclock[i] = oc
            if not o["dma"]:
                last_on[e] = i
            for r in o["r"]:
                readers.setdefault(r, []).append(i)
            for w in o["w"]:
                last_w[w] = i
                readers[w] = []
        cnt = {e: 0 for e in self.ENGS}
        dcnt = {}
        dma_n = {e: 0 for e in self.ENGS}
        ev = [None] * n
        for i, o in enumerate(ops):
            e = o["eng"]
            if e is None:
                continue
            E = engobj[e]
            for j in waits[i]:
                s, v = ev[j]
                E.wait_ge(s, v)
            inst = o["fn"](E)
            if o["dma"]:
                k = dma_n[e] % self.n_dma_sems
                dma_n[e] += 1
                s = sems["dma_" + e][k]
                dcnt[(e, k)] = dcnt.get((e, k), 0) + 16
                inst.then_inc(s, 16)
                ev[i] = (s, dcnt[(e, k)])
            elif sig[i]:
                cnt[e] += 1
                inst.then_inc(sems[e], 1)
                ev[i] = (sems[e], cnt[e])
        return ev


def build(S, depth=DEPTH, debug=False):
    DEPTH_ = depth
    NB = S // 128
    NT = S // 512
    SL = 256
    NB2 = (S + SL - 1) // SL
    TM = 2 * S // SL + NE
    nc = bass.Bass("TRN2", target_bir_lowering=False)
    P = Prog(nc)

    def dram_in(name, shape, dt=F32):
        return nc.dram_tensor(name, list(shape), dt, kind="ExternalInput")

    x_d = dram_in("x", [S, D])
    c_d = dram_in("c", [D])
    ada_w = dram_in("ada_w", [DEPTH, D, 6 * D])
    ada_b = dram_in("ada_b", [DEPTH, 6 * D])
    norm1_g = dram_in("norm1_g", [DEPTH, D])
    w_in = dram_in("w_in", [DEPTH, D, 1536])
    pool_w = dram_in("pool_w", [DEPTH, 4, 128, 128])
    pvec = dram_in("pvec", [DEPTH, 35, 512])
    w_out = dram_in("w_out", [DEPTH, D, D])
    norm2_g = dram_in("norm2_g", [DEPTH, D])
    rw = dram_in("rw", [DEPTH, D, 36])
    rb = dram_in("rb", [DEPTH, 36])
    wg_d = [dram_in(f"wg{l}", [NE * 128, 8 * HID]) for l in range(DEPTH)]
    wu_d = [dram_in(f"wu{l}", [NE * 128, 8 * HID]) for l in range(DEPTH)]
    wd_d = [dram_in(f"wd{l}", [NE * 128, 2 * D]) for l in range(DEPTH)]
    final_g = dram_in("final_g", [D])
    out_d = nc.dram_tensor("out", [S, D], F32, kind="ExternalOutput")
    sk = "ExternalOutput" if debug else "Internal"
    xs1 = nc.dram_tensor("xs1", [S, D], F32, kind=sk)
    xs2 = nc.dram_tensor("xs2", [S, D], F32, kind=sk)
    h2s = nc.dram_tensor("h2s", [S, D], BF16, kind=sk)
    hsort = nc.dram_tensor("hsort", [TM * SL, D], BF16, kind=sk)
    ysort = nc.dram_tensor("ysort", [TM * SL, D], F32, kind=sk)
    dbg = {}
    if debug:
        for nm, shp, dt in (("d_mod", [128, 6 * D], F32), ("d_hT", [128, 8 * 512], BF16), ("d_ycat", [128, 8 * 512], BF16),
                            ("d_lg", [128, 4 * 36], F32), ("d_pos1", [128, NB], I32), ("d_pos2", [128, NB], I32),
                            ("d_w1", [128, NB], F32), ("d_w2", [128, NB], F32), ("d_te", [128, TM], I32), ("d_pvT", [128, 4 * 35], F32)):
            dbg[nm] = nc.dram_tensor(nm, shp, dt, kind="ExternalOutput")

    import contextlib
    es = contextlib.ExitStack()
    with es:
        def sb(name, shape, dt=F32):
            return es.enter_context(nc.sbuf_tensor(name, list(shape), dt))

        def psum(name, shape, dt=F32):
            return es.enter_context(nc.psum_tensor(name, list(shape), dt))

        sems = {}
        for e in Prog.ENGS:
            sems[e] = es.enter_context(nc.semaphore("s_" + e))
            sems["dma_" + e] = [es.enter_context(nc.semaphore(f"d_{e}{k}")) for k in range(P.n_dma_sems)]

        identf = sb("identf", [128, 128])
        identb = sb("identb", [128, 128], BF16)
        onesf = sb("onesf", [128, 128])
        onesb = sb("onesb", [128, 128], BF16)
        trib = sb("trib", [128, 128], BF16)
        trif = sb("trif", [128, 128])
        bavg = sb("bavg", [128, 128], BF16)
        bavgf = sb("bavgf", [128, 128])
        inv16 = sb("inv16", [128, 16])
        iota_i = sb("iota_i", [128, 128], I32)
        iota_f = sb("iota_f", [128, 128])

        P.op("pool", lambda E: E.memset(identf[:], 0.0), w=["identf"])
        P.op("pool", lambda E: E.affine_select(out=identf[:], in_=identf[:], pattern=[[-1, 128]], compare_op=ALU.not_equal,
                                               fill=1.0, base=0, channel_multiplier=1), r=["identf"], w=["identf"])
        P.op("pool", lambda E: E.tensor_copy(out=identb[:], in_=identf[:]), r=["identf"], w=["identb"])
        P.op("pool", lambda E: E.memset(onesf[:], 1.0), w=["onesf"])
        P.op("pool", lambda E: E.memset(onesb[:], 1.0), w=["onesb"])
        P.op("pool", lambda E: E.memset(trif[:], 1.0), w=["trif"])
        P.op("pool", lambda E: E.affine_select(out=trif[:], in_=trif[:], pattern=[[1, 128]], compare_op=ALU.is_gt,
                                               fill=0.0, base=0, channel_multiplier=-1), r=["trif"], w=["trif"])
        P.op("pool", lambda E: E.tensor_copy(out=trib[:], in_=trif[:]), r=["trif"], w=["trib"])
        P.op("pool", lambda E: E.memset(bavgf[:], 0.0), w=["bavgf"])
        P.op("pool", lambda E: E.memset(bavgf[0:64, 0:64], 1.0 / 64), r=["bavgf"], w=["bavgf"])
        P.op("pool", lambda E: E.memset(bavgf[64:128, 64:128], 1.0 / 64), r=["bavgf"], w=["bavgf"])
        P.op("pool", lambda E: E.tensor_copy(out=bavg[:], in_=bavgf[:]), r=["bavgf"], w=["bavg"])
        P.op("pool", lambda E: E.iota(iota_i[:], pattern=[[1, 128]], base=0, channel_multiplier=0), w=["iota_i"])
        P.op("pool", lambda E: E.tensor_copy(out=iota_f[:], in_=iota_i[:]), r=["iota_i"], w=["iota_f"])
        epsr = sb("epsr", [128, 1])
        epsl = sb("epsl", [128, 1])
        P.op("pool", lambda E: E.memset(epsr[:], D * RMS_EPS), w=["epsr"])
        P.op("pool", lambda E: E.memset(epsl[:], LN_EPS), w=["epsl"])
        pidx_i = sb("pidx_i", [128, 1], I32)
        pidx_f = sb("pidx_f", [128, 1])
        P.op("pool", lambda E: E.iota(pidx_i[:], pattern=[[0, 1]], base=0, channel_multiplier=1), w=["pidx_i"])
        P.op("pool", lambda E: E.tensor_copy(out=pidx_f[:], in_=pidx_i[:]), r=["pidx_i"], w=["pidx_f"])
        P.op("dve", lambda E: E.tensor_scalar(out=inv16[:], in0=iota_f[:, 0:16], scalar1=1.0, scalar2=None, op0=ALU.add),
             r=["iota_f"], w=["inv16"])
        P.op("dve", lambda E: E.reciprocal(out=inv16[:], in_=inv16[:]), r=["inv16"], w=["inv16"])

        bcd = {}

        def mk_bc(E):
            reg = E.alloc_register("bcreg")
            inst = E.reg_mov(reg, NE * 128 - 1)
            bcd["v"] = E.snap(reg, donate=True)
            return E.memset(epsl[:], LN_EPS)
        P.op("pool", mk_bc, w=["epsl"])
        NFB = 5
        psf = [psum(f"psf{i}", [128, 512]) for i in range(NFB)]
        psb = [psum(f"psb{i}", [128, 1024], BF16) for i in range(2)]
        psr = psum("psr", [128, 512])
        bank_ctr = [0, 0]

        def fbank():
            i = bank_ctr[0] % NFB
            bank_ctr[0] += 1
            return psf[i], f"psf{i}"

        def bbank():
            i = bank_ctr[1] % 2
            bank_ctr[1] += 1
            return psb[i], f"psb{i}"

        c_sb = sb("c_sb", [128, 8])
        cond = sb("cond", [128, 8])
        condbc = sb("condbc", [128, 8, 128])
        P.dma("sp", lambda E: E.dma_start(out=c_sb[:], in_=c_d.ap().rearrange("(c p) -> p c", p=128), allow_slow_non_contiguous=True),
              w=["c_sb"])
        P.op("act", lambda E: E.activation(out=cond[:], in_=c_sb[:], func=AF.Silu), r=["c_sb"], w=["cond"])
        for c in range(8):
            P.op("act", lambda E, c=c: E.activation(out=condbc[:, c, :], in_=onesf[:], func=AF.Copy, scale=cond[:, c:c + 1]),
                 r=["cond", "onesf"], w=["condbc"])

        mod = sb("mod", [128, 6 * D])
        oh1 = sb("oh1", [128, NB, NE], BF16)
        oh2 = sb("oh2", [128, NB, NE], BF16)
        Aall = sb("Aall", [128, NB, NE], BF16)
        w1 = sb("w1", [128, NB])
        w2 = sb("w2", [128, NB])
        pos1 = sb("pos1", [128, NB], I32)
        pos2 = sb("pos2", [128, NB], I32)
        te_i = sb("te_i", [128, TM], I32)

        B1, A1, G1, B2, A2, G2 = (mod[:, k * D:(k + 1) * D] for k in range(6))

        def layer(l):
            x_src = x_d if l == 0 else xs2
            last = (l == DEPTH_ - 1)
            with contextlib.ExitStack() as sc:
                P.barrier()
                def sbl(name, shape, dt=F32, sc=sc):
                    return sc.enter_context(nc.sbuf_tensor(f"{name}_{l}", list(shape), dt))
                aw = [sbl(f"aw{k}", [128, 8, 512]) for k in range(2)]
                ab = [sbl(f"ab{k}", [128, 512]) for k in range(2)]
                gbc = sbl("gbc", [128, D])
                for nn in range(12):
                    k = nn % 2
                    P.dma("sp", lambda E, nn=nn, k=k: E.dma_start(
                        out=aw[k][:], in_=ada_w[l, :, nn * 512:(nn + 1) * 512].rearrange("(c p) n -> p c n", p=128)),
                        w=[f"aw{k}"])
                    P.dma("sp", lambda E, nn=nn, k=k: E.dma_start(
                        out=ab[k][:], in_=ada_b[l, nn * 512:(nn + 1) * 512].partition_broadcast(128)), w=[f"ab{k}"])
                    pb, pbn = fbank()
                    for c in range(8):
                        P.op("pe", lambda E, c=c, pb=pb, k=k: E.matmul(pb[:], lhsT=condbc[:, c, :], rhs=aw[k][:, c, :],
                                                                      start=(c == 0), stop=(c == 7)),
                             r=["condbc", f"aw{k}"], w=[pbn])
                    P.op("dve", lambda E, nn=nn, pb=pb, k=k: E.tensor_tensor(out=mod[:, nn * 512:(nn + 1) * 512], in0=pb[:], in1=ab[k][:],
                                                                             op=ALU.add), r=[f"ab{k}"], w=[pbn, "mod"])
                for (gsrc, Aap) in ((norm1_g, A1), (norm2_g, A2)):
                    P.dma("sp", lambda E, gsrc=gsrc: E.dma_start(out=gbc[:], in_=gsrc[l, :].partition_broadcast(128)), w=["gbc"])
                    P.op("act", lambda E: E.mul(out=gbc[:], in_=gbc[:], mul=32.0), r=["gbc"], w=["gbc"])
                    P.op("dve", lambda E, Aap=Aap: E.scalar_tensor_tensor(out=Aap, in0=Aap, scalar=1.0, in1=gbc[:], op0=ALU.add,
                                                                          op1=ALU.mult), r=["gbc", "mod"], w=["mod"])
            with contextlib.ExitStack() as sc:
                P.barrier()
                def sbl(name, shape, dt=F32, sc=sc):
                    return sc.enter_context(nc.sbuf_tensor(f"{name}_{l}", list(shape), dt))
                win = sbl("win", [128, 8, 1536], BF16)
                wout = sbl("wout", [128, 8, D], BF16)
                pw = sbl("pw", [128, 4, 128], BF16)
                wr = sbl("wr", [128, 8, 36])
                rbias = sbl("rbias", [128, 4, 36])
                pv = sbl("pv", [35, 512])
                pvT = sbl("pvT", [128, 4, 35])
                diag = sbl("diag", [128, CK, 4, 128], BF16)
                xt = [sbl(f"xt{k}", [128, 4, D]) for k in range(1)]
                ss = sbl("ss", [128, 8])
                rr = sbl("rr", [128, 8])
                tmpA = [sbl(f"tmpA{k}", [128, D]) for k in range(2)]
                hb = sbl("hb", [128, 4, D], BF16)
                hT = sbl("hT", [128, 8, 512], BF16)
                u = sbl("u", [128, 4, 528])
                sA = sbl("sA", [128, 528])
                sB = sbl("sB", [128, 528])
                pooled = sbl("pooled", [128, 4, 512], BF16)
                ycat = sbl("ycat", [128, 8, 512], BF16)
                vbuf = sbl("vbuf", [128, 4, 544], BF16)
                sig = [sbl(f"sig{k}", [128, 512]) for k in range(1)]
                ybf = [sbl(f"ybf{k}", [128, 512], BF16) for k in range(1)]
                dd = [sbl(f"dd{k}", [128, 512]) for k in range(1)]
                sq = [sbl(f"sq{k}", [128, 512], BF16) for k in range(1)]
                rs = [sbl(f"rs{k}", [128, 512]) for k in range(1)]
                yn = dd
                tmpO = [sbl(f"tmpO{k}", [128, 512]) for k in range(2)]
                h2b = hb
                h2T = [sbl(f"h2T{k}", [128, 8, 128]) for k in range(1)]
                lg = sbl("lg", [128, 4, 36])
                gmax = sbl("gmax", [128, 4])
                gd = sbl("gd", [128, 4, 4])
                gsum = sbl("gsum", [128, 4])
                pgrp = sbl("pgrp", [128, 4])
                ohg = sbl("ohg", [128, 4, 4])
                em = sbl("em", [128, 4, NE])
                em2 = sbl("em2", [128, 4, NE])
                m1 = sbl("m1", [128, 4])
                m2 = sbl("m2", [128, 4])
                d12 = sbl("d12", [128, 4])

                for c in range(8):
                    P.dma("pool", lambda E, c=c: E.dma_start(out=win[:, c, :], in_=w_in[l, c * 128:(c + 1) * 128, :]), w=["win"])
                for c in range(8):
                    P.dma("pool", lambda E, c=c: E.dma_start(out=wout[:, c, :], in_=w_out[l, c * 128:(c + 1) * 128, :]), w=["wout"])
                P.dma("pool", lambda E: E.dma_start(out=pw[:], in_=pool_w[l].rearrange("g c d -> c g d")), w=["pw"])
                P.dma("sp", lambda E: E.dma_start(out=wr[:], in_=rw[l].rearrange("(c p) g -> p c g", p=128)), w=["wr"])
                for j in range(4):
                    P.dma("sp", lambda E, j=j: E.dma_start(out=rbias[:, j, :], in_=rb[l, :].partition_broadcast(128)), w=["rbias"])
                P.dma("sp", lambda E: E.dma_start(out=pv[:], in_=pvec[l]), w=["pv"])
                for c in range(4):
                    pb, pbn = fbank()
                    P.op("pe", lambda E, c=c, pb=pb: E.transpose(out=pb[:, 0:35], in_=pv[:, c * 128:(c + 1) * 128], identity=identf[0:35, 0:35]),
                         r=["pv", "identf"], w=[pbn])
                    P.op("act", lambda E, c=c, pb=pb: E.activation(out=pvT[:, c, :], in_=pb[:, 0:35], func=AF.Copy), w=[pbn, "pvT"])
                for k in range(CK):
                    for c in range(4):
                        eng = "dve" if (k * 4 + c) % 2 == 0 else "pool"
                        P.op(eng, lambda E, k=k, c=c: E.tensor_scalar(out=diag[:, k, c, :], in0=identf[:], scalar1=pvT[:, c, k:k + 1],
                                                                      scalar2=None, op0=ALU.mult),
                             r=["identf", "pvT"], w=[f"diag{k}_{c}"])
                P.op("pool", lambda E: E.memset(u[:], 0.0), w=["u0", "u1", "u2", "u3"])
                P.op("pool", lambda E: E.memset(vbuf[:], 0.0), w=["v0", "v1", "v2", "v3"])

                def mtile(i):
                    X = xt[0]
                    Xn = "xt0"
                    rows = slice(i * 512, (i + 1) * 512)
                    P.dma("sp", lambda E, X=X, rows=rows: E.dma_start(out=X[:], in_=x_src[rows, :].rearrange("(j p) d -> p j d", p=128)),
                          r=[f"xs2_{4 * i + q}" for q in range(4)], w=[Xn])
                    P.op("dve", lambda E: E.memset(ss[:], 0.0), w=["ss"])
                    for j in range(4):
                        P.op("act", lambda E, j=j, X=X: E.activation(out=hb[:, j, :], in_=X[:, j, :], func=AF.Square, accum_out=ss[:, j:j + 1]),
                             r=[Xn], w=[f"hb{j}", "ss"])
                    P.op("act", lambda E: E.activation(out=rr[:, 0:4], in_=ss[:, 0:4], func=AF.Sqrt, bias=epsr[:, 0:1]), r=["ss", "epsr"], w=["rr"])
                    P.op("dve", lambda E: E.reciprocal(out=rr[:, 0:4], in_=rr[:, 0:4]), r=["rr"], w=["rr"])
                    for j in range(4):
                        T = tmpA[j % 2]
                        Tn = f"tmpA{j % 2}"
                        P.op("dve", lambda E, j=j, X=X, T=T: E.scalar_tensor_tensor(out=T[:], in0=X[:, j, :], scalar=rr[:, j:j + 1], in1=A1,
                                                                                    op0=ALU.mult, op1=ALU.mult),
                             r=[Xn, "rr", "mod"], w=[Tn])
                        P.op("pool", lambda E, j=j, T=T: E.tensor_tensor(out=hb[:, j, :], in0=T[:], in1=B1, op=ALU.add),
                             r=[Tn, "mod"], w=[f"hb{j}"])
                    for j in range(4):
                        pb, pbn = bbank()
                        for c in range(8):
                            P.op("pe", lambda E, j=j, c=c, pb=pb: E.transpose(out=pb[:, c * 128:(c + 1) * 128], in_=hb[:, j, c * 128:(c + 1) * 128],
                                                                             identity=identb[:]),
                                 r=[f"hb{j}", "identb"], w=[pbn])
                        eng = "act" if j % 2 == 0 else "dve"
                        if eng == "act":
                            P.op("act", lambda E, j=j, pb=pb: E.activation(out=hT[:, :, j * 128:(j + 1) * 128],
                                                                           in_=pb[:].rearrange("p (c t) -> p c t", c=8), func=AF.Copy),
                                 w=[pbn, f"hT{j}"])
                        else:
                            P.op("dve", lambda E, j=j, pb=pb: E.tensor_copy(out=hT[:, :, j * 128:(j + 1) * 128],
                                                                            in_=pb[:].rearrange("p (c t) -> p c t", c=8)),
                                 w=[pbn, f"hT{j}"])
                    hTr = ["hT0", "hT1", "hT2", "hT3"]
                    if debug and l == 0 and i == 0:
                        P.dma("sp", lambda E: E.dma_start(out=dbg["d_hT"].ap(), in_=hT[:].rearrange("p c t -> p (c t)")), r=hTr, w=["dbg1"])
                        P.dma("sp", lambda E: E.dma_start(out=dbg["d_mod"].ap(), in_=mod[:]), r=["mod"], w=["dbg2"])
                        P.dma("sp", lambda E: E.dma_start(out=dbg["d_pvT"].ap(), in_=pvT[:].rearrange("p c t -> p (c t)")), r=["pvT"], w=["dbg2b"])

                    def win_mm(oc):
                        pb, pbn = fbank()
                        for c in range(8):
                            P.op("pe", lambda E, c=c, pb=pb, oc=oc: E.matmul(pb[:], lhsT=win[:, c, oc * 128:(oc + 1) * 128], rhs=hT[:, c, :],
                                                                           start=(c == 0), stop=(c == 7)),
                                 r=["win"] + hTr, w=[pbn])
                        return pb, pbn

                    def pool_chunk(g):
                        wdw = POOL_WINDOWS[g]
                        pb, pbn = win_mm(g)
                        un = f"u{g}"
                        P.op("act", lambda E, g=g, pb=pb: E.activation(out=u[:, g, 16:528], in_=pb[:], func=AF.Copy), w=[pbn, un])
                        src = None
                        step = 1
                        bufs = [sA, sB]
                        bi = 0
                        cur = None
                        lo = 0
                        while step < wdw:
                            dst = bufs[bi]
                            dn = "sA" if bi == 0 else "sB"
                            nlo = lo + step
                            if cur is None:
                                P.op("pool", lambda E, g=g, dst=dst, nlo=nlo, step=step: E.tensor_tensor(
                                    out=dst[:, nlo:528], in0=u[:, g, nlo:528], in1=u[:, g, nlo - step:528 - step], op=ALU.add),
                                    r=[un], w=[dn])
                            else:
                                cs, cn = cur
                                P.op("pool", lambda E, dst=dst, cs=cs, nlo=nlo, step=step: E.tensor_tensor(
                                    out=dst[:, nlo:528], in0=cs[:, nlo:528], in1=cs[:, nlo - step:528 - step], op=ALU.add),
                                    r=[cn], w=[dn])
                            cur = (dst, dn)
                            lo = nlo
                            step *= 2
                            bi ^= 1
                        cs, cn = cur
                        P.op("dve", lambda E, g=g, cs=cs, wdw=wdw: E.scalar_tensor_tensor(
                            out=pooled[:, g, :], in0=cs[:, 16:528], scalar=1.0 / wdw, in1=u[:, g, 16:528], op0=ALU.mult, op1=ALU.subtract),
                            r=[cn, un], w=[f"pooled{g}"])
                        if i == 0:
                            nfix = wdw - 1
                            P.op("pool", lambda E, cs=cs, nfix=nfix: E.tensor_tensor(out=cs[:, 16:16 + nfix], in0=cs[:, 16:16 + nfix],
                                                                                    in1=inv16[:, 0:nfix], op=ALU.mult),
                                 r=[cn, "inv16", f"pooled{g}"], w=[cn])
                            P.op("pool", lambda E, g=g, cs=cs, nfix=nfix: E.tensor_tensor(out=pooled[:, g, 0:nfix], in0=cs[:, 16:16 + nfix],
                                                                                         in1=u[:, g, 16:16 + nfix], op=ALU.subtract),
                                 r=[cn, un], w=[f"pooled{g}"])
                        P.op("pool", lambda E, g=g: E.tensor_copy(out=u[:, g, 0:16], in_=u[:, g, 512:528]), r=[un], w=[un])
                        pb2, pb2n = fbank()
                        P.op("pe", lambda E, g=g, pb2=pb2: E.matmul(pb2[:], lhsT=pw[:, g, :], rhs=pooled[:, g, :], start=True, stop=True),
                             r=["pw", f"pooled{g}"], w=[pb2n])
                        P.op("act", lambda E, g=g, pb2=pb2: E.activation(out=ycat[:, g, :], in_=pb2[:], func=AF.Copy, scale=pvT[:, g, 34:35]),
                             r=["pvT"], w=[pb2n, f"ycat{g}"])
                    for _g in range(4):
                        pool_chunk(_g)
                    def conv_chunk(c):
                        k2 = 0
                        vn = f"v{c}"
                        pa, pan = win_mm(4 + c)
                        pg, pgn = win_mm(8 + c)
                        P.op("act", lambda E, pg=pg, k2=k2: E.activation(out=sig[k2][:], in_=pg[:], func=AF.Sigmoid), w=[pgn, f"sig{k2}"])
                        P.op("dve", lambda E, c=c, pa=pa, k2=k2: E.tensor_tensor(out=vbuf[:, c, 32:544], in0=pa[:], in1=sig[k2][:], op=ALU.mult),
                             r=[f"sig{k2}"], w=[pan, vn])
                        py, pyn = fbank()
                        for k in range(CK):
                            P.op("pe", lambda E, c=c, k=k, py=py: E.matmul(py[:], lhsT=diag[:, k, c, :], rhs=vbuf[:, c, 2 + k:2 + k + 512],
                                                                         start=(k == 0), stop=(k == CK - 1)),
                                 r=[f"diag{k}_{c}", vn], w=[pyn])
                        P.op("pool", lambda E, c=c: E.tensor_copy(out=vbuf[:, c, 2:32], in_=vbuf[:, c, 514:544]), r=[vn], w=[vn])
                        P.op("act", lambda E, c=c, py=py, k2=k2: E.activation(out=ybf[k2][:], in_=py[:], func=AF.Identity, bias=pvT[:, c, 31:32]),
                             r=["pvT"], w=[pyn, f"ybf{k2}"])
                        pm, pmn = fbank()
                        P.op("pe", lambda E, pm=pm, k2=k2: E.matmul(pm[:], lhsT=bavg[:], rhs=ybf[k2][:], start=True, stop=True),
                             r=["bavg", f"ybf{k2}"], w=[pmn])
                        P.op("dve", lambda E, pm=pm, k2=k2: E.tensor_tensor(out=dd[k2][:], in0=ybf[k2][:], in1=pm[:], op=ALU.subtract),
                             r=[f"ybf{k2}"], w=[pmn, f"dd{k2}"])
                        P.op("act", lambda E, k2=k2: E.activation(out=sq[k2][:], in_=dd[k2][:], func=AF.Square), r=[f"dd{k2}"], w=[f"sq{k2}"])
                        pvv, pvn = fbank()
                        P.op("pe", lambda E, pvv=pvv, k2=k2: E.matmul(pvv[:], lhsT=bavg[:], rhs=sq[k2][:], start=True, stop=True),
                             r=["bavg", f"sq{k2}"], w=[pvn])
                        P.op("act", lambda E, pvv=pvv, k2=k2: E.activation(out=rs[k2][:], in_=pvv[:], func=AF.Sqrt, bias=epsl[:, 0:1]),
                             r=["epsl"], w=[pvn, f"rs{k2}"])
                        P.op("dve", lambda E, k2=k2: E.reciprocal(out=rs[k2][:], in_=rs[k2][:]), r=[f"rs{k2}"], w=[f"rs{k2}"])
                        P.op("pool", lambda E, k2=k2: E.tensor_tensor(out=yn[k2][:], in0=dd[k2][:], in1=rs[k2][:], op=ALU.mult),
                             r=[f"dd{k2}", f"rs{k2}"], w=[f"dd{k2}"])
                        P.op("act", lambda E, c=c, k2=k2: E.activation(out=ycat[:, 4 + c, :], in_=yn[k2][:], func=AF.Silu, bias=pvT[:, c, 33:34],
                                                                       scale=pvT[:, c, 32:33]),
                             r=[f"dd{k2}", "pvT"], w=[f"ycat{4 + c}"])
                    for _c in range(4):
                        conv_chunk(_c)
                    ycr = [f"ycat{k}" for k in range(8)]
                    if debug and l == 0 and i == 0:
                        P.dma("sp", lambda E: E.dma_start(out=dbg["d_ycat"].ap(), in_=ycat[:].rearrange("p c t -> p (c t)")), r=ycr, w=["dbg3"])
                    for j in range(4):
                        for dh in range(2):
                            po, pon = fbank()
                            for k in range(8):
                                P.op("pe", lambda E, j=j, dh=dh, k=k, po=po: E.matmul(po[:], lhsT=ycat[:, k, j * 128:(j + 1) * 128],
                                                                                  rhs=wout[:, k, dh * 512:(dh + 1) * 512], start=(k == 0), stop=(k == 7)),
                                     r=ycr + ["wout"], w=[pon])
                            kk = (j * 2 + dh) % 2
                            P.op("dve", lambda E, dh=dh, po=po, kk=kk: E.tensor_tensor(out=tmpO[kk][:], in0=po[:], in1=G1[:, dh * 512:(dh + 1) * 512],
                                                                                      op=ALU.mult), r=["mod"], w=[pon, f"tmpO{kk}"])
                            P.op("pool", lambda E, j=j, dh=dh, X=X, kk=kk: E.tensor_tensor(out=X[:, j, dh * 512:(dh + 1) * 512], in0=tmpO[kk][:],
                                                                                         in1=X[:, j, dh * 512:(dh + 1) * 512], op=ALU.add),
                                 r=[f"tmpO{kk}", Xn], w=[Xn])
                    P.dma("act", lambda E, X=X, rows=rows: E.dma_start(out=xs1[rows, :].rearrange("(j p) d -> p j d", p=128), in_=X[:]),
                          r=[Xn], w=[f"xs1_{i}"])
                    for j in range(4):
                        P.op("act", lambda E, j=j, X=X: E.activation(out=hb[:, j, :], in_=X[:, j, :], func=AF.Square, accum_out=ss[:, 4 + j:5 + j]),
                             r=[Xn], w=[f"hb{j}", "ss"])
                    P.op("act", lambda E: E.activation(out=rr[:, 4:8], in_=ss[:, 4:8], func=AF.Sqrt, bias=epsr[:, 0:1]), r=["ss", "epsr"], w=["rr"])
                    P.op("dve", lambda E: E.reciprocal(out=rr[:, 4:8], in_=rr[:, 4:8]), r=["rr"], w=["rr"])
                    pr, prn = psr, "psr"
                    for j in range(4):
                        T = tmpA[j % 2]
                        Tn = f"tmpA{j % 2}"
                        H = T
                        Hn = Tn
                        P.op("dve", lambda E, j=j, X=X, T=T: E.scalar_tensor_tensor(out=T[:], in0=X[:, j, :], scalar=rr[:, 4 + j:5 + j], in1=A2,
                                                                                    op0=ALU.mult, op1=ALU.mult),
                             r=[Xn, "rr", "mod"], w=[Tn])
                        P.op("pool", lambda E, T=T, H=H: E.tensor_tensor(out=H[:], in0=T[:], in1=B2, op=ALU.add), r=[Tn, "mod"], w=[Hn])
                        P.op("act", lambda E, j=j, H=H: E.activation(out=h2b[:, j, :], in_=H[:], func=AF.Copy), r=[Hn], w=[f"hb{j}"])
                        HT = h2T[0]
                        HTn = "h2T0"
                        for half in range(2):
                            pt, ptn = fbank()
                            for cc in range(4):
                                c = half * 4 + cc
                                P.op("pe", lambda E, c=c, cc=cc, H=H, pt=pt: E.transpose(out=pt[:, cc * 128:(cc + 1) * 128],
                                                                                       in_=H[:, c * 128:(c + 1) * 128], identity=identf[:]),
                                     r=[Hn, "identf"], w=[ptn])
                            eng = "act" if half == 0 else "dve"
                            if eng == "act":
                                P.op("act", lambda E, half=half, HT=HT, pt=pt: E.activation(out=HT[:, half * 4:(half + 1) * 4, :],
                                                                                        in_=pt[:].rearrange("p (c t) -> p c t", c=4), func=AF.Copy),
                                     w=[ptn, HTn])
                            else:
                                P.op("dve", lambda E, half=half, HT=HT, pt=pt: E.tensor_copy(out=HT[:, half * 4:(half + 1) * 4, :],
                                                                                         in_=pt[:].rearrange("p (c t) -> p c t", c=4)),
                                     w=[ptn, HTn])
                        for c in range(8):
                            P.op("pe", lambda E, j=j, c=c, HT=HT, pr=pr: E.matmul(pr[:, j * 36:(j + 1) * 36], lhsT=HT[:, c, :], rhs=wr[:, c, :],
                                                                              start=(c == 0), stop=(c == 7)),
                                 r=[HTn, "wr"], w=[prn])
                    P.dma("act", lambda E, rows=rows: E.dma_start(out=h2s[rows, :].rearrange("(j p) d -> p j d", p=128), in_=h2b[:]), r=["hb0", "hb1", "hb2", "hb3"], w=[f"h2s_{i}"])
                    bs = slice(i * 4, (i + 1) * 4)
                    P.op("dve", lambda E, pr=pr: E.tensor_tensor(out=lg[:], in0=pr[:, 0:144].rearrange("p (j e) -> p j e", j=4), in1=rbias[:],
                                                                 op=ALU.add), r=["rbias"], w=[prn, "lg"])
                    if debug and l == 0 and i == 0:
                        P.dma("sp", lambda E: E.dma_start(out=dbg["d_lg"].ap(), in_=lg[:].rearrange("p c t -> p (c t)")), r=["lg"], w=["dbg4"])
                    gl = lg[:, :, 0:4]
                    el = lg[:, :, 4:36]
                    P.op("dve", lambda E: E.tensor_reduce(out=gmax[:], in_=gl, axis=AX.X, op=ALU.max), r=["lg"], w=["gmax"])
                    P.op("dve", lambda E: E.tensor_tensor(out=gd[:], in0=gl, in1=gmax[:].unsqueeze(2).to_broadcast([128, 4, 4]), op=ALU.subtract),
                         r=["lg", "gmax"], w=["gd"])
                    P.op("dve", lambda E: E.tensor_tensor(out=ohg[:], in0=gl, in1=gmax[:].unsqueeze(2).to_broadcast([128, 4, 4]), op=ALU.is_equal),
                         r=["lg", "gmax"], w=["ohg"])
                    P.op("act", lambda E: E.activation(out=gd[:], in_=gd[:], func=AF.Exp), r=["gd"], w=["gd"])
                    P.op("dve", lambda E: E.tensor_reduce(out=gsum[:], in_=gd[:], axis=AX.X, op=ALU.add), r=["gd"], w=["gsum"])
                    P.op("dve", lambda E: E.reciprocal(out=pgrp[:], in_=gsum[:]), r=["gsum"], w=["pgrp"])
                    P.op("dve", lambda E: E.tensor_scalar(out=ohg[:], in0=ohg[:], scalar1=-1.0, scalar2=1e30, op0=ALU.add, op1=ALU.mult),
                         r=["ohg"], w=["ohg"])
                    P.op("dve", lambda E: E.tensor_tensor(out=em[:].rearrange("p j (g e) -> p j g e", g=4),
                                                          in0=el.rearrange("p j (g e) -> p j g e", g=4),
                                                          in1=ohg[:].unsqueeze(3).to_broadcast([128, 4, 4, 8]), op=ALU.add),
                         r=["lg", "ohg"], w=["em"])
                    P.op("dve", lambda E: E.tensor_reduce(out=m1[:], in_=em[:], axis=AX.X, op=ALU.max), r=["em"], w=["m1"])
                    P.op("dve", lambda E, bs=bs: E.tensor_tensor(out=oh1[:, bs, :], in0=em[:], in1=m1[:].unsqueeze(2).to_broadcast([128, 4, NE]),
                                                                op=ALU.is_equal), r=["em", "m1"], w=["oh1"])
                    P.op("dve", lambda E, bs=bs: E.scalar_tensor_tensor(out=em2[:], in0=oh1[:, bs, :], scalar=-1e30, in1=em[:], op0=ALU.mult,
                                                                       op1=ALU.add), r=["oh1", "em"], w=["em2"])
                    P.op("dve", lambda E: E.tensor_reduce(out=m2[:], in_=em2[:], axis=AX.X, op=ALU.max), r=["em2"], w=["m2"])
                    P.op("dve", lambda E, bs=bs: E.tensor_tensor(out=oh2[:, bs, :], in0=em2[:], in1=m2[:].unsqueeze(2).to_broadcast([128, 4, NE]),
                                                                op=ALU.is_equal), r=["em2", "m2"], w=["oh2"])
                    P.op("dve", lambda E: E.tensor_tensor(out=d12[:], in0=m1[:], in1=m2[:], op=ALU.subtract), r=["m1", "m2"], w=["d12"])
                    P.op("act", lambda E: E.activation(out=d12[:], in_=d12[:], func=AF.Sigmoid), r=["d12"], w=["d12"])
                    P.op("dve", lambda E, bs=bs: E.tensor_tensor(out=w1[:, bs], in0=d12[:], in1=pgrp[:], op=ALU.mult), r=["d12", "pgrp"], w=["w1"])
                    P.op("dve", lambda E, bs=bs: E.tensor_tensor(out=w2[:, bs], in0=pgrp[:], in1=w1[:, bs], op=ALU.subtract),
                         r=["pgrp", "w1"], w=["w2"])
                    P.op("dve", lambda E, bs=bs: E.tensor_tensor(out=Aall[:, bs, :], in0=oh1[:, bs, :], in1=oh2[:, bs, :], op=ALU.add),
                         r=["oh1", "oh2"], w=["Aall"])
                for _i in range(NT):
                    mtile(_i)

            with contextlib.ExitStack() as sc:
                P.barrier()
                def sbl(name, shape, dt=F32, sc=sc):
                    return sc.enter_context(nc.sbuf_tensor(f"{name}_{l}", list(shape), dt))
                NC_ = NB * NE
                within = sbl("within", [128, NB, NE])
                tot = [sbl(f"tot{k}", [128, NB, NE]) for k in range(2)]
                tot0 = sbl("totz", [128, NB, NE])
                cmpb = sbl("cmpb", [128, NE, NB2])
                ntile = [sbl(f"ntile{k}", [128, NE]) for k in range(2)]
                nt0 = sbl("ntz", [128, NE])
                basee = sbl("basee", [128, NE])
                tend = sbl("tend", [128, NE])
                posall = sbl("posall", [128, NB, NE])
                ptmp = sbl("ptmp", [128, NB, NE])
                posf = sbl("posf", [128, NB])
                thr = sbl("thr", [128, NB2])
                cmpt = sbl("cmpt", [128, TM, NE])
                tef = sbl("tef", [128, TM])
                Af = Aall[:].rearrange("p b e -> p (b e)")
                for h0 in range(0, NC_, 512):
                    wd_ = min(512, NC_ - h0)
                    pbw, pbwn = fbank()
                    P.op("pe", lambda E, h0=h0, wd_=wd_, pbw=pbw: E.matmul(pbw[:, 0:wd_], lhsT=trib[:], rhs=Af[:, h0:h0 + wd_], start=True, stop=True),
                         r=["trib", "Aall"], w=[pbwn])
                    P.op("act", lambda E, h0=h0, wd_=wd_, pbw=pbw: E.activation(out=within[:].rearrange("p b e -> p (b e)")[:, h0:h0 + wd_],
                                                                           in_=pbw[:, 0:wd_], func=AF.Copy), w=[pbwn, "within"])
                    pbt, pbtn = fbank()
                    P.op("pe", lambda E, h0=h0, wd_=wd_, pbt=pbt: E.matmul(pbt[:, 0:wd_], lhsT=onesb[:], rhs=Af[:, h0:h0 + wd_], start=True, stop=True),
                         r=["onesb", "Aall"], w=[pbtn])
                    P.op("dve", lambda E, h0=h0, wd_=wd_, pbt=pbt: E.tensor_copy(out=tot0[:].rearrange("p b e -> p (b e)")[:, h0:h0 + wd_],
                                                                            in_=pbt[:, 0:wd_]), w=[pbtn, "tot0"])
                cur, curn = tot0, "tot0"
                step = 1
                bi = 0
                while step < NB:
                    dst, dn = tot[bi], f"tot{bi}"
                    P.op("dve", lambda E, dst=dst, cur=cur, step=step: E.tensor_copy(out=dst[:, 0:step, :], in_=cur[:, 0:step, :]), r=[curn], w=[dn])
                    P.op("dve", lambda E, dst=dst, cur=cur, step=step: E.tensor_tensor(out=dst[:, step:NB, :], in0=cur[:, step:NB, :],
                                                                                   in1=cur[:, 0:NB - step, :], op=ALU.add), r=[curn], w=[dn])
                    cur, curn = dst, dn
                    step *= 2
                    bi ^= 1
                incl, incln = cur, curn
                P.op("dve", lambda E: E.tensor_tensor(out=posall[:], in0=within[:], in1=incl[:], op=ALU.add), r=["within", incln], w=["posall"])
                P.op("dve", lambda E: E.tensor_tensor(out=posall[:], in0=posall[:], in1=tot0[:], op=ALU.subtract), r=["posall", "tot0"], w=["posall"])
                cnt = incl[:, NB - 1, :]
                P.op("dve", lambda E: E.tensor_scalar(out=thr[:], in0=iota_f[:, 0:NB2], scalar1=float(SL), scalar2=None, op0=ALU.mult),
                     r=["iota_f"], w=["thr"])
                P.op("dve", lambda E: E.tensor_tensor(out=cmpb[:], in0=cnt.unsqueeze(2).to_broadcast([128, NE, NB2]),
                                                      in1=thr[:].unsqueeze(1).to_broadcast([128, NE, NB2]), op=ALU.is_gt),
                     r=[incln, "thr"], w=["cmpb"])
                P.op("dve", lambda E: E.tensor_reduce(out=nt0[:], in_=cmpb[:], axis=AX.X, op=ALU.add), r=["cmpb"], w=["nt0"])
                cur, curn = nt0, "nt0"
                step = 1
                bi = 0
                while step < NE:
                    dst, dn = ntile[bi], f"ntile{bi}"
                    P.op("dve", lambda E, dst=dst, cur=cur, step=step: E.tensor_copy(out=dst[:, 0:step], in_=cur[:, 0:step]), r=[curn], w=[dn])
                    P.op("dve", lambda E, dst=dst, cur=cur, step=step: E.tensor_tensor(out=dst[:, step:NE], in0=cur[:, step:NE],
                                                                                   in1=cur[:, 0:NE - step], op=ALU.add), r=[curn], w=[dn])
                    cur, curn = dst, dn
                    step *= 2
                    bi ^= 1
                P.op("dve", lambda E, cur=cur: E.tensor_copy(out=tend[:], in_=cur[:]), r=[curn], w=["tend"])
                P.op("dve", lambda E: E.tensor_tensor(out=basee[:], in0=tend[:], in1=nt0[:], op=ALU.subtract), r=["tend", "nt0"], w=["basee"])
                P.op("dve", lambda E: E.tensor_scalar(out=basee[:], in0=basee[:], scalar1=float(SL), scalar2=None, op0=ALU.mult), r=["basee"], w=["basee"])
                P.op("dve", lambda E: E.tensor_tensor(out=posall[:], in0=posall[:], in1=basee[:].unsqueeze(1).to_broadcast([128, NB, NE]), op=ALU.add),
                     r=["posall", "basee"], w=["posall"])
                for (ohk, posk, nm) in ((oh1, pos1, "pos1"), (oh2, pos2, "pos2")):
                    P.op("dve", lambda E, ohk=ohk: E.tensor_tensor(out=ptmp[:], in0=ohk[:], in1=posall[:], op=ALU.mult),
                         r=["oh1", "oh2", "posall"], w=["ptmp"])
                    P.op("dve", lambda E: E.tensor_reduce(out=posf[:], in_=ptmp[:], axis=AX.X, op=ALU.add), r=["ptmp"], w=["posf"])
                    P.op("dve", lambda E, posk=posk: E.tensor_copy(out=posk[:], in_=posf[:]), r=["posf"], w=[nm])
                for j0 in range(0, TM, 128):
                    jn = min(128, TM - j0)
                    P.op("dve", lambda E, j0=j0, jn=jn: E.tensor_scalar(out=tef[:, j0:j0 + jn], in0=iota_f[:, 0:jn], scalar1=float(j0), scalar2=None,
                                                                       op0=ALU.add), r=["iota_f"], w=["tef"])
                P.op("dve", lambda E: E.tensor_tensor(out=cmpt[:], in0=tend[:].unsqueeze(1).to_broadcast([128, TM, NE]),
                                                      in1=tef[:].unsqueeze(2).to_broadcast([128, TM, NE]), op=ALU.is_le),
                     r=["tend", "tef"], w=["cmpt"])
                P.op("dve", lambda E: E.tensor_reduce(out=tef[:], in_=cmpt[:], axis=AX.X, op=ALU.add), r=["cmpt"], w=["tef"])
                P.op("dve", lambda E: E.tensor_scalar(out=tef[:], in0=tef[:], scalar1=128.0, scalar2=pidx_f[:, 0:1], op0=ALU.mult, op1=ALU.add),
                     r=["tef", "pidx_f"], w=["tef"])
                P.op("dve", lambda E: E.tensor_copy(out=te_i[:], in_=tef[:]), r=["tef"], w=["te_i"])

            if debug and l == 0:
                for nm, t_, rn in (("d_pos1", pos1, "pos1"), ("d_pos2", pos2, "pos2"), ("d_w1", w1, "w1"), ("d_w2", w2, "w2"), ("d_te", te_i, "te_i")):
                    P.dma("sp", lambda E, nm=nm, t_=t_: E.dma_start(out=dbg[nm].ap(), in_=t_[:]), r=[rn], w=["dbg_" + nm])
            with contextlib.ExitStack() as sc:
                P.barrier()
                def sbl(name, shape, dt=F32, sc=sc):
                    return sc.enter_context(nc.sbuf_tensor(f"{name}_{l}", list(shape), dt))
                hrow = [sbl(f"hrow{k}", [128, D], BF16) for k in range(3)]
                def sblock(b):
                    k = b % 3
                    P.dma("sp", lambda E, b=b, k=k: E.dma_start(out=hrow[k][:], in_=h2s[b * 128:(b + 1) * 128, :]), r=[f"h2s_{b // 4}"], w=[f"hrow{k}"])
                    for (posk, nm) in ((pos1, "pos1"), (pos2, "pos2")):
                        P.dma("pool", lambda E, b=b, k=k, posk=posk: E.indirect_dma_start(
                            out=hsort[:, :], out_offset=bass.IndirectOffsetOnAxis(ap=posk[:, b:b + 1], axis=0),
                            in_=hrow[k][:], in_offset=None), r=[f"hrow{k}", nm], w=[f"hsortw{b}"])
                for _b in range(NB):
                    sblock(_b)
            with contextlib.ExitStack() as sc:
                P.barrier()
                def sbl(name, shape, dt=F32, sc=sc):
                    return sc.enter_context(nc.sbuf_tensor(f"{name}_{l}", list(shape), dt))
                NW = 3
                wgb = [sbl(f"wgb{k}", [128, 8, HID], BF16) for k in range(NW)]
                wub = [sbl(f"wub{k}", [128, 8, HID], BF16) for k in range(NW)]
                wdb = [sbl(f"wdb{k}", [128, 2, D], BF16) for k in range(NW)]
                stt = [sbl(f"stt{k}", [128, 2, D], BF16) for k in range(2)]
                hsT = [sbl(f"hsT{k}", [128, 8, SL], BF16) for k in range(2)]
                sgl = [sbl(f"sgl{k}", [128, 2 * SL]) for k in range(2)]
                hid = [sbl(f"hid{k}", [128, 2, SL], BF16) for k in range(2)]
                yo = [sbl(f"yo{k}", [128, 2, D]) for k in range(2)]
                allsc = [f"hsortw{b}" for b in range(NB)]

                def etile(j):
                    k = j % NW
                    k2 = j % 2
                    for (wb, wsrc, nm) in ((wgb, wg_d, "wgb"), (wub, wu_d, "wub"), (wdb, wd_d, "wdb")):
                        P.dma("pool", lambda E, wb=wb, wsrc=wsrc: E.indirect_dma_start(
                            out=wb[k][:].rearrange("p c h -> p (c h)"), out_offset=None, in_=wsrc[l][:, :],
                            in_offset=bass.IndirectOffsetOnAxis(ap=te_i[:, j:j + 1], axis=0), bounds_check=bcd["v"], oob_is_err=False),
                            r=["te_i"], w=[f"{nm}{k}"])
                    P.dma("sp", lambda E: E.dma_start(out=stt[k2][:], in_=hsort[j * SL:(j + 1) * SL, :].rearrange("(q p) d -> p q d", p=128)),
                          r=allsc, w=[f"stt{k2}"])
                    for q in range(2):
                        pb, pbn = bbank()
                        for c in range(8):
                            P.op("pe", lambda E, c=c, q=q, pb=pb: E.transpose(out=pb[:, c * 128:(c + 1) * 128], in_=stt[k2][:, q, c * 128:(c + 1) * 128],
                                                                           identity=identb[:]), r=[f"stt{k2}", "identb"], w=[pbn])
                        if q == 0:
                            P.op("act", lambda E, q=q, pb=pb: E.activation(out=hsT[k2][:, :, q * 128:(q + 1) * 128],
                                                                         in_=pb[:].rearrange("p (c t) -> p c t", c=8), func=AF.Copy), w=[pbn, f"hsT{k2}_{q}"])
                        else:
                            P.op("dve", lambda E, q=q, pb=pb: E.tensor_copy(out=hsT[k2][:, :, q * 128:(q + 1) * 128],
                                                                          in_=pb[:].rearrange("p (c t) -> p c t", c=8)), w=[pbn, f"hsT{k2}_{q}"])
                    hsr = [f"hsT{k2}_0", f"hsT{k2}_1"]
                    pg, pgn = fbank()
                    pu, pun = fbank()
                    for (W, Wn, pz, pzn) in ((wgb[k], f"wgb{k}", pg, pgn), (wub[k], f"wub{k}", pu, pun)):
                        for hc in range(2):
                            for c in range(8):
                                P.op("pe", lambda E, c=c, W=W, hc=hc, pz=pz: E.matmul(
                                    pz[:, hc * SL:(hc + 1) * SL], lhsT=W[:, c, hc * 128:(hc + 1) * 128], rhs=hsT[k2][:, c, :],
                                    start=(c == 0), stop=(c == 7)), r=[Wn] + hsr, w=[pzn])
                    P.op("act", lambda E: E.activation(out=sgl[k2][:], in_=pg[:, 0:2 * SL], func=AF.Silu), w=[pgn, f"sgl{k2}"])
                    P.op("dve", lambda E: E.tensor_tensor(out=hid[k2][:].rearrange("p c t -> p (c t)"), in0=pu[:, 0:2 * SL],
                                                          in1=sgl[k2][:], op=ALU.mult), r=[f"sgl{k2}"], w=[pun, f"hid{k2}"])
                    for q in range(2):
                        for dh in range(2):
                            pd, pdn = fbank()
                            for kc in range(2):
                                P.op("pe", lambda E, q=q, dh=dh, kc=kc, pd=pd: E.matmul(pd[:], lhsT=hid[k2][:, kc, q * 128:(q + 1) * 128],
                                                                                     rhs=wdb[k][:, kc, dh * 512:(dh + 1) * 512],
                                                                                     start=(kc == 0), stop=(kc == 1)),
                                     r=[f"hid{k2}", f"wdb{k}"], w=[pdn])
                            if dh == 0:
                                P.op("act", lambda E, q=q, pd=pd: E.activation(out=yo[k2][:, q, 0:512], in_=pd[:], func=AF.Copy), w=[pdn, f"yo{k2}_{q}a"])
                            else:
                                P.op("dve", lambda E, q=q, pd=pd: E.tensor_copy(out=yo[k2][:, q, 512:1024], in_=pd[:]), w=[pdn, f"yo{k2}_{q}b"])
                    P.dma("act", lambda E: E.dma_start(out=ysort[j * SL:(j + 1) * SL, :].rearrange("(q p) d -> p q d", p=128), in_=yo[k2][:]),
                          r=[f"yo{k2}_{q}{h}" for q in range(2) for h in "ab"], w=[f"ysortw{j}"])
                for _j in range(TM):
                    etile(_j)

            with contextlib.ExitStack() as sc:
                P.barrier()
                def sbl(name, shape, dt=F32, sc=sc):
                    return sc.enter_context(nc.sbuf_tensor(f"{name}_{l}", list(shape), dt))
                r1 = [sbl(f"r1{k}", [128, D]) for k in range(2)]
                r2 = [sbl(f"r2{k}", [128, D]) for k in range(2)]
                xc = [sbl(f"xc{k}", [128, D]) for k in range(2)]
                fgb = sbl("fgb", [128, D])
                ssf = sbl("ssf", [128, NB])
                rrf = sbl("rrf", [128, NB])
                junk2 = sbl("junk2", [128, D], BF16)
                ally = [f"ysortw{j}" for j in range(TM)]
                if last:
                    P.dma("sp", lambda E: E.dma_start(out=fgb[:], in_=final_g.ap().partition_broadcast(128)), w=["fgb"])
                    P.op("act", lambda E: E.mul(out=fgb[:], in_=fgb[:], mul=32.0), r=["fgb"], w=["fgb"])
                    P.op("dve", lambda E: E.memset(ssf[:], 0.0), w=["ssf"])
                def cblock(b):
                    k = b % 2
                    rows = slice(b * 128, (b + 1) * 128)
                    P.dma("pool", lambda E, b=b, k=k: E.indirect_dma_start(out=r1[k][:], out_offset=None, in_=ysort[:, :],
                                                                          in_offset=bass.IndirectOffsetOnAxis(ap=pos1[:, b:b + 1], axis=0)),
                          r=ally + ["pos1"], w=[f"r1{k}"])
                    P.dma("pool", lambda E, b=b, k=k: E.indirect_dma_start(out=r2[k][:], out_offset=None, in_=ysort[:, :],
                                                                          in_offset=bass.IndirectOffsetOnAxis(ap=pos2[:, b:b + 1], axis=0)),
                          r=ally + ["pos2"], w=[f"r2{k}"])
                    P.dma("sp", lambda E, k=k, rows=rows: E.dma_start(out=xc[k][:], in_=xs1[rows, :]), r=[f"xs1_{b // 4}"], w=[f"xc{k}"])
                    P.op("act", lambda E, b=b, k=k: E.activation(out=r1[k][:], in_=r1[k][:], func=AF.Copy, scale=w1[:, b:b + 1]),
                         r=[f"r1{k}", "w1"], w=[f"r1{k}"])
                    P.op("dve", lambda E, b=b, k=k: E.scalar_tensor_tensor(out=r2[k][:], in0=r2[k][:], scalar=w2[:, b:b + 1], in1=r1[k][:],
                                                                          op0=ALU.mult, op1=ALU.add), r=[f"r2{k}", f"r1{k}", "w2"], w=[f"r2{k}"])
                    P.op("pool", lambda E, k=k: E.tensor_tensor(out=r2[k][:], in0=r2[k][:], in1=G2, op=ALU.mult), r=[f"r2{k}", "mod"], w=[f"r2{k}"])
                    P.op("dve", lambda E, k=k: E.tensor_tensor(out=xc[k][:], in0=xc[k][:], in1=r2[k][:], op=ALU.add), r=[f"xc{k}", f"r2{k}"],
                         w=[f"xc{k}"])
                    if not last:
                        P.dma("act", lambda E, k=k, rows=rows: E.dma_start(out=xs2[rows, :], in_=xc[k][:]), r=[f"xc{k}"], w=[f"xs2_{b}"])
                    else:
                        P.op("act", lambda E, b=b, k=k: E.activation(out=junk2[:], in_=xc[k][:], func=AF.Square, accum_out=ssf[:, b:b + 1]),
                             r=[f"xc{k}"], w=["junk2", "ssf"])
                        P.op("act", lambda E, b=b: E.activation(out=rrf[:, b:b + 1], in_=ssf[:, b:b + 1], func=AF.Sqrt, bias=epsr[:, 0:1]),
                             r=["ssf", "epsr"], w=["rrf"])
                        P.op("dve", lambda E, b=b: E.reciprocal(out=rrf[:, b:b + 1], in_=rrf[:, b:b + 1]), r=["rrf"], w=["rrf"])
                        P.op("dve", lambda E, b=b, k=k: E.scalar_tensor_tensor(out=xc[k][:], in0=xc[k][:], scalar=rrf[:, b:b + 1], in1=fgb[:],
                                                                              op0=ALU.mult, op1=ALU.mult), r=[f"xc{k}", "rrf", "fgb"], w=[f"xc{k}"])
                        P.dma("act", lambda E, k=k, rows=rows: E.dma_start(out=out_d[rows, :], in_=xc[k][:]), r=[f"xc{k}"], w=[f"out_{b}"])
                for _b in range(NB):
                    cblock(_b)
        for _l in range(DEPTH_):
            layer(_l)
        P.op("sp", lambda E: E.nop(), r=[f"out_{b}" for b in range(NB)] + [k for k in ("dbg1", "dbg2", "dbg2b", "dbg3", "dbg4", "dbg_d_pos1",
             "dbg_d_pos2", "dbg_d_w1", "dbg_d_w2", "dbg_d_te")], w=["done"])
        P.emit(sems)
    return nc


_CACHE = {}


def _prep(inputs, S):
    f = lambda a: np.ascontiguousarray(np.asarray(a, dtype=np.float32))
    pvec = np.concatenate([f(inputs["conv_w"]), f(inputs["conv_b"])[:, None, :], f(inputs["conv_ln_g"])[:, None, :],
                           f(inputs["conv_ln_b"])[:, None, :], f(inputs["pool_scale"])[:, None, :]], axis=1)
    rw = np.concatenate([f(inputs["router_group_w"]), f(inputs["router_expert_w"])], axis=2)
    rb = np.concatenate([f(inputs["router_group_b"]), f(inputs["router_expert_b"])], axis=1)
    shared = dict(ada_w=f(inputs["ada_w"]), ada_b=f(inputs["ada_b"]), norm1_g=f(inputs["norm1_g"]), w_in=f(inputs["w_in"]),
                  pool_w=f(inputs["pool_w"]), pvec=np.ascontiguousarray(pvec), w_out=f(inputs["w_out"]), norm2_g=f(inputs["norm2_g"]),
                  rw=np.ascontiguousarray(rw), rb=np.ascontiguousarray(rb), final_g=f(inputs["final_g"]))
    for nm, key, nch in (("wg", "expert_w_gate", 8), ("wu", "expert_w_up", 8), ("wd", "expert_w_down", 2)):
        w = f(inputs[key])
        L, E_, K, F_ = w.shape
        wr_ = w.reshape(L, E_, nch, 128, F_).transpose(0, 1, 3, 2, 4).reshape(L, E_ * 128, nch * F_)
        for l in range(L):
            shared[f"{nm}{l}"] = np.ascontiguousarray(wr_[l])
    x = f(inputs["x"])
    c = f(inputs["c"])
    in_maps = []
    for b in range(x.shape[0]):
        m = dict(shared)
        m["x"] = np.ascontiguousarray(x[b])
        m["c"] = np.ascontiguousarray(c[b])
        in_maps.append(m)
    return in_maps


def kernel(**inputs):
    x = np.asarray(inputs["x"])
    B, S, _ = x.shape
    assert B == N_CORES
    if S not in _CACHE:
        _CACHE[S] = build(S)
    nc = _CACHE[S]
    in_maps = _prep(inputs, S)
    res = run_bass_kernel_spmd(nc, in_maps, core_ids=list(range(B)))
    return np.stack([np.asarray(r["out"], dtype=np.float32) for r in res.results], axis=0)
```

```python
import numpy as np
import concourse.bass as bass
import concourse.mybir as mybir
from concourse.bass_utils import run_bass_kernel_spmd

F32 = mybir.dt.float32
BF16 = mybir.dt.bfloat16
I32 = mybir.dt.int32
AF = mybir.ActivationFunctionType
ALU = mybir.AluOpType
AX = mybir.AxisListType

D = 1024
DEPTH = 2
NE = 32
HID = 256
RMS_EPS = 1e-6
LN_EPS = 1e-5
POOL_WINDOWS = (2, 4, 8, 16)
CK = 31
N_CORES = 8


class Prog:
    ENGS = ("pe", "act", "dve", "pool", "sp")

    def __init__(self, nc, n_dma_sems=6):
        self.nc = nc
        self.ops = []
        self.n_dma_sems = n_dma_sems

    def op(self, eng, fn, r=(), w=()):
        self.ops.append(dict(eng=eng, fn=fn, r=tuple(r), w=tuple(w), dma=False))

    def dma(self, q, fn, r=(), w=()):
        self.ops.append(dict(eng=q, fn=fn, r=tuple(r), w=tuple(w), dma=True))

    def barrier(self):
        self.ops.append(dict(eng=None, fn=None, r=(), w=(), dma=False, barrier=True))

    def emit(self, sems):
        nc = self.nc
        engobj = dict(pe=nc.tensor, act=nc.scalar, dve=nc.vector, pool=nc.gpsimd, sp=nc.sync)
        ops = self.ops
        n = len(ops)
        last_w = {}
        readers = {}
        clock = {e: dict(pe=-1, act=-1, dve=-1, pool=-1, sp=-1, dma=set()) for e in self.ENGS}
        opclock = [None] * n
        dma_hist = {e: [] for e in self.ENGS}
        waits = [None] * n
        sig = [False] * n
        bar_deps = set()
        last_on = {}
        for i, o in enumerate(ops):
            e = o["eng"]
            if e is None:
                bar_deps = set(last_on.values())
                for q in self.ENGS:
                    bar_deps |= set(dma_hist[q][-self.n_dma_sems:])
                waits[i] = []
                opclock[i] = None
                continue
            deps = set(bar_deps)
            for r in o["r"]:
                if r in last_w:
                    deps.add(last_w[r])
            for w in o["w"]:
                if w in last_w:
                    deps.add(last_w[w])
                for j in readers.get(w, ()):
                    deps.add(j)
            if o["dma"]:
                h = dma_hist[e]
                if len(h) >= self.n_dma_sems:
                    deps.add(h[-self.n_dma_sems])
                h.append(i)
            deps.discard(i)
            need = []
            ck = clock[e]
            for j in sorted(deps):
                pj = ops[j]
                if pj["dma"]:
                    if j in ck["dma"]:
                        continue
                else:
                    if pj["eng"] == e and e == "pe" and not o["dma"]:
                        continue
                    if ck[pj["eng"]] >= j:
                        continue
                need.append(j)
                sig[j] = True
                oc = opclock[j]
                for k in ("pe", "act", "dve", "pool", "sp"):
                    if oc[k] > ck[k]:
                        ck[k] = oc[k]
                ck["dma"] |= oc["dma"]
            waits[i] = need
            oc = dict(pe=ck["pe"], act=ck["act"], dve=ck["dve"], pool=ck["pool"], sp=ck["sp"], dma=set(ck["dma"]))
            if o["dma"]:
                oc["dma"].add(i)
            else:
                oc[e] = i
            opclock[i] = oc
            if not o["dma"]:
                last_on[e] = i
            for r in o["r"]:
                readers.setdefault(r, []).append(i)
            for w in o["w"]:
                last_w[w] = i
                readers[w] = []
        cnt = {e: 0 for e in self.ENGS}
        dcnt = {}
        dma_n = {e: 0 for e in self.ENGS}
        ev = [None] * n
        for i, o in enumerate(ops):
            e = o["eng"]
            if e is None:
                continue
            E = engobj[e]
            for j in waits[i]:
                s, v = ev[j]
                E.wait_ge(s, v)
            inst = o["fn"](E)
            if o["dma"]:
                k = dma_n[e] % self.n_dma_sems
                dma_n[e] += 1
                s = sems["dma_" + e][k]
                dcnt[(e, k)] = dcnt.get((e, k), 0) + 16
                inst.then_inc(s, 16)
                ev[i] = (s, dcnt[(e, k)])
            elif sig[i]:
                cnt[e] += 1
                inst.then_inc(sems[e], 1)
                ev[i] = (sems[e], cnt[e])
        return ev


def build(S, depth=DEPTH, debug=False):
    DEPTH_ = depth
    NB = S // 128
    NT = S // 512
    SL = 256
    NB2 = (S + SL - 1) // SL
    TM = 2 * S // SL + NE
    nc = bass.Bass("TRN2", target_bir_lowering=False)
    P = Prog(nc)

    def dram_in(name, shape, dt=F32):
        return nc.dram_tensor(name, list(shape), dt, kind="ExternalInput")

    x_d = dram_in("x", [S, D])
    c_d = dram_in("c", [D])
    ada_w = dram_in("ada_w", [DEPTH, D, 6 * D])
    ada_b = dram_in("ada_b", [DEPTH, 6 * D])
    norm1_g = dram_in("norm1_g", [DEPTH, D])
    w_in = dram_in("w_in", [DEPTH, D, 1536])
    pool_w = dram_in("pool_w", [DEPTH, 4, 128, 128])
    pvec = dram_in("pvec", [DEPTH, 35, 512])
    w_out = dram_in("w_out", [DEPTH, D, D])
    norm2_g = dram_in("norm2_g", [DEPTH, D])
    rw = dram_in("rw", [DEPTH, D, 36])
    rb = dram_in("rb", [DEPTH, 36])
    wg_d = [dram_in(f"wg{l}", [NE * 128, 8 * HID]) for l in range(DEPTH)]
    wu_d = [dram_in(f"wu{l}", [NE * 128, 8 * HID]) for l in range(DEPTH)]
    wd_d = [dram_in(f"wd{l}", [NE * 128, 2 * D]) for l in range(DEPTH)]
    final_g = dram_in("final_g", [D])
    out_d = nc.dram_tensor("out", [S, D], F32, kind="ExternalOutput")
    sk = "ExternalOutput" if debug else "Internal"
    xs1 = nc.dram_tensor("xs1", [S, D], F32, kind=sk)
    xs2 = nc.dram_tensor("xs2", [S, D], F32, kind=sk)
    h2s = nc.dram_tensor("h2s", [S, D], BF16, kind=sk)
    hsort = nc.dram_tensor("hsort", [TM * SL, D], BF16, kind=sk)
    ysort = nc.dram_tensor("ysort", [TM * SL, D], F32, kind=sk)
    dbg = {}
    if debug:
        for nm, shp, dt in (("d_mod", [128, 6 * D], F32), ("d_hT", [128, 8 * 512], BF16), ("d_ycat", [128, 8 * 512], BF16),
                            ("d_lg", [128, 4 * 36], F32), ("d_pos1", [128, NB], I32), ("d_pos2", [128, NB], I32),
                            ("d_w1", [128, NB], F32), ("d_w2", [128, NB], F32), ("d_te", [128, TM], I32), ("d_pvT", [128, 4 * 35], F32)):
            dbg[nm] = nc.dram_tensor(nm, shp, dt, kind="ExternalOutput")

    import contextlib
    es = contextlib.ExitStack()
    with es:
        def sb(name, shape, dt=F32):
            return es.enter_context(nc.sbuf_tensor(name, list(shape), dt))

        def psum(name, shape, dt=F32):
            return es.enter_context(nc.psum_tensor(name, list(shape), dt))

        sems = {}
        for e in Prog.ENGS:
            sems[e] = es.enter_context(nc.semaphore("s_" + e))
            sems["dma_" + e] = [es.enter_context(nc.semaphore(f"d_{e}{k}")) for k in range(P.n_dma_sems)]

        identf = sb("identf", [128, 128])
        identb = sb("identb", [128, 128], BF16)
        onesf = sb("onesf", [128, 128])
        onesb = sb("onesb", [128, 128], BF16)
        trib = sb("trib", [128, 128], BF16)
        trif = sb("trif", [128, 128])
        bavg = sb("bavg", [128, 128], BF16)
        bavgf = sb("bavgf", [128, 128])
        inv16 = sb("inv16", [128, 16])
        iota_i = sb("iota_i", [128, 128], I32)
        iota_f = sb("iota_f", [128, 128])

        P.op("pool", lambda E: E.memset(identf[:], 0.0), w=["identf"])
        P.op("pool", lambda E: E.affine_select(out=identf[:], in_=identf[:], pattern=[[-1, 128]], compare_op=ALU.not_equal,
                                               fill=1.0, base=0, channel_multiplier=1), r=["identf"], w=["identf"])
        P.op("pool", lambda E: E.tensor_copy(out=identb[:], in_=identf[:]), r=["identf"], w=["identb"])
        P.op("pool", lambda E: E.memset(onesf[:], 1.0), w=["onesf"])
        P.op("pool", lambda E: E.memset(onesb[:], 1.0), w=["onesb"])
        P.op("pool", lambda E: E.memset(trif[:], 1.0), w=["trif"])
        P.op("pool", lambda E: E.affine_select(out=trif[:], in_=trif[:], pattern=[[1, 128]], compare_op=ALU.is_gt,
                                               fill=0.0, base=0, channel_multiplier=-1), r=["trif"], w=["trif"])
        P.op("pool", lambda E: E.tensor_copy(out=trib[:], in_=trif[:]), r=["trif"], w=["trib"])
        P.op("pool", lambda E: E.memset(bavgf[:], 0.0), w=["bavgf"])
        P.op("pool", lambda E: E.memset(bavgf[0:64, 0:64], 1.0 / 64), r=["bavgf"], w=["bavgf"])
        P.op("pool", lambda E: E.memset(bavgf[64:128, 64:128], 1.0 / 64), r=["bavgf"], w=["bavgf"])
        P.op("pool", lambda E: E.tensor_copy(out=bavg[:], in_=bavgf[:]), r=["bavgf"], w=["bavg"])
        P.op("pool", lambda E: E.iota(iota_i[:], pattern=[[1, 128]], base=0, channel_multiplier=0), w=["iota_i"])
        P.op("pool", lambda E: E.tensor_copy(out=iota_f[:], in_=iota_i[:]), r=["iota_i"], w=["iota_f"])
        epsr = sb("epsr", [128, 1])
        epsl = sb("epsl", [128, 1])
        P.op("pool", lambda E: E.memset(epsr[:], D * RMS_EPS), w=["epsr"])
        P.op("pool", lambda E: E.memset(epsl[:], LN_EPS), w=["epsl"])
        pidx_i = sb("pidx_i", [128, 1], I32)
        pidx_f = sb("pidx_f", [128, 1])
        P.op("pool", lambda E: E.iota(pidx_i[:], pattern=[[0, 1]], base=0, channel_multiplier=1), w=["pidx_i"])
        P.op("pool", lambda E: E.tensor_copy(out=pidx_f[:], in_=pidx_i[:]), r=["pidx_i"], w=["pidx_f"])
        P.op("dve", lambda E: E.tensor_scalar(out=inv16[:], in0=iota_f[:, 0:16], scalar1=1.0, scalar2=None, op0=ALU.add),
             r=["iota_f"], w=["inv16"])
        P.op("dve", lambda E: E.reciprocal(out=inv16[:], in_=inv16[:]), r=["inv16"], w=["inv16"])

        bcd = {}

        def mk_bc(E):
            reg = E.alloc_register("bcreg")
            inst = E.reg_mov(reg, NE * 128 - 1)
            bcd["v"] = E.snap(reg, donate=True)
            return E.memset(epsl[:], LN_EPS)
        P.op("pool", mk_bc, w=["epsl"])
        NFB = 5
        psf = [psum(f"psf{i}", [128, 512]) for i in range(NFB)]
        psb = [psum(f"psb{i}", [128, 1024], BF16) for i in range(2)]
        psr = psum("psr", [128, 512])
        bank_ctr = [0, 0]

        def fbank():
            i = bank_ctr[0] % NFB
            bank_ctr[0] += 1
            return psf[i], f"psf{i}"

        def bbank():
            i = bank_ctr[1] % 2
            bank_ctr[1] += 1
            return psb[i], f"psb{i}"

        c_sb = sb("c_sb", [128, 8])
        cond = sb("cond", [128, 8])
        condbc = sb("condbc", [128, 8, 128])
        P.dma("sp", lambda E: E.dma_start(out=c_sb[:], in_=c_d.ap().rearrange("(c p) -> p c", p=128), allow_slow_non_contiguous=True),
              w=["c_sb"])
        P.op("act", lambda E: E.activation(out=cond[:], in_=c_sb[:], func=AF.Silu), r=["c_sb"], w=["cond"])
        for c in range(8):
            P.op("act", lambda E, c=c: E.activation(out=condbc[:, c, :], in_=onesf[:], func=AF.Copy, scale=cond[:, c:c + 1]),
                 r=["cond", "onesf"], w=["condbc"])

        mod = sb("mod", [128, 6 * D])
        oh1 = sb("oh1", [128, NB, NE], BF16)
        oh2 = sb("oh2", [128, NB, NE], BF16)
        Aall = sb("Aall", [128, NB, NE], BF16)
        w1 = sb("w1", [128, NB])
        w2 = sb("w2", [128, NB])
        pos1 = sb("pos1", [128, NB], I32)
        pos2 = sb("pos2", [128, NB], I32)
        te_i = sb("te_i", [128, TM], I32)
        lg_all = sb("lg_all", [128, NB, 36])

        B1, A1, G1, B2, A2, G2 = (mod[:, k * D:(k + 1) * D] for k in range(6))

        def layer(l):
            x_src = x_d if l == 0 else xs2
            last = (l == DEPTH_ - 1)
            with contextlib.ExitStack() as sc:
                P.barrier()
                def sbl(name, shape, dt=F32, sc=sc):
                    return sc.enter_context(nc.sbuf_tensor(f"{name}_{l}", list(shape), dt))
                aw = [sbl(f"aw{k}", [128, 8, 512]) for k in range(2)]
                ab = [sbl(f"ab{k}", [128, 512]) for k in range(2)]
                gbc = sbl("gbc", [128, D])
                for nn in range(12):
                    k = nn % 2
                    P.dma("sp", lambda E, nn=nn, k=k: E.dma_start(
                        out=aw[k][:], in_=ada_w[l, :, nn * 512:(nn + 1) * 512].rearrange("(c p) n -> p c n", p=128)),
                        w=[f"aw{k}"])
                    P.dma("sp", lambda E, nn=nn, k=k: E.dma_start(
                        out=ab[k][:], in_=ada_b[l, nn * 512:(nn + 1) * 512].partition_broadcast(128)), w=[f"ab{k}"])
                    pb, pbn = fbank()
                    for c in range(8):
                        P.op("pe", lambda E, c=c, pb=pb, k=k: E.matmul(pb[:], lhsT=condbc[:, c, :], rhs=aw[k][:, c, :],
                                                                      start=(c == 0), stop=(c == 7)),
                             r=["condbc", f"aw{k}"], w=[pbn])
                    P.op("dve", lambda E, nn=nn, pb=pb, k=k: E.tensor_tensor(out=mod[:, nn * 512:(nn + 1) * 512], in0=pb[:], in1=ab[k][:],
                                                                             op=ALU.add), r=[f"ab{k}"], w=[pbn, "mod"])
                for (gsrc, Aap) in ((norm1_g, A1), (norm2_g, A2)):
                    P.dma("sp", lambda E, gsrc=gsrc: E.dma_start(out=gbc[:], in_=gsrc[l, :].partition_broadcast(128)), w=["gbc"])
                    P.op("act", lambda E: E.mul(out=gbc[:], in_=gbc[:], mul=32.0), r=["gbc"], w=["gbc"])
                    P.op("dve", lambda E, Aap=Aap: E.scalar_tensor_tensor(out=Aap, in0=Aap, scalar=1.0, in1=gbc[:], op0=ALU.add,
                                                                          op1=ALU.mult), r=["gbc", "mod"], w=["mod"])
            with contextlib.ExitStack() as sc:
                P.barrier()
                def sbl(name, shape, dt=F32, sc=sc):
                    return sc.enter_context(nc.sbuf_tensor(f"{name}_{l}", list(shape), dt))
                win = sbl("win", [128, 8, 1536], BF16)
                wout = sbl("wout", [128, 8, D], BF16)
                pw = sbl("pw", [128, 4, 128], BF16)
                wr = sbl("wr", [128, 8, 36])
                rbias = sbl("rbias", [128, 4, 36])
                pv = sbl("pv", [35, 512])
                pvT = sbl("pvT", [128, 4, 35])
                diag = sbl("diag", [128, CK, 4, 128], BF16)
                xt = [sbl(f"xt{k}", [128, 4, D]) for k in range(1)]
                ss = sbl("ss", [128, 8])
                rr = sbl("rr", [128, 8])
                tmpA = [sbl(f"tmpA{k}", [128, D]) for k in range(2)]
                hb = sbl("hb", [128, 4, D], BF16)
                hT = sbl("hT", [128, 8, 512], BF16)
                u = sbl("u", [128, 4, 528])
                sA = sbl("sA", [128, 528])
                sB = sbl("sB", [128, 528])
                pooled = sbl("pooled", [128, 4, 512], BF16)
                ycat = sbl("ycat", [128, 8, 512], BF16)
                vbuf = sbl("vbuf", [128, 4, 544], BF16)
                sig = [sbl(f"sig{k}", [128, 512]) for k in range(1)]
                ybf = [sbl(f"ybf{k}", [128, 512], BF16) for k in range(2)]
                dd = [sbl(f"dd{k}", [128, 512]) for k in range(1)]
                sq = [sbl(f"sq{k}", [128, 512], BF16) for k in range(1)]
                rs = [sbl(f"rs{k}", [128, 512]) for k in range(1)]
                yn = dd
                tmpO = [sbl(f"tmpO{k}", [128, 512]) for k in range(2)]
                h2b = hb
                h2T = [sbl(f"h2T{k}", [128, 8, 128]) for k in range(1)]

                for c in range(8):
                    P.dma("pool", lambda E, c=c: E.dma_start(out=win[:, c, :], in_=w_in[l, c * 128:(c + 1) * 128, :]), w=["win"])
                for c in range(8):
                    P.dma("pool", lambda E, c=c: E.dma_start(out=wout[:, c, :], in_=w_out[l, c * 128:(c + 1) * 128, :]), w=["wout"])
                P.dma("pool", lambda E: E.dma_start(out=pw[:], in_=pool_w[l].rearrange("g c d -> c g d")), w=["pw"])
                P.dma("sp", lambda E: E.dma_start(out=wr[:], in_=rw[l].rearrange("(c p) g -> p c g", p=128)), w=["wr"])
                for j in range(4):
                    P.dma("sp", lambda E, j=j: E.dma_start(out=rbias[:, j, :], in_=rb[l, :].partition_broadcast(128)), w=["rbias"])
                P.dma("sp", lambda E: E.dma_start(out=pv[:], in_=pvec[l]), w=["pv"])
                for c in range(4):
                    pb, pbn = fbank()
                    P.op("pe", lambda E, c=c, pb=pb: E.transpose(out=pb[:, 0:35], in_=pv[:, c * 128:(c + 1) * 128], identity=identf[0:35, 0:35]),
                         r=["pv", "identf"], w=[pbn])
                    P.op("act", lambda E, c=c, pb=pb: E.activation(out=pvT[:, c, :], in_=pb[:, 0:35], func=AF.Copy), w=[pbn, "pvT"])
                for k in range(CK):
                    for c in range(4):
                        eng = "dve" if (k * 4 + c) % 2 == 0 else "pool"
                        P.op(eng, lambda E, k=k, c=c: E.tensor_scalar(out=diag[:, k, c, :], in0=identf[:], scalar1=pvT[:, c, k:k + 1],
                                                                      scalar2=None, op0=ALU.mult),
                             r=["identf", "pvT"], w=[f"diag{k}_{c}"])
                P.op("pool", lambda E: E.memset(u[:], 0.0), w=["u0", "u1", "u2", "u3"])
                P.op("pool", lambda E: E.memset(vbuf[:], 0.0), w=["v0", "v1", "v2", "v3"])

                def mtile(i):
                    X = xt[0]
                    Xn = "xt0"
                    rows = slice(i * 512, (i + 1) * 512)
                    P.dma("sp", lambda E, X=X, rows=rows: E.dma_start(out=X[:], in_=x_src[rows, :].rearrange("(j p) d -> p j d", p=128)),
                          r=[f"xs2_{4 * i + q}" for q in range(4)], w=[Xn])
                    P.op("dve", lambda E: E.memset(ss[:], 0.0), w=["ss"])
                    for j in range(4):
                        P.op("act", lambda E, j=j, X=X: E.activation(out=hb[:, j, :], in_=X[:, j, :], func=AF.Square, accum_out=ss[:, j:j + 1]),
                             r=[Xn], w=[f"hb{j}", "ss"])
                    P.op("act", lambda E: E.activation(out=rr[:, 0:4], in_=ss[:, 0:4], func=AF.Sqrt, bias=epsr[:, 0:1]), r=["ss", "epsr"], w=["rr"])
                    P.op("dve", lambda E: E.reciprocal(out=rr[:, 0:4], in_=rr[:, 0:4]), r=["rr"], w=["rr"])
                    for j in range(4):
                        T = tmpA[j % 2]
                        Tn = f"tmpA{j % 2}"
                        P.op("dve", lambda E, j=j, X=X, T=T: E.scalar_tensor_tensor(out=T[:], in0=X[:, j, :], scalar=rr[:, j:j + 1], in1=A1,
                                                                                    op0=ALU.mult, op1=ALU.mult),
                             r=[Xn, "rr", "mod"], w=[Tn])
                        P.op("pool", lambda E, j=j, T=T: E.tensor_tensor(out=hb[:, j, :], in0=T[:], in1=B1, op=ALU.add),
                             r=[Tn, "mod"], w=[f"hb{j}"])
                    for j in range(4):
                        pb, pbn = bbank()
                        for c in range(8):
                            P.op("pe", lambda E, j=j, c=c, pb=pb: E.transpose(out=pb[:, c * 128:(c + 1) * 128], in_=hb[:, j, c * 128:(c + 1) * 128],
                                                                             identity=identb[:]),
                                 r=[f"hb{j}", "identb"], w=[pbn])
                        eng = "act" if j % 2 == 0 else "dve"
                        if eng == "act":
                            P.op("act", lambda E, j=j, pb=pb: E.activation(out=hT[:, :, j * 128:(j + 1) * 128],
                                                                           in_=pb[:].rearrange("p (c t) -> p c t", c=8), func=AF.Copy),
                                 w=[pbn, f"hT{j}"])
                        else:
                            P.op("dve", lambda E, j=j, pb=pb: E.tensor_copy(out=hT[:, :, j * 128:(j + 1) * 128],
                                                                            in_=pb[:].rearrange("p (c t) -> p c t", c=8)),
                                 w=[pbn, f"hT{j}"])
                    hTr = ["hT0", "hT1", "hT2", "hT3"]
                    if debug and l == 0 and i == 0:
                        P.dma("sp", lambda E: E.dma_start(out=dbg["d_hT"].ap(), in_=hT[:].rearrange("p c t -> p (c t)")), r=hTr, w=["dbg1"])
                        P.dma("sp", lambda E: E.dma_start(out=dbg["d_mod"].ap(), in_=mod[:]), r=["mod"], w=["dbg2"])
                        P.dma("sp", lambda E: E.dma_start(out=dbg["d_pvT"].ap(), in_=pvT[:].rearrange("p c t -> p (c t)")), r=["pvT"], w=["dbg2b"])

                    def win_mm(oc):
                        pb, pbn = fbank()
                        for c in range(8):
                            P.op("pe", lambda E, c=c, pb=pb, oc=oc: E.matmul(pb[:], lhsT=win[:, c, oc * 128:(oc + 1) * 128], rhs=hT[:, c, :],
                                                                           start=(c == 0), stop=(c == 7)),
                                 r=["win"] + hTr, w=[pbn])
                        return pb, pbn

                    def pU(g):
                        pb, pbn = win_mm(g)
                        P.op("act", lambda E, pb=pb: E.activation(out=u[:, g, 16:528], in_=pb[:], func=AF.Copy), w=[pbn, f"u{g}"])

                    def pD(g):
                        wdw = POOL_WINDOWS[g]
                        un = f"u{g}"
                        step = 1
                        bufs = [sA, sB]
                        bi = 0
                        cur = None
                        lo = 0
                        while step < wdw:
                            dst = bufs[bi]
                            dn = "sA" if bi == 0 else "sB"
                            nlo = lo + step
                            if cur is None:
                                P.op("pool", lambda E, dst=dst, nlo=nlo, step=step: E.tensor_tensor(
                                    out=dst[:, nlo:528], in0=u[:, g, nlo:528], in1=u[:, g, nlo - step:528 - step], op=ALU.add),
                                    r=[un], w=[dn])
                            else:
                                cs, cn = cur
                                P.op("pool", lambda E, dst=dst, cs=cs, nlo=nlo, step=step: E.tensor_tensor(
                                    out=dst[:, nlo:528], in0=cs[:, nlo:528], in1=cs[:, nlo - step:528 - step], op=ALU.add),
                                    r=[cn], w=[dn])
                            cur = (dst, dn)
                            lo = nlo
                            step *= 2
                            bi ^= 1
                        cs, cn = cur
                        P.op("dve", lambda E, cs=cs, wdw=wdw: E.scalar_tensor_tensor(
                            out=pooled[:, g, :], in0=cs[:, 16:528], scalar=1.0 / wdw, in1=u[:, g, 16:528], op0=ALU.mult, op1=ALU.subtract),
                            r=[cn, un], w=[f"pooled{g}"])
                        if i == 0:
                            nfix = wdw - 1
                            P.op("pool", lambda E, cs=cs, nfix=nfix: E.tensor_tensor(out=cs[:, 16:16 + nfix], in0=cs[:, 16:16 + nfix],
                                                                                    in1=inv16[:, 0:nfix], op=ALU.mult),
                                 r=[cn, "inv16", f"pooled{g}"], w=[cn])
                            P.op("pool", lambda E, cs=cs, nfix=nfix: E.tensor_tensor(out=pooled[:, g, 0:nfix], in0=cs[:, 16:16 + nfix],
                                                                                    in1=u[:, g, 16:16 + nfix], op=ALU.subtract),
                                 r=[cn, un], w=[f"pooled{g}"])
                        P.op("pool", lambda E: E.tensor_copy(out=u[:, g, 0:16], in_=u[:, g, 512:528]), r=[un], w=[un])

                    def pPW(g):
                        pb2, pb2n = fbank()
                        P.op("pe", lambda E, pb2=pb2: E.matmul(pb2[:], lhsT=pw[:, g, :], rhs=pooled[:, g, :], start=True, stop=True),
                             r=["pw", f"pooled{g}"], w=[pb2n])
                        P.op("act", lambda E, pb2=pb2: E.activation(out=ycat[:, g, :], in_=pb2[:], func=AF.Copy, scale=pvT[:, g, 34:35]),
                             r=["pvT"], w=[pb2n, f"ycat{g}"])

                    def cF(c):
                        kb = c % 2
                        vn = f"v{c}"
                        pa, pan = win_mm(4 + c)
                        pg, pgn = win_mm(8 + c)
                        P.op("act", lambda E, pg=pg: E.activation(out=sig[0][:], in_=pg[:], func=AF.Sigmoid), w=[pgn, "sig0"])
                        P.op("dve", lambda E, pa=pa: E.tensor_tensor(out=vbuf[:, c, 32:544], in0=pa[:], in1=sig[0][:], op=ALU.mult),
                             r=["sig0"], w=[pan, vn])
                        py, pyn = fbank()
                        for k in range(CK):
                            P.op("pe", lambda E, k=k, py=py: E.matmul(py[:], lhsT=diag[:, k, c, :], rhs=vbuf[:, c, 2 + k:2 + k + 512],
                                                                    start=(k == 0), stop=(k == CK - 1)),
                                 r=[f"diag{k}_{c}", vn], w=[pyn])
                        P.op("pool", lambda E: E.tensor_copy(out=vbuf[:, c, 2:32], in_=vbuf[:, c, 514:544]), r=[vn], w=[vn])
                        P.op("act", lambda E, py=py: E.activation(out=ybf[kb][:], in_=py[:], func=AF.Identity, bias=pvT[:, c, 31:32]),
                             r=["pvT"], w=[pyn, f"ybf{kb}"])

                    def cB1(c):
                        kb = c % 2
                        pm, pmn = fbank()
                        P.op("pe", lambda E, pm=pm: E.matmul(pm[:], lhsT=bavg[:], rhs=ybf[kb][:], start=True, stop=True),
                             r=["bavg", f"ybf{kb}"], w=[pmn])
                        P.op("dve", lambda E, pm=pm: E.tensor_tensor(out=dd[0][:], in0=ybf[kb][:], in1=pm[:], op=ALU.subtract),
                             r=[f"ybf{kb}"], w=[pmn, "dd0"])
                        P.op("act", lambda E: E.activation(out=sq[0][:], in_=dd[0][:], func=AF.Square), r=["dd0"], w=["sq0"])

                    def cB2(c):
                        pvv, pvn = fbank()
                        P.op("pe", lambda E, pvv=pvv: E.matmul(pvv[:], lhsT=bavg[:], rhs=sq[0][:], start=True, stop=True),
                             r=["bavg", "sq0"], w=[pvn])
                        P.op("act", lambda E, pvv=pvv: E.activation(out=rs[0][:], in_=pvv[:], func=AF.Sqrt, bias=epsl[:, 0:1]),
                             r=["epsl"], w=[pvn, "rs0"])
                        P.op("dve", lambda E: E.reciprocal(out=rs[0][:], in_=rs[0][:]), r=["rs0"], w=["rs0"])
                        P.op("pool", lambda E: E.tensor_tensor(out=dd[0][:], in0=dd[0][:], in1=rs[0][:], op=ALU.mult),
                             r=["dd0", "rs0"], w=["dd0"])
                        P.op("act", lambda E: E.activation(out=ycat[:, 4 + c, :], in_=dd[0][:], func=AF.Silu, bias=pvT[:, c, 33:34],
                                                           scale=pvT[:, c, 32:33]),
                             r=["dd0", "pvT"], w=[f"ycat{4 + c}"])

                    for g_ in range(4):
                        pU(g_)
                    cF(0); pD(0); cF(1); pD(1); cB1(0); cF(2); pD(2); cB2(0); cB1(1); cF(3); pD(3); cB2(1); cB1(2)
                    pPW(0); pPW(1); cB2(2); cB1(3); pPW(2); pPW(3); cB2(3)
                    ycr = [f"ycat{k}" for k in range(8)]
                    if debug and l == 0 and i == 0:
                        P.dma("sp", lambda E: E.dma_start(out=dbg["d_ycat"].ap(), in_=ycat[:].rearrange("p c t -> p (c t)")), r=ycr, w=["dbg3"])
                    for j in range(4):
                        for dh in range(2):
                            po, pon = fbank()
                            for k in range(8):
                                P.op("pe", lambda E, j=j, dh=dh, k=k, po=po: E.matmul(po[:], lhsT=ycat[:, k, j * 128:(j + 1) * 128],
                                                                                  rhs=wout[:, k, dh * 512:(dh + 1) * 512], start=(k == 0), stop=(k == 7)),
                                     r=ycr + ["wout"], w=[pon])
                            kk = (j * 2 + dh) % 2
                            P.op("dve", lambda E, dh=dh, po=po, kk=kk: E.tensor_tensor(out=tmpO[kk][:], in0=po[:], in1=G1[:, dh * 512:(dh + 1) * 512],
                                                                                      op=ALU.mult), r=["mod"], w=[pon, f"tmpO{kk}"])
                            P.op("pool", lambda E, j=j, dh=dh, X=X, kk=kk: E.tensor_tensor(out=X[:, j, dh * 512:(dh + 1) * 512], in0=tmpO[kk][:],
                                                                                         in1=X[:, j, dh * 512:(dh + 1) * 512], op=ALU.add),
                                 r=[f"tmpO{kk}", Xn], w=[Xn])
                    P.dma("act", lambda E, X=X, rows=rows: E.dma_start(out=xs1[rows, :].rearrange("(j p) d -> p j d", p=128), in_=X[:]),
                          r=[Xn], w=[f"xs1_{i}"])
                    for j in range(4):
                        P.op("act", lambda E, j=j, X=X: E.activation(out=hb[:, j, :], in_=X[:, j, :], func=AF.Square, accum_out=ss[:, 4 + j:5 + j]),
                             r=[Xn], w=[f"hb{j}", "ss"])
                    P.op("act", lambda E: E.activation(out=rr[:, 4:8], in_=ss[:, 4:8], func=AF.Sqrt, bias=epsr[:, 0:1]), r=["ss", "epsr"], w=["rr"])
                    P.op("dve", lambda E: E.reciprocal(out=rr[:, 4:8], in_=rr[:, 4:8]), r=["rr"], w=["rr"])
                    pr, prn = psr, "psr"
                    for j in range(4):
                        T = tmpA[j % 2]
                        Tn = f"tmpA{j % 2}"
                        H = T
                        Hn = Tn
                        P.op("dve", lambda E, j=j, X=X, T=T: E.scalar_tensor_tensor(out=T[:], in0=X[:, j, :], scalar=rr[:, 4 + j:5 + j], in1=A2,
                                                                                    op0=ALU.mult, op1=ALU.mult),
                             r=[Xn, "rr", "mod"], w=[Tn])
                        P.op("pool", lambda E, T=T, H=H: E.tensor_tensor(out=H[:], in0=T[:], in1=B2, op=ALU.add), r=[Tn, "mod"], w=[Hn])
                        P.op("act", lambda E, j=j, H=H: E.activation(out=h2b[:, j, :], in_=H[:], func=AF.Copy), r=[Hn], w=[f"hb{j}"])
                        HT = h2T[0]
                        HTn = "h2T0"
                        for half in range(2):
                            pt, ptn = fbank()
                            for cc in range(4):
                                c = half * 4 + cc
                                P.op("pe", lambda E, c=c, cc=cc, H=H, pt=pt: E.transpose(out=pt[:, cc * 128:(cc + 1) * 128],
                                                                                       in_=H[:, c * 128:(c + 1) * 128], identity=identf[:]),
                                     r=[Hn, "identf"], w=[ptn])
                            eng = "act" if half == 0 else "dve"
                            if eng == "act":
                                P.op("act", lambda E, half=half, HT=HT, pt=pt: E.activation(out=HT[:, half * 4:(half + 1) * 4, :],
                                                                                        in_=pt[:].rearrange("p (c t) -> p c t", c=4), func=AF.Copy),
                                     w=[ptn, HTn])
                            else:
                                P.op("dve", lambda E, half=half, HT=HT, pt=pt: E.tensor_copy(out=HT[:, half * 4:(half + 1) * 4, :],
                                                                                         in_=pt[:].rearrange("p (c t) -> p c t", c=4)),
                                     w=[ptn, HTn])
                        for c in range(8):
                            P.op("pe", lambda E, j=j, c=c, HT=HT, pr=pr: E.matmul(pr[:, j * 36:(j + 1) * 36], lhsT=HT[:, c, :], rhs=wr[:, c, :],
                                                                              start=(c == 0), stop=(c == 7)),
                                 r=[HTn, "wr"], w=[prn])
                    P.dma("act", lambda E, rows=rows: E.dma_start(out=h2s[rows, :].rearrange("(j p) d -> p j d", p=128), in_=h2b[:]), r=["hb0", "hb1", "hb2", "hb3"], w=[f"h2s_{i}"])
                    P.op("dve", lambda E, pr=pr: E.tensor_tensor(out=lg_all[:, 4 * i:4 * i + 4, :], in0=pr[:, 0:144].rearrange("p (j e) -> p j e", j=4),
                                                                 in1=rbias[:], op=ALU.add), r=["rbias"], w=[prn, "lg_all"])
                    if debug and l == 0 and i == 0:
                        P.dma("sp", lambda E: E.dma_start(out=dbg["d_lg"].ap(), in_=lg_all[:, 0:4, :].rearrange("p c t -> p (c t)")), r=["lg_all"], w=["dbg4"])
                for _i in range(NT):
                    mtile(_i)

            with contextlib.ExitStack() as sc:
                P.barrier()
                def sbl(name, shape, dt=F32, sc=sc):
                    return sc.enter_context(nc.sbuf_tensor(f"{name}_{l}", list(shape), dt))
                NC_ = NB * NE
                within = sbl("within", [128, NB, NE])
                tot = [sbl(f"tot{k}", [128, NB, NE]) for k in range(2)]
                tot0 = sbl("totz", [128, NB, NE])
                cmpb = sbl("cmpb", [128, NE, NB2])
                ntile = [sbl(f"ntile{k}", [128, NE]) for k in range(2)]
                nt0 = sbl("ntz", [128, NE])
                basee = sbl("basee", [128, NE])
                tend = sbl("tend", [128, NE])
                posall = sbl("posall", [128, NB, NE])
                ptmp = sbl("ptmp", [128, NB, NE])
                posf = sbl("posf", [128, NB])
                thr = sbl("thr", [128, NB2])
                cmpt = sbl("cmpt", [128, TM, NE])
                tef = sbl("tef", [128, TM])
                gmax = sbl("gmax", [128, NB])
                gd = sbl("gd", [128, NB, 4])
                gsum = sbl("gsum", [128, NB])
                pgrp = sbl("pgrp", [128, NB])
                ohg = sbl("ohg", [128, NB, 4])
                em = sbl("em", [128, NB, NE])
                em2 = sbl("em2", [128, NB, NE])
                m1 = sbl("m1", [128, NB])
                m2 = sbl("m2", [128, NB])
                d12 = sbl("d12", [128, NB])
                gl = lg_all[:, :, 0:4]
                el = lg_all[:, :, 4:36]
                P.op("dve", lambda E: E.tensor_reduce(out=gmax[:], in_=gl, axis=AX.X, op=ALU.max), r=["lg_all"], w=["gmax"])
                P.op("dve", lambda E: E.tensor_tensor(out=gd[:], in0=gl, in1=gmax[:].unsqueeze(2).to_broadcast([128, NB, 4]), op=ALU.subtract),
                     r=["lg_all", "gmax"], w=["gd"])
                P.op("dve", lambda E: E.tensor_tensor(out=ohg[:], in0=gl, in1=gmax[:].unsqueeze(2).to_broadcast([128, NB, 4]), op=ALU.is_equal),
                     r=["lg_all", "gmax"], w=["ohg"])
                P.op("act", lambda E: E.activation(out=gd[:], in_=gd[:], func=AF.Exp), r=["gd"], w=["gd"])
                P.op("dve", lambda E: E.tensor_reduce(out=gsum[:], in_=gd[:], axis=AX.X, op=ALU.add), r=["gd"], w=["gsum"])
                P.op("dve", lambda E: E.reciprocal(out=pgrp[:], in_=gsum[:]), r=["gsum"], w=["pgrp"])
                P.op("dve", lambda E: E.tensor_scalar(out=ohg[:], in0=ohg[:], scalar1=-1.0, scalar2=1e30, op0=ALU.add, op1=ALU.mult),
                     r=["ohg"], w=["ohg"])
                P.op("dve", lambda E: E.tensor_tensor(out=em[:].rearrange("p j (g e) -> p j g e", g=4),
                                                      in0=el.rearrange("p j (g e) -> p j g e", g=4),
                                                      in1=ohg[:].unsqueeze(3).to_broadcast([128, NB, 4, 8]), op=ALU.add),
                     r=["lg_all", "ohg"], w=["em"])
                P.op("dve", lambda E: E.tensor_reduce(out=m1[:], in_=em[:], axis=AX.X, op=ALU.max), r=["em"], w=["m1"])
                P.op("dve", lambda E: E.tensor_tensor(out=oh1[:], in0=em[:], in1=m1[:].unsqueeze(2).to_broadcast([128, NB, NE]),
                                                      op=ALU.is_equal), r=["em", "m1"], w=["oh1"])
                P.op("dve", lambda E: E.scalar_tensor_tensor(out=em2[:], in0=oh1[:], scalar=-1e30, in1=em[:], op0=ALU.mult,
                                                             op1=ALU.add), r=["oh1", "em"], w=["em2"])
                P.op("dve", lambda E: E.tensor_reduce(out=m2[:], in_=em2[:], axis=AX.X, op=ALU.max), r=["em2"], w=["m2"])
                P.op("dve", lambda E: E.tensor_tensor(out=oh2[:], in0=em2[:], in1=m2[:].unsqueeze(2).to_broadcast([128, NB, NE]),
                                                      op=ALU.is_equal), r=["em2", "m2"], w=["oh2"])
                P.op("dve", lambda E: E.tensor_tensor(out=d12[:], in0=m1[:], in1=m2[:], op=ALU.subtract), r=["m1", "m2"], w=["d12"])
                P.op("act", lambda E: E.activation(out=d12[:], in_=d12[:], func=AF.Sigmoid), r=["d12"], w=["d12"])
                P.op("dve", lambda E: E.tensor_tensor(out=w1[:], in0=d12[:], in1=pgrp[:], op=ALU.mult), r=["d12", "pgrp"], w=["w1"])
                P.op("dve", lambda E: E.tensor_tensor(out=w2[:], in0=pgrp[:], in1=w1[:], op=ALU.subtract), r=["pgrp", "w1"], w=["w2"])
                P.op("dve", lambda E: E.tensor_tensor(out=Aall[:], in0=oh1[:], in1=oh2[:], op=ALU.add), r=["oh1", "oh2"], w=["Aall"])
                Af = Aall[:].rearrange("p b e -> p (b e)")
                for h0 in range(0, NC_, 512):
                    wd_ = min(512, NC_ - h0)
                    pbw, pbwn = fbank()
                    P.op("pe", lambda E, h0=h0, wd_=wd_, pbw=pbw: E.matmul(pbw[:, 0:wd_], lhsT=trib[:], rhs=Af[:, h0:h0 + wd_], start=True, stop=True),
                         r=["trib", "Aall"], w=[pbwn])
                    P.op("act", lambda E, h0=h0, wd_=wd_, pbw=pbw: E.activation(out=within[:].rearrange("p b e -> p (b e)")[:, h0:h0 + wd_],
                                                                           in_=pbw[:, 0:wd_], func=AF.Copy), w=[pbwn, "within"])
                    pbt, pbtn = fbank()
                    P.op("pe", lambda E, h0=h0, wd_=wd_, pbt=pbt: E.matmul(pbt[:, 0:wd_], lhsT=onesb[:], rhs=Af[:, h0:h0 + wd_], start=True, stop=True),
                         r=["onesb", "Aall"], w=[pbtn])
                    P.op("dve", lambda E, h0=h0, wd_=wd_, pbt=pbt: E.tensor_copy(out=tot0[:].rearrange("p b e -> p (b e)")[:, h0:h0 + wd_],
                                                                            in_=pbt[:, 0:wd_]), w=[pbtn, "tot0"])
                cur, curn = tot0, "tot0"
                step = 1
                bi = 0
                while step < NB:
                    dst, dn = tot[bi], f"tot{bi}"
                    P.op("dve", lambda E, dst=dst, cur=cur, step=step: E.tensor_copy(out=dst[:, 0:step, :], in_=cur[:, 0:step, :]), r=[curn], w=[dn])
                    P.op("dve", lambda E, dst=dst, cur=cur, step=step: E.tensor_tensor(out=dst[:, step:NB, :], in0=cur[:, step:NB, :],
                                                                                   in1=cur[:, 0:NB - step, :], op=ALU.add), r=[curn], w=[dn])
                    cur, curn = dst, dn
                    step *= 2
                    bi ^= 1
                incl, incln = cur, curn
                P.op("dve", lambda E: E.tensor_tensor(out=posall[:], in0=within[:], in1=incl[:], op=ALU.add), r=["within", incln], w=["posall"])
                P.op("dve", lambda E: E.tensor_tensor(out=posall[:], in0=posall[:], in1=tot0[:], op=ALU.subtract), r=["posall", "tot0"], w=["posall"])
                cnt = incl[:, NB - 1, :]
                P.op("dve", lambda E: E.tensor_scalar(out=thr[:], in0=iota_f[:, 0:NB2], scalar1=float(SL), scalar2=None, op0=ALU.mult),
                     r=["iota_f"], w=["thr"])
                P.op("dve", lambda E: E.tensor_tensor(out=cmpb[:], in0=cnt.unsqueeze(2).to_broadcast([128, NE, NB2]),
                                                      in1=thr[:].unsqueeze(1).to_broadcast([128, NE, NB2]), op=ALU.is_gt),
                     r=[incln, "thr"], w=["cmpb"])
                P.op("dve", lambda E: E.tensor_reduce(out=nt0[:], in_=cmpb[:], axis=AX.X, op=ALU.add), r=["cmpb"], w=["nt0"])
                cur, curn = nt0, "nt0"
                step = 1
                bi = 0
                while step < NE:
                    dst, dn = ntile[bi], f"ntile{bi}"
                    P.op("dve", lambda E, dst=dst, cur=cur, step=step: E.tensor_copy(out=dst[:, 0:step], in_=cur[:, 0:step]), r=[curn], w=[dn])
                    P.op("dve", lambda E, dst=dst, cur=cur, step=step: E.tensor_tensor(out=dst[:, step:NE], in0=cur[:, step:NE],
                                                                                   in1=cur[:, 0:NE - step], op=ALU.add), r=[curn], w=[dn])
                    cur, curn = dst, dn
                    step *= 2
                    bi ^= 1
                P.op("dve", lambda E, cur=cur: E.tensor_copy(out=tend[:], in_=cur[:]), r=[curn], w=["tend"])
                P.op("dve", lambda E: E.tensor_tensor(out=basee[:], in0=tend[:], in1=nt0[:], op=ALU.subtract), r=["tend", "nt0"], w=["basee"])
                P.op("dve", lambda E: E.tensor_scalar(out=basee[:], in0=basee[:], scalar1=float(SL), scalar2=None, op0=ALU.mult), r=["basee"], w=["basee"])
                P.op("dve", lambda E: E.tensor_tensor(out=posall[:], in0=posall[:], in1=basee[:].unsqueeze(1).to_broadcast([128, NB, NE]), op=ALU.add),
                     r=["posall", "basee"], w=["posall"])
                for (ohk, posk, nm) in ((oh1, pos1, "pos1"), (oh2, pos2, "pos2")):
                    P.op("dve", lambda E, ohk=ohk: E.tensor_tensor(out=ptmp[:], in0=ohk[:], in1=posall[:], op=ALU.mult),
                         r=["oh1", "oh2", "posall"], w=["ptmp"])
                    P.op("dve", lambda E: E.tensor_reduce(out=posf[:], in_=ptmp[:], axis=AX.X, op=ALU.add), r=["ptmp"], w=["posf"])
                    P.op("dve", lambda E, posk=posk: E.tensor_copy(out=posk[:], in_=posf[:]), r=["posf"], w=[nm])
                for j0 in range(0, TM, 128):
                    jn = min(128, TM - j0)
                    P.op("dve", lambda E, j0=j0, jn=jn: E.tensor_scalar(out=tef[:, j0:j0 + jn], in0=iota_f[:, 0:jn], scalar1=float(j0), scalar2=None,
                                                                       op0=ALU.add), r=["iota_f"], w=["tef"])
                P.op("dve", lambda E: E.tensor_tensor(out=cmpt[:], in0=tend[:].unsqueeze(1).to_broadcast([128, TM, NE]),
                                                      in1=tef[:].unsqueeze(2).to_broadcast([128, TM, NE]), op=ALU.is_le),
                     r=["tend", "tef"], w=["cmpt"])
                P.op("dve", lambda E: E.tensor_reduce(out=tef[:], in_=cmpt[:], axis=AX.X, op=ALU.add), r=["cmpt"], w=["tef"])
                P.op("dve", lambda E: E.tensor_scalar(out=tef[:], in0=tef[:], scalar1=128.0, scalar2=pidx_f[:, 0:1], op0=ALU.mult, op1=ALU.add),
                     r=["tef", "pidx_f"], w=["tef"])
                P.op("dve", lambda E: E.tensor_copy(out=te_i[:], in_=tef[:]), r=["tef"], w=["te_i"])

            if debug and l == 0:
                for nm, t_, rn in (("d_pos1", pos1, "pos1"), ("d_pos2", pos2, "pos2"), ("d_w1", w1, "w1"), ("d_w2", w2, "w2"), ("d_te", te_i, "te_i")):
                    P.dma("sp", lambda E, nm=nm, t_=t_: E.dma_start(out=dbg[nm].ap(), in_=t_[:]), r=[rn], w=["dbg_" + nm])
            with contextlib.ExitStack() as sc:
                P.barrier()
                def sbl(name, shape, dt=F32, sc=sc):
                    return sc.enter_context(nc.sbuf_tensor(f"{name}_{l}", list(shape), dt))
                hrow = [sbl(f"hrow{k}", [128, D], BF16) for k in range(3)]
                def sblock(b):
                    k = b % 3
                    P.dma("sp", lambda E, b=b, k=k: E.dma_start(out=hrow[k][:], in_=h2s[b * 128:(b + 1) * 128, :]), r=[f"h2s_{b // 4}"], w=[f"hrow{k}"])
                    for (posk, nm) in ((pos1, "pos1"), (pos2, "pos2")):
                        P.dma("pool", lambda E, b=b, k=k, posk=posk: E.indirect_dma_start(
                            out=hsort[:, :], out_offset=bass.IndirectOffsetOnAxis(ap=posk[:, b:b + 1], axis=0),
                            in_=hrow[k][:], in_offset=None), r=[f"hrow{k}", nm], w=[f"hsortw{b}"])
                for _b in range(NB):
                    sblock(_b)
            with contextlib.ExitStack() as sc:
                P.barrier()
                def sbl(name, shape, dt=F32, sc=sc):
                    return sc.enter_context(nc.sbuf_tensor(f"{name}_{l}", list(shape), dt))
                NW = 3
                wgb = [sbl(f"wgb{k}", [128, 8, HID], BF16) for k in range(NW)]
                wub = [sbl(f"wub{k}", [128, 8, HID], BF16) for k in range(NW)]
                wdb = [sbl(f"wdb{k}", [128, 2, D], BF16) for k in range(NW)]
                stt = [sbl(f"stt{k}", [128, 2, D], BF16) for k in range(2)]
                hsT = [sbl(f"hsT{k}", [128, 8, SL], BF16) for k in range(2)]
                sgl = [sbl(f"sgl{k}", [128, 2 * SL]) for k in range(2)]
                hid = [sbl(f"hid{k}", [128, 2, SL], BF16) for k in range(2)]
                yo = [sbl(f"yo{k}", [128, 2, D]) for k in range(2)]
                allsc = [f"hsortw{b}" for b in range(NB)]

                def etile(j):
                    k = j % NW
                    k2 = j % 2
                    for (wb, wsrc, nm) in ((wgb, wg_d, "wgb"), (wub, wu_d, "wub"), (wdb, wd_d, "wdb")):
                        P.dma("pool", lambda E, wb=wb, wsrc=wsrc: E.indirect_dma_start(
                            out=wb[k][:].rearrange("p c h -> p (c h)"), out_offset=None, in_=wsrc[l][:, :],
                            in_offset=bass.IndirectOffsetOnAxis(ap=te_i[:, j:j + 1], axis=0), bounds_check=bcd["v"], oob_is_err=False),
                            r=["te_i"], w=[f"{nm}{k}"])
                    P.dma("sp", lambda E: E.dma_start(out=stt[k2][:], in_=hsort[j * SL:(j + 1) * SL, :].rearrange("(q p) d -> p q d", p=128)),
                          r=allsc, w=[f"stt{k2}"])
                    for q in range(2):
                        pb, pbn = bbank()
                        for c in range(8):
                            P.op("pe", lambda E, c=c, q=q, pb=pb: E.transpose(out=pb[:, c * 128:(c + 1) * 128], in_=stt[k2][:, q, c * 128:(c + 1) * 128],
                                                                           identity=identb[:]), r=[f"stt{k2}", "identb"], w=[pbn])
                        if q == 0:
                            P.op("act", lambda E, q=q, pb=pb: E.activation(out=hsT[k2][:, :, q * 128:(q + 1) * 128],
                                                                         in_=pb[:].rearrange("p (c t) -> p c t", c=8), func=AF.Copy), w=[pbn, f"hsT{k2}_{q}"])
                        else:
                            P.op("dve", lambda E, q=q, pb=pb: E.tensor_copy(out=hsT[k2][:, :, q * 128:(q + 1) * 128],
                                                                          in_=pb[:].rearrange("p (c t) -> p c t", c=8)), w=[pbn, f"hsT{k2}_{q}"])
                    hsr = [f"hsT{k2}_0", f"hsT{k2}_1"]
                    pg, pgn = fbank()
                    pu, pun = fbank()
                    for (W, Wn, pz, pzn) in ((wgb[k], f"wgb{k}", pg, pgn), (wub[k], f"wub{k}", pu, pun)):
                        for hc in range(2):
                            for c in range(8):
                                P.op("pe", lambda E, c=c, W=W, hc=hc, pz=pz: E.matmul(
                                    pz[:, hc * SL:(hc + 1) * SL], lhsT=W[:, c, hc * 128:(hc + 1) * 128], rhs=hsT[k2][:, c, :],
                                    start=(c == 0), stop=(c == 7)), r=[Wn] + hsr, w=[pzn])
                    P.op("act", lambda E: E.activation(out=sgl[k2][:], in_=pg[:, 0:2 * SL], func=AF.Silu), w=[pgn, f"sgl{k2}"])
                    P.op("dve", lambda E: E.tensor_tensor(out=hid[k2][:].rearrange("p c t -> p (c t)"), in0=pu[:, 0:2 * SL],
                                                          in1=sgl[k2][:], op=ALU.mult), r=[f"sgl{k2}"], w=[pun, f"hid{k2}"])
                    for q in range(2):
                        for dh in range(2):
                            pd, pdn = fbank()
                            for kc in range(2):
                                P.op("pe", lambda E, q=q, dh=dh, kc=kc, pd=pd: E.matmul(pd[:], lhsT=hid[k2][:, kc, q * 128:(q + 1) * 128],
                                                                                     rhs=wdb[k][:, kc, dh * 512:(dh + 1) * 512],
                                                                                     start=(kc == 0), stop=(kc == 1)),
                                     r=[f"hid{k2}", f"wdb{k}"], w=[pdn])
                            if dh == 0:
                                P.op("act", lambda E, q=q, pd=pd: E.activation(out=yo[k2][:, q, 0:512], in_=pd[:], func=AF.Copy), w=[pdn, f"yo{k2}_{q}a"])
                            else:
                                P.op("dve", lambda E, q=q, pd=pd: E.tensor_copy(out=yo[k2][:, q, 512:1024], in_=pd[:]), w=[pdn, f"yo{k2}_{q}b"])
                    P.dma("act", lambda E: E.dma_start(out=ysort[j * SL:(j + 1) * SL, :].rearrange("(q p) d -> p q d", p=128), in_=yo[k2][:]),
                          r=[f"yo{k2}_{q}{h}" for q in range(2) for h in "ab"], w=[f"ysortw{j}"])
                for _j in range(TM):
                    etile(_j)

            with contextlib.ExitStack() as sc:
                P.barrier()
                def sbl(name, shape, dt=F32, sc=sc):
                    return sc.enter_context(nc.sbuf_tensor(f"{name}_{l}", list(shape), dt))
                r1 = [sbl(f"r1{k}", [128, D]) for k in range(2)]
                r2 = [sbl(f"r2{k}", [128, D]) for k in range(2)]
                xc = [sbl(f"xc{k}", [128, D]) for k in range(2)]
                fgb = sbl("fgb", [128, D])
                ssf = sbl("ssf", [128, NB])
                rrf = sbl("rrf", [128, NB])
                junk2 = sbl("junk2", [128, D], BF16)
                ally = [f"ysortw{j}" for j in range(TM)]
                if last:
                    P.dma("sp", lambda E: E.dma_start(out=fgb[:], in_=final_g.ap().partition_broadcast(128)), w=["fgb"])
                    P.op("act", lambda E: E.mul(out=fgb[:], in_=fgb[:], mul=32.0), r=["fgb"], w=["fgb"])
                    P.op("dve", lambda E: E.memset(ssf[:], 0.0), w=["ssf"])
                def cblock(b):
                    k = b % 2
                    rows = slice(b * 128, (b + 1) * 128)
                    P.dma("pool", lambda E, b=b, k=k: E.indirect_dma_start(out=r1[k][:], out_offset=None, in_=ysort[:, :],
                                                                          in_offset=bass.IndirectOffsetOnAxis(ap=pos1[:, b:b + 1], axis=0)),
                          r=ally + ["pos1"], w=[f"r1{k}"])
                    P.dma("pool", lambda E, b=b, k=k: E.indirect_dma_start(out=r2[k][:], out_offset=None, in_=ysort[:, :],
                                                                          in_offset=bass.IndirectOffsetOnAxis(ap=pos2[:, b:b + 1], axis=0)),
                          r=ally + ["pos2"], w=[f"r2{k}"])
                    P.dma("sp", lambda E, k=k, rows=rows: E.dma_start(out=xc[k][:], in_=xs1[rows, :]), r=[f"xs1_{b // 4}"], w=[f"xc{k}"])
                    P.op("act", lambda E, b=b, k=k: E.activation(out=r1[k][:], in_=r1[k][:], func=AF.Copy, scale=w1[:, b:b + 1]),
                         r=[f"r1{k}", "w1"], w=[f"r1{k}"])
                    P.op("dve", lambda E, b=b, k=k: E.scalar_tensor_tensor(out=r2[k][:], in0=r2[k][:], scalar=w2[:, b:b + 1], in1=r1[k][:],
                                                                          op0=ALU.mult, op1=ALU.add), r=[f"r2{k}", f"r1{k}", "w2"], w=[f"r2{k}"])
                    P.op("pool", lambda E, k=k: E.tensor_tensor(out=r2[k][:], in0=r2[k][:], in1=G2, op=ALU.mult), r=[f"r2{k}", "mod"], w=[f"r2{k}"])
                    P.op("dve", lambda E, k=k: E.tensor_tensor(out=xc[k][:], in0=xc[k][:], in1=r2[k][:], op=ALU.add), r=[f"xc{k}", f"r2{k}"],
                         w=[f"xc{k}"])
                    if not last:
                        P.dma("act", lambda E, k=k, rows=rows: E.dma_start(out=xs2[rows, :], in_=xc[k][:]), r=[f"xc{k}"], w=[f"xs2_{b}"])
                    else:
                        P.op("act", lambda E, b=b, k=k: E.activation(out=junk2[:], in_=xc[k][:], func=AF.Square, accum_out=ssf[:, b:b + 1]),
                             r=[f"xc{k}"], w=["junk2", "ssf"])
                        P.op("act", lambda E, b=b: E.activation(out=rrf[:, b:b + 1], in_=ssf[:, b:b + 1], func=AF.Sqrt, bias=epsr[:, 0:1]),
                             r=["ssf", "epsr"], w=["rrf"])
                        P.op("dve", lambda E, b=b: E.reciprocal(out=rrf[:, b:b + 1], in_=rrf[:, b:b + 1]), r=["rrf"], w=["rrf"])
                        P.op("dve", lambda E, b=b, k=k: E.scalar_tensor_tensor(out=xc[k][:], in0=xc[k][:], scalar=rrf[:, b:b + 1], in1=fgb[:],
                                                                              op0=ALU.mult, op1=ALU.mult), r=[f"xc{k}", "rrf", "fgb"], w=[f"xc{k}"])
                        P.dma("act", lambda E, k=k, rows=rows: E.dma_start(out=out_d[rows, :], in_=xc[k][:]), r=[f"xc{k}"], w=[f"out_{b}"])
                for _b in range(NB):
                    cblock(_b)
        for _l in range(DEPTH_):
            layer(_l)
        P.op("sp", lambda E: E.nop(), r=[f"out_{b}" for b in range(NB)] + [k for k in ("dbg1", "dbg2", "dbg2b", "dbg3", "dbg4", "dbg_d_pos1",
             "dbg_d_pos2", "dbg_d_w1", "dbg_d_w2", "dbg_d_te")], w=["done"])
        P.emit(sems)
    return nc


_CACHE = {}


def _prep(inputs, S):
    f = lambda a: np.ascontiguousarray(np.asarray(a, dtype=np.float32))
    pvec = np.concatenate([f(inputs["conv_w"]), f(inputs["conv_b"])[:, None, :], f(inputs["conv_ln_g"])[:, None, :],
                           f(inputs["conv_ln_b"])[:, None, :], f(inputs["pool_scale"])[:, None, :]], axis=1)
    rw = np.concatenate([f(inputs["router_group_w"]), f(inputs["router_expert_w"])], axis=2)
    rb = np.concatenate([f(inputs["router_group_b"]), f(inputs["router_expert_b"])], axis=1)
    shared = dict(ada_w=f(inputs["ada_w"]), ada_b=f(inputs["ada_b"]), norm1_g=f(inputs["norm1_g"]), w_in=f(inputs["w_in"]),
                  pool_w=f(inputs["pool_w"]), pvec=np.ascontiguousarray(pvec), w_out=f(inputs["w_out"]), norm2_g=f(inputs["norm2_g"]),
                  rw=np.ascontiguousarray(rw), rb=np.ascontiguousarray(rb), final_g=f(inputs["final_g"]))
    for nm, key, nch in (("wg", "expert_w_gate", 8), ("wu", "expert_w_up", 8), ("wd", "expert_w_down", 2)):
        w = f(inputs[key])
        L, E_, K, F_ = w.shape
        wr_ = w.reshape(L, E_, nch, 128, F_).transpose(0, 1, 3, 2, 4).reshape(L, E_ * 128, nch * F_)
        for l in range(L):
            shared[f"{nm}{l}"] = np.ascontiguousarray(wr_[l])
    x = f(inputs["x"])
    c = f(inputs["c"])
    in_maps = []
    for b in range(x.shape[0]):
        m = dict(shared)
        m["x"] = np.ascontiguousarray(x[b])
        m["c"] = np.ascontiguousarray(c[b])
        in_maps.append(m)
    return in_maps


def kernel(**inputs):
    x = np.asarray(inputs["x"])
    B, S, _ = x.shape
    assert B == N_CORES
    if S not in _CACHE:
        _CACHE[S] = build(S)
    nc = _CACHE[S]
    in_maps = _prep(inputs, S)
    res = run_bass_kernel_spmd(nc, in_maps, core_ids=list(range(B)))
    return np.stack([np.asarray(r["out"], dtype=np.float32) for r in res.results], axis=0)
```

```python
import numpy as np
import concourse.bass as bass
import concourse.mybir as mybir
from concourse.bass_utils import run_bass_kernel_spmd

F32 = mybir.dt.float32
BF16 = mybir.dt.bfloat16
I32 = mybir.dt.int32
AF = mybir.ActivationFunctionType
ALU = mybir.AluOpType
AX = mybir.AxisListType

D = 1024
DEPTH = 2
NE = 32
HID = 256
RMS_EPS = 1e-6
LN_EPS = 1e-5
POOL_WINDOWS = (2, 4, 8, 16)
CK = 31
N_CORES = 8


class Prog:
    ENGS = ("pe", "act", "dve", "pool", "sp")

    def __init__(self, nc, n_dma_sems=6):
        self.nc = nc
        self.ops = []
        self.n_dma_sems = n_dma_sems
        self.tens = {}

    def reg(self, short, handle):
        m = self.nc.lookup_mloc(handle)
        self.tens[short] = (m.name, int(m.addr), int(m.addr) + int(m.dims[1]))

    def _resolve(self, name):
        n = name
        while True:
            if n in self.tens:
                return (name,) + self.tens[n]
            n2 = n.rstrip("0123456789")
            if n2.endswith("_"):
                n2 = n2[:-1]
            if n2 == n or not n2:
                self.unres = getattr(self, "unres", set())
                self.unres.add(name.rstrip("0123456789"))
                return (name, None, 0, 0)
            n = n2

    def op(self, eng, fn, r=(), w=()):
        self.ops.append(dict(eng=eng, fn=fn, r=tuple(self._resolve(x) for x in r), w=tuple(self._resolve(x) for x in w), dma=False))

    def dma(self, q, fn, r=(), w=()):
        self.ops.append(dict(eng=q, fn=fn, r=tuple(self._resolve(x) for x in r), w=tuple(self._resolve(x) for x in w), dma=True))

    @staticmethod
    def _key(nm, par, recs, lo, hi, bins):
        key = (nm, par)
        if key not in recs:
            recs[key] = dict(par=par, lo=lo, hi=hi, w=None, r=[])
            if par is not None:
                for b_ in range(lo // 2048, (hi - 1) // 2048 + 1):
                    bins.setdefault(b_, []).append(key)
        return key

    def barrier(self):
        self.ops.append(dict(eng=None, fn=None, r=(), w=(), dma=False, barrier=True))

    def emit(self, sems):
        nc = self.nc
        engobj = dict(pe=nc.tensor, act=nc.scalar, dve=nc.vector, pool=nc.gpsimd, sp=nc.sync)
        ops = self.ops
        n = len(ops)
        recs = {}
        bins = {}
        clock = {e: dict(pe=-1, act=-1, dve=-1, pool=-1, sp=-1, dma=set()) for e in self.ENGS}
        opclock = [None] * n
        dma_hist = {e: [] for e in self.ENGS}
        waits = [None] * n
        sig = [False] * n
        bar_deps = set()
        last_on = {}
        for i, o in enumerate(ops):
            e = o["eng"]
            if e is None:
                bar_deps = set(last_on.values())
                for q in self.ENGS:
                    bar_deps |= set(dma_hist[q][-self.n_dma_sems:])
                waits[i] = []
                opclock[i] = None
                continue
            deps = set(bar_deps)
            for (nm, par, lo, hi) in o["r"]:
                key = self._key(nm, par, recs, lo, hi, bins)
                lw = recs[key]["w"]
                if lw is not None:
                    deps.add(lw)
            for (nm, par, lo, hi) in o["w"]:
                key = self._key(nm, par, recs, lo, hi, bins)
                rc = recs[key]
                if rc["w"] is not None:
                    deps.add(rc["w"])
                deps.update(rc["r"])
                if par is not None:
                    seen = set()
                    for b_ in range(lo // 2048, (hi - 1) // 2048 + 1):
                        for k2_ in bins.get(b_, ()):
                            if k2_ in seen or k2_ == key:
                                continue
                            seen.add(k2_)
                            r2 = recs[k2_]
                            if r2["par"] != par and r2["lo"] < hi and lo < r2["hi"]:
                                if r2["w"] is not None:
                                    deps.add(r2["w"])
                                deps.update(r2["r"])
            if o["dma"]:
                h = dma_hist[e]
                if len(h) >= self.n_dma_sems:
                    deps.add(h[-self.n_dma_sems])
                h.append(i)
            deps.discard(i)
            need = []
            ck = clock[e]
            for j in sorted(deps):
                pj = ops[j]
                if pj["dma"]:
                    if j in ck["dma"]:
                        continue
                else:
                    if pj["eng"] == e and e == "pe" and not o["dma"]:
                        continue
                    if ck[pj["eng"]] >= j:
                        continue
                need.append(j)
                sig[j] = True
                oc = opclock[j]
                for k in ("pe", "act", "dve", "pool", "sp"):
                    if oc[k] > ck[k]:
                        ck[k] = oc[k]
                ck["dma"] |= oc["dma"]
            waits[i] = need
            oc = dict(pe=ck["pe"], act=ck["act"], dve=ck["dve"], pool=ck["pool"], sp=ck["sp"], dma=set(ck["dma"]))
            if o["dma"]:
                oc["dma"].add(i)
            else:
                oc[e] = i
            opclock[i] = oc
            if not o["dma"]:
                last_on[e] = i
            for (nm, par, lo, hi) in o["r"]:
                recs[self._key(nm, par, recs, lo, hi, bins)]["r"].append(i)
            for (nm, par, lo, hi) in o["w"]:
                rc = recs[self._key(nm, par, recs, lo, hi, bins)]
                rc["w"] = i
                rc["r"] = []
        cnt = {e: 0 for e in self.ENGS}
        dcnt = {}
        dma_n = {e: 0 for e in self.ENGS}
        ev = [None] * n
        for i, o in enumerate(ops):
            e = o["eng"]
            if e is None:
                continue
            E = engobj[e]
            for j in waits[i]:
                s, v = ev[j]
                E.wait_ge(s, v)
            inst = o["fn"](E)
            if o["dma"]:
                k = dma_n[e] % self.n_dma_sems
                dma_n[e] += 1
                s = sems["dma_" + e][k]
                dcnt[(e, k)] = dcnt.get((e, k), 0) + 16
                inst.then_inc(s, 16)
                ev[i] = (s, dcnt[(e, k)])
            elif sig[i]:
                cnt[e] += 1
                inst.then_inc(sems[e], 1)
                ev[i] = (sems[e], cnt[e])
        return ev


def build(S, depth=DEPTH, debug=False):
    DEPTH_ = depth
    NB = S // 128
    NT = S // 512
    SL = 256
    NB2 = (S + SL - 1) // SL
    TM = 2 * S // SL + NE
    nc = bass.Bass("TRN2", target_bir_lowering=False)
    P = Prog(nc)

    def dram_in(name, shape, dt=F32):
        return nc.dram_tensor(name, list(shape), dt, kind="ExternalInput")

    x_d = dram_in("x", [S, D])
    c_d = dram_in("c", [D])
    ada_w = dram_in("ada_w", [DEPTH, D, 6 * D])
    ada_b = dram_in("ada_b", [DEPTH, 6 * D])
    norm1_g = dram_in("norm1_g", [DEPTH, D])
    w_in = dram_in("w_in", [DEPTH, D, 1536])
    pool_w = dram_in("pool_w", [DEPTH, 4, 128, 128])
    pvec = dram_in("pvec", [DEPTH, 35, 512])
    w_out = dram_in("w_out", [DEPTH, D, D])
    norm2_g = dram_in("norm2_g", [DEPTH, D])
    rw = dram_in("rw", [DEPTH, D, 36])
    rb = dram_in("rb", [DEPTH, 36])
    wg_d = [dram_in(f"wg{l}", [NE * 128, 8 * HID]) for l in range(DEPTH)]
    wu_d = [dram_in(f"wu{l}", [NE * 128, 8 * HID]) for l in range(DEPTH)]
    wd_d = [dram_in(f"wd{l}", [NE * 128, 2 * D]) for l in range(DEPTH)]
    final_g = dram_in("final_g", [D])
    out_d = nc.dram_tensor("out", [S, D], F32, kind="ExternalOutput")
    sk = "ExternalOutput" if debug else "Internal"
    xs1 = nc.dram_tensor("xs1", [S, D], F32, kind=sk)
    xs2 = nc.dram_tensor("xs2", [S, D], F32, kind=sk)
    h2s = nc.dram_tensor("h2s", [S, D], BF16, kind=sk)
    hsort = nc.dram_tensor("hsort", [TM * SL, D], BF16, kind=sk)
    ysort = nc.dram_tensor("ysort", [TM * SL, D], F32, kind=sk)
    dbg = {}
    if debug:
        for nm, shp, dt in (("d_mod", [128, 6 * D], F32), ("d_hT", [128, 8 * 512], BF16), ("d_ycat", [128, 8 * 512], BF16),
                            ("d_lg", [128, 4 * 36], F32), ("d_pos1", [128, NB], I32), ("d_pos2", [128, NB], I32),
                            ("d_w1", [128, NB], F32), ("d_w2", [128, NB], F32), ("d_te", [128, TM], I32), ("d_pvT", [128, 4 * 35], F32)):
            dbg[nm] = nc.dram_tensor(nm, shp, dt, kind="ExternalOutput")

    import contextlib
    es = contextlib.ExitStack()
    with es:
        def sb(name, shape, dt=F32):
            t_ = es.enter_context(nc.sbuf_tensor(name, list(shape), dt))
            P.reg(name, t_)
            return t_

        def psum(name, shape, dt=F32):
            return es.enter_context(nc.psum_tensor(name, list(shape), dt))

        sems = {}
        for e in Prog.ENGS:
            sems[e] = es.enter_context(nc.semaphore("s_" + e))
            sems["dma_" + e] = [es.enter_context(nc.semaphore(f"d_{e}{k}")) for k in range(P.n_dma_sems)]

        identf = sb("identf", [128, 128])
        identb = sb("identb", [128, 128], BF16)
        onesf = sb("onesf", [128, 128])
        onesb = sb("onesb", [128, 128], BF16)
        trib = sb("trib", [128, 128], BF16)
        trif = sb("trif", [128, 128])
        bavg = sb("bavg", [128, 128], BF16)
        bavgf = sb("bavgf", [128, 128])
        inv16 = sb("inv16", [128, 16])
        iota_i = sb("iota_i", [128, 128], I32)
        iota_f = sb("iota_f", [128, 128])

        P.op("pool", lambda E: E.memset(identf[:], 0.0), w=["identf"])
        P.op("pool", lambda E: E.affine_select(out=identf[:], in_=identf[:], pattern=[[-1, 128]], compare_op=ALU.not_equal,
                                               fill=1.0, base=0, channel_multiplier=1), r=["identf"], w=["identf"])
        P.op("pool", lambda E: E.tensor_copy(out=identb[:], in_=identf[:]), r=["identf"], w=["identb"])
        P.op("pool", lambda E: E.memset(onesf[:], 1.0), w=["onesf"])
        P.op("pool", lambda E: E.memset(onesb[:], 1.0), w=["onesb"])
        P.op("pool", lambda E: E.memset(trif[:], 1.0), w=["trif"])
        P.op("pool", lambda E: E.affine_select(out=trif[:], in_=trif[:], pattern=[[1, 128]], compare_op=ALU.is_gt,
                                               fill=0.0, base=0, channel_multiplier=-1), r=["trif"], w=["trif"])
        P.op("pool", lambda E: E.tensor_copy(out=trib[:], in_=trif[:]), r=["trif"], w=["trib"])
        P.op("pool", lambda E: E.memset(bavgf[:], 0.0), w=["bavgf"])
        P.op("pool", lambda E: E.memset(bavgf[0:64, 0:64], 1.0 / 64), r=["bavgf"], w=["bavgf"])
        P.op("pool", lambda E: E.memset(bavgf[64:128, 64:128], 1.0 / 64), r=["bavgf"], w=["bavgf"])
        P.op("pool", lambda E: E.tensor_copy(out=bavg[:], in_=bavgf[:]), r=["bavgf"], w=["bavg"])
        P.op("pool", lambda E: E.iota(iota_i[:], pattern=[[1, 128]], base=0, channel_multiplier=0), w=["iota_i"])
        P.op("pool", lambda E: E.tensor_copy(out=iota_f[:], in_=iota_i[:]), r=["iota_i"], w=["iota_f"])
        epsr = sb("epsr", [128, 1])
        epsl = sb("epsl", [128, 1])
        P.op("pool", lambda E: E.memset(epsr[:], D * RMS_EPS), w=["epsr"])
        P.op("pool", lambda E: E.memset(epsl[:], LN_EPS), w=["epsl"])
        pidx_i = sb("pidx_i", [128, 1], I32)
        pidx_f = sb("pidx_f", [128, 1])
        P.op("pool", lambda E: E.iota(pidx_i[:], pattern=[[0, 1]], base=0, channel_multiplier=1), w=["pidx_i"])
        P.op("pool", lambda E: E.tensor_copy(out=pidx_f[:], in_=pidx_i[:]), r=["pidx_i"], w=["pidx_f"])
        P.op("dve", lambda E: E.tensor_scalar(out=inv16[:], in0=iota_f[:, 0:16], scalar1=1.0, scalar2=None, op0=ALU.add),
             r=["iota_f"], w=["inv16"])
        P.op("dve", lambda E: E.reciprocal(out=inv16[:], in_=inv16[:]), r=["inv16"], w=["inv16"])

        bcd = {}

        def mk_bc(E):
            reg = E.alloc_register("bcreg")
            inst = E.reg_mov(reg, NE * 128 - 1)
            bcd["v"] = E.snap(reg, donate=True)
            return E.memset(epsl[:], LN_EPS)
        P.op("pool", mk_bc, w=["epsl"])
        NFB = 5
        psf = [psum(f"psf{i}", [128, 512]) for i in range(NFB)]
        psb = [psum(f"psb{i}", [128, 1024], BF16) for i in range(2)]
        psr = psum("psr", [128, 512])
        bank_ctr = [0, 0]

        def fbank():
            i = bank_ctr[0] % NFB
            bank_ctr[0] += 1
            return psf[i], f"psf{i}"

        def bbank():
            i = bank_ctr[1] % 2
            bank_ctr[1] += 1
            return psb[i], f"psb{i}"

        c_sb = sb("c_sb", [128, 8])
        cond = sb("cond", [128, 8])
        condbc = sb("condbc", [128, 8, 128])
        P.dma("sp", lambda E: E.dma_start(out=c_sb[:], in_=c_d.ap().rearrange("(c p) -> p c", p=128), allow_slow_non_contiguous=True),
              w=["c_sb"])
        P.op("act", lambda E: E.activation(out=cond[:], in_=c_sb[:], func=AF.Silu), r=["c_sb"], w=["cond"])
        for c in range(8):
            P.op("act", lambda E, c=c: E.activation(out=condbc[:, c, :], in_=onesf[:], func=AF.Copy, scale=cond[:, c:c + 1]),
                 r=["cond", "onesf"], w=["condbc"])

        mod = sb("mod", [128, 6 * D])
        oh1 = sb("oh1", [128, NB, NE], BF16)
        oh2 = sb("oh2", [128, NB, NE], BF16)
        Aall = sb("Aall", [128, NB, NE], BF16)
        w1 = sb("w1", [128, NB])
        w2 = sb("w2", [128, NB])
        pos1 = sb("pos1", [128, NB], I32)
        pos2 = sb("pos2", [128, NB], I32)
        te_i = sb("te_i", [128, TM], I32)
        lg_all = sb("lg_all", [128, NB, 36])

        B1, A1, G1, B2, A2, G2 = (mod[:, k * D:(k + 1) * D] for k in range(6))

        win = sb("win", [128, 8, 1536], BF16)
        wout = sb("wout", [128, 8, D], BF16)
        pw = sb("pw", [128, 4, 128], BF16)
        wr = sb("wr", [128, 8, 36])
        rbias = sb("rbias", [128, 4, 36])
        pv = sb("pv", [35, 512])
        pvT = sb("pvT", [128, 4, 35])
        diag = sb("diag", [128, CK, 4, 128], BF16)

        def prologue(l, n0, n1, do_scale, aw, ab, gbc):
            for nn in range(n0, n1):
                k = nn % len(aw)
                blk = (nn * 256) // 512
                P.dma("sp", lambda E, nn=nn, k=k: E.dma_start(
                    out=aw[k][:], in_=ada_w[l, :, nn * 256:(nn + 1) * 256].rearrange("(c p) n -> p c n", p=128)), w=[f"aw{k}"])
                P.dma("sp", lambda E, nn=nn, k=k: E.dma_start(
                    out=ab[k][:], in_=ada_b[l, nn * 256:(nn + 1) * 256].partition_broadcast(128)), w=[f"ab{k}"])
                pb, pbn = fbank()
                for c in range(8):
                    P.op("pe", lambda E, c=c, pb=pb, k=k: E.matmul(pb[:, 0:256], lhsT=condbc[:, c, :], rhs=aw[k][:, c, :],
                                                                  start=(c == 0), stop=(c == 7)), r=["condbc", f"aw{k}"], w=[pbn])
                P.op("dve", lambda E, nn=nn, pb=pb, k=k: E.tensor_tensor(out=mod[:, nn * 256:(nn + 1) * 256], in0=pb[:, 0:256], in1=ab[k][:],
                                                                         op=ALU.add), r=[f"ab{k}"], w=[pbn, f"mod{blk}"])
            if do_scale:
                for (gsrc, Aap, mr) in ((norm1_g, A1, ["mod2", "mod3"]), (norm2_g, A2, ["mod8", "mod9"])):
                    P.dma("sp", lambda E, gsrc=gsrc: E.dma_start(out=gbc[:], in_=gsrc[l, :].partition_broadcast(128)), w=["gbc"])
                    P.op("act", lambda E: E.mul(out=gbc[:], in_=gbc[:], mul=32.0), r=["gbc"], w=["gbc"])
                    P.op("dve", lambda E, Aap=Aap: E.scalar_tensor_tensor(out=Aap, in0=Aap, scalar=1.0, in1=gbc[:], op0=ALU.add,
                                                                          op1=ALU.mult), r=["gbc"] + mr, w=mr)

        def load_weights(l):
            for c in range(8):
                P.dma("pool", lambda E, c=c: E.dma_start(out=win[:, c, :], in_=w_in[l, c * 128:(c + 1) * 128, :]), w=["win"])
            for c in range(8):
                P.dma("pool", lambda E, c=c: E.dma_start(out=wout[:, c, :], in_=w_out[l, c * 128:(c + 1) * 128, :]), w=["wout"])
            P.dma("pool", lambda E: E.dma_start(out=pw[:], in_=pool_w[l].rearrange("g c d -> c g d")), w=["pw"])
            P.dma("sp", lambda E: E.dma_start(out=wr[:], in_=rw[l].rearrange("(c p) g -> p c g", p=128)), w=["wr"])
            for j in range(4):
                P.dma("sp", lambda E, j=j: E.dma_start(out=rbias[:, j, :], in_=rb[l, :].partition_broadcast(128)), w=["rbias"])
            P.dma("sp", lambda E: E.dma_start(out=pv[:], in_=pvec[l]), w=["pv"])
            for c in range(4):
                pb, pbn = fbank()
                P.op("pe", lambda E, c=c, pb=pb: E.transpose(out=pb[:, 0:35], in_=pv[:, c * 128:(c + 1) * 128], identity=identf[0:35, 0:35]),
                     r=["pv", "identf"], w=[pbn])
                P.op("act", lambda E, c=c, pb=pb: E.activation(out=pvT[:, c, :], in_=pb[:, 0:35], func=AF.Copy), w=[pbn, "pvT"])
            for k in range(CK):
                for c in range(4):
                    eng = "dve" if (k * 4 + c) % 2 == 0 else "pool"
                    P.op(eng, lambda E, k=k, c=c: E.tensor_scalar(out=diag[:, k, c, :], in0=identf[:], scalar1=pvT[:, c, k:k + 1],
                                                                  scalar2=None, op0=ALU.mult),
                         r=["identf", "pvT"], w=[f"diag{k}_{c}"])

        def pro_bufs(sc, l, nbuf=2):
            def sbl(name, shape, dt=F32):
                t_ = sc.enter_context(nc.sbuf_tensor(f"{name}_{l}", list(shape), dt))
                P.reg(name, t_)
                return t_
            aw = [sbl(f"aw{k}", [128, 8, 256]) for k in range(nbuf)]
            ab = [sbl(f"ab{k}", [128, 256]) for k in range(nbuf)]
            gbc = sbl("gbc", [128, D])
            return aw, ab, gbc

        with contextlib.ExitStack() as sc0:
            aw_, ab_, gbc_ = pro_bufs(sc0, "p0")
            prologue(0, 0, 24, True, aw_, ab_, gbc_)
        load_weights(0)

        def layer(l):
            x_src = x_d if l == 0 else xs2
            last = (l == DEPTH_ - 1)
            with contextlib.ExitStack() as sc:
                def sbl(name, shape, dt=F32, sc=sc):
                    t_ = sc.enter_context(nc.sbuf_tensor(f"{name}_{l}", list(shape), dt))
                    P.reg(name, t_)
                    return t_
                xt = [sbl(f"xt{k}", [128, 4, D]) for k in range(1)]
                ss = sbl("ss", [128, 8])
                rr = sbl("rr", [128, 8])
                tmpA = [sbl(f"tmpA{k}", [128, D]) for k in range(2)]
                hb = sbl("hb", [128, 4, D], BF16)
                hT = sbl("hT", [128, 8, 512], BF16)
                u = sbl("u", [128, 4, 528])
                sA = sbl("sA", [128, 528])
                sB = sbl("sB", [128, 528])
                pooled = sbl("pooled", [128, 4, 512], BF16)
                ycat = sbl("ycat", [128, 8, 512], BF16)
                vbuf = sbl("vbuf", [128, 4, 544], BF16)
                sig = [sbl(f"sig{k}", [128, 512]) for k in range(1)]
                ybf = [sbl(f"ybf{k}", [128, 512], BF16) for k in range(2)]
                dd = [sbl(f"dd{k}", [128, 512]) for k in range(1)]
                sq = [sbl(f"sq{k}", [128, 512], BF16) for k in range(1)]
                rs = [sbl(f"rs{k}", [128, 512]) for k in range(1)]
                yn = dd
                tmpO = [sbl(f"tmpO{k}", [128, 512]) for k in range(2)]
                h2b = hb
                h2T = [sbl(f"h2T{k}", [128, 8, 128]) for k in range(1)]

                P.op("pool", lambda E: E.memset(u[:], 0.0), w=["u0", "u1", "u2", "u3"])
                P.op("pool", lambda E: E.memset(vbuf[:], 0.0), w=["vbuf0", "vbuf1", "vbuf2", "vbuf3"])

                def mtile(i):
                    X = xt[0]
                    Xn = "xt0"
                    rows = slice(i * 512, (i + 1) * 512)
                    P.dma("sp", lambda E, X=X, rows=rows: E.dma_start(out=X[:], in_=x_src[rows, :].rearrange("(j p) d -> p j d", p=128)),
                          r=[f"xs2_{4 * i + q}" for q in range(4)], w=[Xn])
                    P.op("dve", lambda E: E.memset(ss[:], 0.0), w=["ss"])
                    for j in range(4):
                        P.op("act", lambda E, j=j, X=X: E.activation(out=hb[:, j, :], in_=X[:, j, :], func=AF.Square, accum_out=ss[:, j:j + 1]),
                             r=[Xn], w=[f"hb{j}", "ss"])
                    P.op("act", lambda E: E.activation(out=rr[:, 0:4], in_=ss[:, 0:4], func=AF.Sqrt, bias=epsr[:, 0:1]), r=["ss", "epsr"], w=["rr"])
                    P.op("dve", lambda E: E.reciprocal(out=rr[:, 0:4], in_=rr[:, 0:4]), r=["rr"], w=["rr"])
                    for j in range(4):
                        T = tmpA[j % 2]
                        Tn = f"tmpA{j % 2}"
                        P.op("dve", lambda E, j=j, X=X, T=T: E.scalar_tensor_tensor(out=T[:], in0=X[:, j, :], scalar=rr[:, j:j + 1], in1=A1,
                                                                                    op0=ALU.mult, op1=ALU.mult),
                             r=[Xn, "rr", "mod2", "mod3"], w=[Tn])
                        P.op("pool", lambda E, j=j, T=T: E.tensor_tensor(out=hb[:, j, :], in0=T[:], in1=B1, op=ALU.add),
                             r=[Tn, "mod0", "mod1"], w=[f"hb{j}"])
                    for j in range(4):
                        pb, pbn = bbank()
                        for c in range(8):
                            P.op("pe", lambda E, j=j, c=c, pb=pb: E.transpose(out=pb[:, c * 128:(c + 1) * 128], in_=hb[:, j, c * 128:(c + 1) * 128],
                                                                             identity=identb[:]),
                                 r=[f"hb{j}", "identb"], w=[pbn])
                        eng = "act" if j % 2 == 0 else "dve"
                        if eng == "act":
                            P.op("act", lambda E, j=j, pb=pb: E.activation(out=hT[:, :, j * 128:(j + 1) * 128],
                                                                           in_=pb[:].rearrange("p (c t) -> p c t", c=8), func=AF.Copy),
                                 w=[pbn, f"hT{j}"])
                        else:
                            P.op("dve", lambda E, j=j, pb=pb: E.tensor_copy(out=hT[:, :, j * 128:(j + 1) * 128],
                                                                            in_=pb[:].rearrange("p (c t) -> p c t", c=8)),
                                 w=[pbn, f"hT{j}"])
                    hTr = ["hT0", "hT1", "hT2", "hT3"]
                    if debug and l == 0 and i == 0:
                        P.dma("sp", lambda E: E.dma_start(out=dbg["d_hT"].ap(), in_=hT[:].rearrange("p c t -> p (c t)")), r=hTr, w=["dbg1"])
                        P.dma("sp", lambda E: E.dma_start(out=dbg["d_mod"].ap(), in_=mod[:]), r=[f"mod{q}" for q in range(12)], w=["dbg2"])
                        P.dma("sp", lambda E: E.dma_start(out=dbg["d_pvT"].ap(), in_=pvT[:].rearrange("p c t -> p (c t)")), r=["pvT"], w=["dbg2b"])

                    def win_mm(oc):
                        pb, pbn = fbank()
                        for c in range(8):
                            P.op("pe", lambda E, c=c, pb=pb, oc=oc: E.matmul(pb[:], lhsT=win[:, c, oc * 128:(oc + 1) * 128], rhs=hT[:, c, :],
                                                                           start=(c == 0), stop=(c == 7)),
                                 r=["win"] + hTr, w=[pbn])
                        return pb, pbn

                    def pU(g):
                        pb, pbn = win_mm(g)
                        P.op("act", lambda E, pb=pb: E.activation(out=u[:, g, 16:528], in_=pb[:], func=AF.Copy), w=[pbn, f"u{g}"])

                    def pD(g):
                        wdw = POOL_WINDOWS[g]
                        un = f"u{g}"
                        step = 1
                        bufs = [sA, sB]
                        bi = 0
                        cur = None
                        lo = 0
                        while step < wdw:
                            dst = bufs[bi]
                            dn = "sA" if bi == 0 else "sB"
                            nlo = lo + step
                            if cur is None:
                                P.op("pool", lambda E, dst=dst, nlo=nlo, step=step: E.tensor_tensor(
                                    out=dst[:, nlo:528], in0=u[:, g, nlo:528], in1=u[:, g, nlo - step:528 - step], op=ALU.add),
                                    r=[un], w=[dn])
                            else:
                                cs, cn = cur
                                P.op("pool", lambda E, dst=dst, cs=cs, nlo=nlo, step=step: E.tensor_tensor(
                                    out=dst[:, nlo:528], in0=cs[:, nlo:528], in1=cs[:, nlo - step:528 - step], op=ALU.add),
                                    r=[cn], w=[dn])
                            cur = (dst, dn)
                            lo = nlo
                            step *= 2
                            bi ^= 1
                        cs, cn = cur
                        P.op("dve", lambda E, cs=cs, wdw=wdw: E.scalar_tensor_tensor(
                            out=pooled[:, g, :], in0=cs[:, 16:528], scalar=1.0 / wdw, in1=u[:, g, 16:528], op0=ALU.mult, op1=ALU.subtract),
                            r=[cn, un], w=[f"pooled{g}"])
                        if i == 0:
                            nfix = wdw - 1
                            P.op("pool", lambda E, cs=cs, nfix=nfix: E.tensor_tensor(out=cs[:, 16:16 + nfix], in0=cs[:, 16:16 + nfix],
                                                                                    in1=inv16[:, 0:nfix], op=ALU.mult),
                                 r=[cn, "inv16", f"pooled{g}"], w=[cn])
                            P.op("pool", lambda E, cs=cs, nfix=nfix: E.tensor_tensor(out=pooled[:, g, 0:nfix], in0=cs[:, 16:16 + nfix],
                                                                                    in1=u[:, g, 16:16 + nfix], op=ALU.subtract),
                                 r=[cn, un], w=[f"pooled{g}"])
                        P.op("pool", lambda E: E.tensor_copy(out=u[:, g, 0:16], in_=u[:, g, 512:528]), r=[un], w=[un])

                    def pPW(g):
                        pb2, pb2n = fbank()
                        P.op("pe", lambda E, pb2=pb2: E.matmul(pb2[:], lhsT=pw[:, g, :], rhs=pooled[:, g, :], start=True, stop=True),
                             r=["pw", f"pooled{g}"], w=[pb2n])
                        P.op("act", lambda E, pb2=pb2: E.activation(out=ycat[:, g, :], in_=pb2[:], func=AF.Copy, scale=pvT[:, g, 34:35]),
                             r=["pvT"], w=[pb2n, f"ycat{g}"])

                    def cF(c):
                        kb = c % 2
                        vn = f"vbuf{c}"
                        pa, pan = win_mm(4 + c)
                        pg, pgn = win_mm(8 + c)
                        P.op("act", lambda E, pg=pg: E.activation(out=sig[0][:], in_=pg[:], func=AF.Sigmoid), w=[pgn, "sig0"])
                        P.op("dve", lambda E, pa=pa: E.tensor_tensor(out=vbuf[:, c, 32:544], in0=pa[:], in1=sig[0][:], op=ALU.mult),
                             r=["sig0"], w=[pan, vn])
                        py, pyn = fbank()
                        for k in range(CK):
                            P.op("pe", lambda E, k=k, py=py: E.matmul(py[:], lhsT=diag[:, k, c, :], rhs=vbuf[:, c, 2 + k:2 + k + 512],
                                                                    start=(k == 0), stop=(k == CK - 1)),
                                 r=[f"diag{k}_{c}", vn], w=[pyn])
                        P.op("pool", lambda E: E.tensor_copy(out=vbuf[:, c, 2:32], in_=vbuf[:, c, 514:544]), r=[vn], w=[vn])
                        P.op("act", lambda E, py=py: E.activation(out=ybf[kb][:], in_=py[:], func=AF.Identity, bias=pvT[:, c, 31:32]),
                             r=["pvT"], w=[pyn, f"ybf{kb}"])

                    def cB1(c):
                        kb = c % 2
                        pm, pmn = fbank()
                        P.op("pe", lambda E, pm=pm: E.matmul(pm[:], lhsT=bavg[:], rhs=ybf[kb][:], start=True, stop=True),
                             r=["bavg", f"ybf{kb}"], w=[pmn])
                        P.op("dve", lambda E, pm=pm: E.tensor_tensor(out=dd[0][:], in0=ybf[kb][:], in1=pm[:], op=ALU.subtract),
                             r=[f"ybf{kb}"], w=[pmn, "dd0"])
                        P.op("act", lambda E: E.activation(out=sq[0][:], in_=dd[0][:], func=AF.Square), r=["dd0"], w=["sq0"])

                    def cB2(c):
                        pvv, pvn = fbank()
                        P.op("pe", lambda E, pvv=pvv: E.matmul(pvv[:], lhsT=bavg[:], rhs=sq[0][:], start=True, stop=True),
                             r=["bavg", "sq0"], w=[pvn])
                        P.op("act", lambda E, pvv=pvv: E.activation(out=rs[0][:], in_=pvv[:], func=AF.Sqrt, bias=epsl[:, 0:1]),
                             r=["epsl"], w=[pvn, "rs0"])
                        P.op("dve", lambda E: E.reciprocal(out=rs[0][:], in_=rs[0][:]), r=["rs0"], w=["rs0"])
                        P.op("pool", lambda E: E.tensor_tensor(out=dd[0][:], in0=dd[0][:], in1=rs[0][:], op=ALU.mult),
                             r=["dd0", "rs0"], w=["dd0"])
                        P.op("act", lambda E: E.activation(out=ycat[:, 4 + c, :], in_=dd[0][:], func=AF.Silu, bias=pvT[:, c, 33:34],
                                                           scale=pvT[:, c, 32:33]),
                             r=["dd0", "pvT"], w=[f"ycat{4 + c}"])

                    for g_ in range(4):
                        pU(g_)
                    cF(0); pD(0); cF(1); pD(1); cB1(0); cF(2); pD(2); cB2(0); cB1(1); cF(3); pD(3); cB2(1); cB1(2)
                    pPW(0); pPW(1); cB2(2); cB1(3); pPW(2); pPW(3); cB2(3)
                    ycr = [f"ycat{k}" for k in range(8)]
                    if debug and l == 0 and i == 0:
                        P.dma("sp", lambda E: E.dma_start(out=dbg["d_ycat"].ap(), in_=ycat[:].rearrange("p c t -> p (c t)")), r=ycr, w=["dbg3"])
                    for j in range(4):
                        for dh in range(2):
                            po, pon = fbank()
                            for k in range(8):
                                P.op("pe", lambda E, j=j, dh=dh, k=k, po=po: E.matmul(po[:], lhsT=ycat[:, k, j * 128:(j + 1) * 128],
                                                                                  rhs=wout[:, k, dh * 512:(dh + 1) * 512], start=(k == 0), stop=(k == 7)),
                                     r=ycr + ["wout"], w=[pon])
                            kk = (j * 2 + dh) % 2
                            P.op("dve", lambda E, dh=dh, po=po, kk=kk: E.tensor_tensor(out=tmpO[kk][:], in0=po[:], in1=G1[:, dh * 512:(dh + 1) * 512],
                                                                                      op=ALU.mult), r=[f"mod{4 + dh}"], w=[pon, f"tmpO{kk}"])
                            P.op("pool", lambda E, j=j, dh=dh, X=X, kk=kk: E.tensor_tensor(out=X[:, j, dh * 512:(dh + 1) * 512], in0=tmpO[kk][:],
                                                                                         in1=X[:, j, dh * 512:(dh + 1) * 512], op=ALU.add),
                                 r=[f"tmpO{kk}", Xn], w=[Xn])
                    P.dma("act", lambda E, X=X, rows=rows: E.dma_start(out=xs1[rows, :].rearrange("(j p) d -> p j d", p=128), in_=X[:]),
                          r=[Xn], w=[f"xs1_{i}"])
                    for j in range(4):
                        P.op("act", lambda E, j=j, X=X: E.activation(out=hb[:, j, :], in_=X[:, j, :], func=AF.Square, accum_out=ss[:, 4 + j:5 + j]),
                             r=[Xn], w=[f"hb{j}", "ss"])
                    P.op("act", lambda E: E.activation(out=rr[:, 4:8], in_=ss[:, 4:8], func=AF.Sqrt, bias=epsr[:, 0:1]), r=["ss", "epsr"], w=["rr"])
                    P.op("dve", lambda E: E.reciprocal(out=rr[:, 4:8], in_=rr[:, 4:8]), r=["rr"], w=["rr"])
                    pr, prn = psr, "psr"
                    for j in range(4):
                        T = tmpA[j % 2]
                        Tn = f"tmpA{j % 2}"
                        H = T
                        Hn = Tn
                        P.op("dve", lambda E, j=j, X=X, T=T: E.scalar_tensor_tensor(out=T[:], in0=X[:, j, :], scalar=rr[:, 4 + j:5 + j], in1=A2,
                                                                                    op0=ALU.mult, op1=ALU.mult),
                             r=[Xn, "rr", "mod8", "mod9"], w=[Tn])
                        P.op("pool", lambda E, T=T, H=H: E.tensor_tensor(out=H[:], in0=T[:], in1=B2, op=ALU.add), r=[Tn, "mod6", "mod7"], w=[Hn])
                        P.op("act", lambda E, j=j, H=H: E.activation(out=h2b[:, j, :], in_=H[:], func=AF.Copy), r=[Hn], w=[f"hb{j}"])
                        HT = h2T[0]
                        HTn = "h2T0"
                        for half in range(2):
                            pt, ptn = fbank()
                            for cc in range(4):
                                c = half * 4 + cc
                                P.op("pe", lambda E, c=c, cc=cc, H=H, pt=pt: E.transpose(out=pt[:, cc * 128:(cc + 1) * 128],
                                                                                       in_=H[:, c * 128:(c + 1) * 128], identity=identf[:]),
                                     r=[Hn, "identf"], w=[ptn])
                            eng = "act" if half == 0 else "dve"
                            if eng == "act":
                                P.op("act", lambda E, half=half, HT=HT, pt=pt: E.activation(out=HT[:, half * 4:(half + 1) * 4, :],
                                                                                        in_=pt[:].rearrange("p (c t) -> p c t", c=4), func=AF.Copy),
                                     w=[ptn, HTn])
                            else:
                                P.op("dve", lambda E, half=half, HT=HT, pt=pt: E.tensor_copy(out=HT[:, half * 4:(half + 1) * 4, :],
                                                                                         in_=pt[:].rearrange("p (c t) -> p c t", c=4)),
                                     w=[ptn, HTn])
                        for c in range(8):
                            P.op("pe", lambda E, j=j, c=c, HT=HT, pr=pr: E.matmul(pr[:, j * 36:(j + 1) * 36], lhsT=HT[:, c, :], rhs=wr[:, c, :],
                                                                              start=(c == 0), stop=(c == 7)),
                                 r=[HTn, "wr"], w=[prn])
                    P.dma("act", lambda E, rows=rows: E.dma_start(out=h2s[rows, :].rearrange("(j p) d -> p j d", p=128), in_=h2b[:]), r=["hb0", "hb1", "hb2", "hb3"], w=[f"h2s_{i}"])
                    P.op("dve", lambda E, pr=pr: E.tensor_tensor(out=lg_all[:, 4 * i:4 * i + 4, :], in0=pr[:, 0:144].rearrange("p (j e) -> p j e", j=4),
                                                                 in1=rbias[:], op=ALU.add), r=["rbias"], w=[prn, "lg_all"])
                    if debug and l == 0 and i == 0:
                        P.dma("sp", lambda E: E.dma_start(out=dbg["d_lg"].ap(), in_=lg_all[:, 0:4, :].rearrange("p c t -> p (c t)")), r=["lg_all"], w=["dbg4"])
                for _i in range(NT):
                    mtile(_i)

            nxt = (l + 1 < DEPTH_)
            scP = contextlib.ExitStack()
            if nxt:
                load_weights(l + 1)
                aw_n, ab_n, gbc_n = pro_bufs(scP, f"p{l + 1}", nbuf=1)
                prologue(l + 1, 0, 20, True, aw_n, ab_n, gbc_n)
            with contextlib.ExitStack() as sc:
                def sbl(name, shape, dt=F32, sc=sc):
                    t_ = sc.enter_context(nc.sbuf_tensor(f"{name}_{l}", list(shape), dt))
                    P.reg(name, t_)
                    return t_
                NC_ = NB * NE
                within = sbl("within", [128, NB, NE])
                tot = [sbl(f"tot{k}", [128, NB, NE]) for k in range(2)]
                tot0 = sbl("totz", [128, NB, NE])
                cmpb = sbl("cmpb", [128, NE, NB2])
                ntile = [sbl(f"ntile{k}", [128, NE]) for k in range(2)]
                nt0 = sbl("ntz", [128, NE])
                basee = sbl("basee", [128, NE])
                tend = sbl("tend", [128, NE])
                posall = sbl("posall", [128, NB, NE])
                ptmp = sbl("ptmp", [128, NB, NE])
                posf = sbl("posf", [128, NB])
                thr = sbl("thr", [128, NB2])
                cmpt = sbl("cmpt", [128, TM, NE])
                tef = sbl("tef", [128, TM])
                gmax = sbl("gmax", [128, NB])
                gd = sbl("gd", [128, NB, 4])
                gsum = sbl("gsum", [128, NB])
                pgrp = sbl("pgrp", [128, NB])
                ohg = sbl("ohg", [128, NB, 4])
                em = sbl("em", [128, NB, NE])
                em2 = sbl("em2", [128, NB, NE])
                m1 = sbl("m1", [128, NB])
                m2 = sbl("m2", [128, NB])
                d12 = sbl("d12", [128, NB])
                gl = lg_all[:, :, 0:4]
                el = lg_all[:, :, 4:36]
                P.op("dve", lambda E: E.tensor_reduce(out=gmax[:], in_=gl, axis=AX.X, op=ALU.max), r=["lg_all"], w=["gmax"])
                P.op("dve", lambda E: E.tensor_tensor(out=gd[:], in0=gl, in1=gmax[:].unsqueeze(2).to_broadcast([128, NB, 4]), op=ALU.subtract),
                     r=["lg_all", "gmax"], w=["gd"])
                P.op("dve", lambda E: E.tensor_tensor(out=ohg[:], in0=gl, in1=gmax[:].unsqueeze(2).to_broadcast([128, NB, 4]), op=ALU.is_equal),
                     r=["lg_all", "gmax"], w=["ohg"])
                P.op("act", lambda E: E.activation(out=gd[:], in_=gd[:], func=AF.Exp), r=["gd"], w=["gd"])
                P.op("dve", lambda E: E.tensor_reduce(out=gsum[:], in_=gd[:], axis=AX.X, op=ALU.add), r=["gd"], w=["gsum"])
                P.op("dve", lambda E: E.reciprocal(out=pgrp[:], in_=gsum[:]), r=["gsum"], w=["pgrp"])
                P.op("dve", lambda E: E.tensor_scalar(out=ohg[:], in0=ohg[:], scalar1=-1.0, scalar2=1e30, op0=ALU.add, op1=ALU.mult),
                     r=["ohg"], w=["ohg"])
                P.op("dve", lambda E: E.tensor_tensor(out=em[:].rearrange("p j (g e) -> p j g e", g=4),
                                                      in0=el.rearrange("p j (g e) -> p j g e", g=4),
                                                      in1=ohg[:].unsqueeze(3).to_broadcast([128, NB, 4, 8]), op=ALU.add),
                     r=["lg_all", "ohg"], w=["em"])
                P.op("dve", lambda E: E.tensor_reduce(out=m1[:], in_=em[:], axis=AX.X, op=ALU.max), r=["em"], w=["m1"])
                P.op("dve", lambda E: E.tensor_tensor(out=oh1[:], in0=em[:], in1=m1[:].unsqueeze(2).to_broadcast([128, NB, NE]),
                                                      op=ALU.is_equal), r=["em", "m1"], w=["oh1"])
                P.op("dve", lambda E: E.scalar_tensor_tensor(out=em2[:], in0=oh1[:], scalar=-1e30, in1=em[:], op0=ALU.mult,
                                                             op1=ALU.add), r=["oh1", "em"], w=["em2"])
                P.op("dve", lambda E: E.tensor_reduce(out=m2[:], in_=em2[:], axis=AX.X, op=ALU.max), r=["em2"], w=["m2"])
                P.op("dve", lambda E: E.tensor_tensor(out=oh2[:], in0=em2[:], in1=m2[:].unsqueeze(2).to_broadcast([128, NB, NE]),
                                                      op=ALU.is_equal), r=["em2", "m2"], w=["oh2"])
                P.op("dve", lambda E: E.tensor_tensor(out=d12[:], in0=m1[:], in1=m2[:], op=ALU.subtract), r=["m1", "m2"], w=["d12"])
                P.op("act", lambda E: E.activation(out=d12[:], in_=d12[:], func=AF.Sigmoid), r=["d12"], w=["d12"])
                P.op("dve", lambda E: E.tensor_tensor(out=w1[:], in0=d12[:], in1=pgrp[:], op=ALU.mult), r=["d12", "pgrp"], w=["w1"])
                P.op("dve", lambda E: E.tensor_tensor(out=w2[:], in0=pgrp[:], in1=w1[:], op=ALU.subtract), r=["pgrp", "w1"], w=["w2"])
                P.op("dve", lambda E: E.tensor_tensor(out=Aall[:], in0=oh1[:], in1=oh2[:], op=ALU.add), r=["oh1", "oh2"], w=["Aall"])
                Af = Aall[:].rearrange("p b e -> p (b e)")
                for h0 in range(0, NC_, 512):
                    wd_ = min(512, NC_ - h0)
                    pbw, pbwn = fbank()
                    P.op("pe", lambda E, h0=h0, wd_=wd_, pbw=pbw: E.matmul(pbw[:, 0:wd_], lhsT=trib[:], rhs=Af[:, h0:h0 + wd_], start=True, stop=True),
                         r=["trib", "Aall"], w=[pbwn])
                    P.op("act", lambda E, h0=h0, wd_=wd_, pbw=pbw: E.activation(out=within[:].rearrange("p b e -> p (b e)")[:, h0:h0 + wd_],
                                                                           in_=pbw[:, 0:wd_], func=AF.Copy), w=[pbwn, "within"])
                    pbt, pbtn = fbank()
                    P.op("pe", lambda E, h0=h0, wd_=wd_, pbt=pbt: E.matmul(pbt[:, 0:wd_], lhsT=onesb[:], rhs=Af[:, h0:h0 + wd_], start=True, stop=True),
                         r=["onesb", "Aall"], w=[pbtn])
                    P.op("dve", lambda E, h0=h0, wd_=wd_, pbt=pbt: E.tensor_copy(out=tot0[:].rearrange("p b e -> p (b e)")[:, h0:h0 + wd_],
                                                                            in_=pbt[:, 0:wd_]), w=[pbtn, "totz"])
                cur, curn = tot0, "totz"
                step = 1
                bi = 0
                while step < NB:
                    dst, dn = tot[bi], f"tot{bi}"
                    P.op("dve", lambda E, dst=dst, cur=cur, step=step: E.tensor_copy(out=dst[:, 0:step, :], in_=cur[:, 0:step, :]), r=[curn], w=[dn])
                    P.op("dve", lambda E, dst=dst, cur=cur, step=step: E.tensor_tensor(out=dst[:, step:NB, :], in0=cur[:, step:NB, :],
                                                                                   in1=cur[:, 0:NB - step, :], op=ALU.add), r=[curn], w=[dn])
                    cur, curn = dst, dn
                    step *= 2
                    bi ^= 1
                incl, incln = cur, curn
                P.op("dve", lambda E: E.tensor_tensor(out=posall[:], in0=within[:], in1=incl[:], op=ALU.add), r=["within", incln], w=["posall"])
                P.op("dve", lambda E: E.tensor_tensor(out=posall[:], in0=posall[:], in1=tot0[:], op=ALU.subtract), r=["posall", "totz"], w=["posall"])
                cnt = incl[:, NB - 1, :]
                P.op("dve", lambda E: E.tensor_scalar(out=thr[:], in0=iota_f[:, 0:NB2], scalar1=float(SL), scalar2=None, op0=ALU.mult),
                     r=["iota_f"], w=["thr"])
                P.op("dve", lambda E: E.tensor_tensor(out=cmpb[:], in0=cnt.unsqueeze(2).to_broadcast([128, NE, NB2]),
                                                      in1=thr[:].unsqueeze(1).to_broadcast([128, NE, NB2]), op=ALU.is_gt),
                     r=[incln, "thr"], w=["cmpb"])
                P.op("dve", lambda E: E.tensor_reduce(out=nt0[:], in_=cmpb[:], axis=AX.X, op=ALU.add), r=["cmpb"], w=["ntz"])
                cur, curn = nt0, "ntz"
                step = 1
                bi = 0
                while step < NE:
                    dst, dn = ntile[bi], f"ntile{bi}"
                    P.op("dve", lambda E, dst=dst, cur=cur, step=step: E.tensor_copy(out=dst[:, 0:step], in_=cur[:, 0:step]), r=[curn], w=[dn])
                    P.op("dve", lambda E, dst=dst, cur=cur, step=step: E.tensor_tensor(out=dst[:, step:NE], in0=cur[:, step:NE],
                                                                                   in1=cur[:, 0:NE - step], op=ALU.add), r=[curn], w=[dn])
                    cur, curn = dst, dn
                    step *= 2
                    bi ^= 1
                P.op("dve", lambda E, cur=cur: E.tensor_copy(out=tend[:], in_=cur[:]), r=[curn], w=["tend"])
                P.op("dve", lambda E: E.tensor_tensor(out=basee[:], in0=tend[:], in1=nt0[:], op=ALU.subtract), r=["tend", "ntz"], w=["basee"])
                P.op("dve", lambda E: E.tensor_scalar(out=basee[:], in0=basee[:], scalar1=float(SL), scalar2=None, op0=ALU.mult), r=["basee"], w=["basee"])
                P.op("dve", lambda E: E.tensor_tensor(out=posall[:], in0=posall[:], in1=basee[:].unsqueeze(1).to_broadcast([128, NB, NE]), op=ALU.add),
                     r=["posall", "basee"], w=["posall"])
                for (ohk, posk, nm) in ((oh1, pos1, "pos1"), (oh2, pos2, "pos2")):
                    P.op("dve", lambda E, ohk=ohk: E.tensor_tensor(out=ptmp[:], in0=ohk[:], in1=posall[:], op=ALU.mult),
                         r=["oh1", "oh2", "posall"], w=["ptmp"])
                    P.op("dve", lambda E: E.tensor_reduce(out=posf[:], in_=ptmp[:], axis=AX.X, op=ALU.add), r=["ptmp"], w=["posf"])
                    P.op("dve", lambda E, posk=posk: E.tensor_copy(out=posk[:], in_=posf[:]), r=["posf"], w=[nm])
                for j0 in range(0, TM, 128):
                    jn = min(128, TM - j0)
                    P.op("dve", lambda E, j0=j0, jn=jn: E.tensor_scalar(out=tef[:, j0:j0 + jn], in0=iota_f[:, 0:jn], scalar1=float(j0), scalar2=None,
                                                                       op0=ALU.add), r=["iota_f"], w=["tef"])
                P.op("dve", lambda E: E.tensor_tensor(out=cmpt[:], in0=tend[:].unsqueeze(1).to_broadcast([128, TM, NE]),
                                                      in1=tef[:].unsqueeze(2).to_broadcast([128, TM, NE]), op=ALU.is_le),
                     r=["tend", "tef"], w=["cmpt"])
                P.op("dve", lambda E: E.tensor_reduce(out=tef[:], in_=cmpt[:], axis=AX.X, op=ALU.add), r=["cmpt"], w=["tef"])
                P.op("dve", lambda E: E.tensor_scalar(out=tef[:], in0=tef[:], scalar1=128.0, scalar2=pidx_f[:, 0:1], op0=ALU.mult, op1=ALU.add),
                     r=["tef", "pidx_f"], w=["tef"])
                P.op("dve", lambda E: E.tensor_copy(out=te_i[:], in_=tef[:]), r=["tef"], w=["te_i"])

            if debug and l == 0:
                for nm, t_, rn in (("d_pos1", pos1, "pos1"), ("d_pos2", pos2, "pos2"), ("d_w1", w1, "w1"), ("d_w2", w2, "w2"), ("d_te", te_i, "te_i")):
                    P.dma("sp", lambda E, nm=nm, t_=t_: E.dma_start(out=dbg[nm].ap(), in_=t_[:]), r=[rn], w=["dbg_" + nm])
            with contextlib.ExitStack() as sc:
                def sbl(name, shape, dt=F32, sc=sc):
                    t_ = sc.enter_context(nc.sbuf_tensor(f"{name}_{l}", list(shape), dt))
                    P.reg(name, t_)
                    return t_
                hrow = [sbl(f"hrow{k}", [128, D], BF16) for k in range(3)]
                def sblock(b):
                    k = b % 3
                    P.dma("sp", lambda E, b=b, k=k: E.dma_start(out=hrow[k][:], in_=h2s[b * 128:(b + 1) * 128, :]), r=[f"h2s_{b // 4}"], w=[f"hrow{k}"])
                    for (posk, nm) in ((pos1, "pos1"), (pos2, "pos2")):
                        P.dma("pool", lambda E, b=b, k=k, posk=posk: E.indirect_dma_start(
                            out=hsort[:, :], out_offset=bass.IndirectOffsetOnAxis(ap=posk[:, b:b + 1], axis=0),
                            in_=hrow[k][:], in_offset=None), r=[f"hrow{k}", nm], w=[f"hsortw{b}"])
                for _b in range(NB):
                    sblock(_b)
            with contextlib.ExitStack() as sc:
                def sbl(name, shape, dt=F32, sc=sc):
                    t_ = sc.enter_context(nc.sbuf_tensor(f"{name}_{l}", list(shape), dt))
                    P.reg(name, t_)
                    return t_
                NW = 3
                wgb = [sbl(f"wgb{k}", [128, 8, HID], BF16) for k in range(NW)]
                wub = [sbl(f"wub{k}", [128, 8, HID], BF16) for k in range(NW)]
                wdb = [sbl(f"wdb{k}", [128, 2, D], BF16) for k in range(NW)]
                stt = [sbl(f"stt{k}", [128, 2, D], BF16) for k in range(2)]
                hsT = [sbl(f"hsT{k}", [128, 8, SL], BF16) for k in range(2)]
                sgl = [sbl(f"sgl{k}", [128, 2 * SL]) for k in range(2)]
                hid = [sbl(f"hid{k}", [128, 2, SL], BF16) for k in range(2)]
                yo = [sbl(f"yo{k}", [128, 2, D]) for k in range(2)]
                allsc = [f"hsortw{b}" for b in range(NB)]

                def etile(j):
                    k = j % NW
                    k2 = j % 2
                    for (wb, wsrc, nm) in ((wgb, wg_d, "wgb"), (wub, wu_d, "wub"), (wdb, wd_d, "wdb")):
                        P.dma("pool", lambda E, wb=wb, wsrc=wsrc: E.indirect_dma_start(
                            out=wb[k][:].rearrange("p c h -> p (c h)"), out_offset=None, in_=wsrc[l][:, :],
                            in_offset=bass.IndirectOffsetOnAxis(ap=te_i[:, j:j + 1], axis=0), bounds_check=bcd["v"], oob_is_err=False),
                            r=["te_i"], w=[f"{nm}{k}"])
                    P.dma("sp", lambda E: E.dma_start(out=stt[k2][:], in_=hsort[j * SL:(j + 1) * SL, :].rearrange("(q p) d -> p q d", p=128)),
                          r=allsc, w=[f"stt{k2}"])
                    for q in range(2):
                        pb, pbn = bbank()
                        for c in range(8):
                            P.op("pe", lambda E, c=c, q=q, pb=pb: E.transpose(out=pb[:, c * 128:(c + 1) * 128], in_=stt[k2][:, q, c * 128:(c + 1) * 128],
                                                                           identity=identb[:]), r=[f"stt{k2}", "identb"], w=[pbn])
                        if q == 0:
                            P.op("act", lambda E, q=q, pb=pb: E.activation(out=hsT[k2][:, :, q * 128:(q + 1) * 128],
                                                                         in_=pb[:].rearrange("p (c t) -> p c t", c=8), func=AF.Copy), w=[pbn, f"hsT{k2}_{q}"])
                        else:
                            P.op("dve", lambda E, q=q, pb=pb: E.tensor_copy(out=hsT[k2][:, :, q * 128:(q + 1) * 128],
                                                                          in_=pb[:].rearrange("p (c t) -> p c t", c=8)), w=[pbn, f"hsT{k2}_{q}"])
                    hsr = [f"hsT{k2}_0", f"hsT{k2}_1"]
                    pg, pgn = fbank()
                    pu, pun = fbank()
                    for (W, Wn, pz, pzn) in ((wgb[k], f"wgb{k}", pg, pgn), (wub[k], f"wub{k}", pu, pun)):
                        for hc in range(2):
                            for c in range(8):
                                P.op("pe", lambda E, c=c, W=W, hc=hc, pz=pz: E.matmul(
                                    pz[:, hc * SL:(hc + 1) * SL], lhsT=W[:, c, hc * 128:(hc + 1) * 128], rhs=hsT[k2][:, c, :],
                                    start=(c == 0), stop=(c == 7)), r=[Wn] + hsr, w=[pzn])
                    P.op("act", lambda E: E.activation(out=sgl[k2][:], in_=pg[:, 0:2 * SL], func=AF.Silu), w=[pgn, f"sgl{k2}"])
                    P.op("dve", lambda E: E.tensor_tensor(out=hid[k2][:].rearrange("p c t -> p (c t)"), in0=pu[:, 0:2 * SL],
                                                          in1=sgl[k2][:], op=ALU.mult), r=[f"sgl{k2}"], w=[pun, f"hid{k2}"])
                    for q in range(2):
                        for dh in range(2):
                            pd, pdn = fbank()
                            for kc in range(2):
                                P.op("pe", lambda E, q=q, dh=dh, kc=kc, pd=pd: E.matmul(pd[:], lhsT=hid[k2][:, kc, q * 128:(q + 1) * 128],
                                                                                     rhs=wdb[k][:, kc, dh * 512:(dh + 1) * 512],
                                                                                     start=(kc == 0), stop=(kc == 1)),
                                     r=[f"hid{k2}", f"wdb{k}"], w=[pdn])
                            if dh == 0:
                                P.op("act", lambda E, q=q, pd=pd: E.activation(out=yo[k2][:, q, 0:512], in_=pd[:], func=AF.Copy), w=[pdn, f"yo{k2}_{q}0"])
                            else:
                                P.op("dve", lambda E, q=q, pd=pd: E.tensor_copy(out=yo[k2][:, q, 512:1024], in_=pd[:]), w=[pdn, f"yo{k2}_{q}1"])
                    P.dma("act", lambda E: E.dma_start(out=ysort[j * SL:(j + 1) * SL, :].rearrange("(q p) d -> p q d", p=128), in_=yo[k2][:]),
                          r=[f"yo{k2}_{q}{h}" for q in range(2) for h in "01"], w=[f"ysortw{j}"])
                for _j in range(TM):
                    etile(_j)

            scP.close()
            with contextlib.ExitStack() as sc:
                def sbl(name, shape, dt=F32, sc=sc):
                    t_ = sc.enter_context(nc.sbuf_tensor(f"{name}_{l}", list(shape), dt))
                    P.reg(name, t_)
                    return t_
                r1 = [sbl(f"r1{k}", [128, D]) for k in range(2)]
                r2 = [sbl(f"r2{k}", [128, D]) for k in range(2)]
                xc = [sbl(f"xc{k}", [128, D]) for k in range(2)]
                fgb = sbl("fgb", [128, D])
                ssf = sbl("ssf", [128, NB])
                rrf = sbl("rrf", [128, NB])
                junk2 = sbl("junk2", [128, D], BF16)
                ally = [f"ysortw{j}" for j in range(TM)]
                if last:
                    P.dma("sp", lambda E: E.dma_start(out=fgb[:], in_=final_g.ap().partition_broadcast(128)), w=["fgb"])
                    P.op("act", lambda E: E.mul(out=fgb[:], in_=fgb[:], mul=32.0), r=["fgb"], w=["fgb"])
                    P.op("dve", lambda E: E.memset(ssf[:], 0.0), w=["ssf"])
                def cblock(b):
                    k = b % 2
                    rows = slice(b * 128, (b + 1) * 128)
                    P.dma("pool", lambda E, b=b, k=k: E.indirect_dma_start(out=r1[k][:], out_offset=None, in_=ysort[:, :],
                                                                          in_offset=bass.IndirectOffsetOnAxis(ap=pos1[:, b:b + 1], axis=0)),
                          r=ally + ["pos1"], w=[f"r1{k}"])
                    P.dma("pool", lambda E, b=b, k=k: E.indirect_dma_start(out=r2[k][:], out_offset=None, in_=ysort[:, :],
                                                                          in_offset=bass.IndirectOffsetOnAxis(ap=pos2[:, b:b + 1], axis=0)),
                          r=ally + ["pos2"], w=[f"r2{k}"])
                    P.dma("sp", lambda E, k=k, rows=rows: E.dma_start(out=xc[k][:], in_=xs1[rows, :]), r=[f"xs1_{b // 4}"], w=[f"xc{k}"])
                    P.op("act", lambda E, b=b, k=k: E.activation(out=r1[k][:], in_=r1[k][:], func=AF.Copy, scale=w1[:, b:b + 1]),
                         r=[f"r1{k}", "w1"], w=[f"r1{k}"])
                    P.op("dve", lambda E, b=b, k=k: E.scalar_tensor_tensor(out=r2[k][:], in0=r2[k][:], scalar=w2[:, b:b + 1], in1=r1[k][:],
                                                                          op0=ALU.mult, op1=ALU.add), r=[f"r2{k}", f"r1{k}", "w2"], w=[f"r2{k}"])
                    P.op("pool", lambda E, k=k: E.tensor_tensor(out=r2[k][:], in0=r2[k][:], in1=G2, op=ALU.mult), r=[f"r2{k}", "mod10", "mod11"], w=[f"r2{k}"])
                    P.op("dve", lambda E, k=k: E.tensor_tensor(out=xc[k][:], in0=xc[k][:], in1=r2[k][:], op=ALU.add), r=[f"xc{k}", f"r2{k}"],
                         w=[f"xc{k}"])
                    if not last:
                        P.dma("act", lambda E, k=k, rows=rows: E.dma_start(out=xs2[rows, :], in_=xc[k][:]), r=[f"xc{k}"], w=[f"xs2_{b}"])
                    else:
                        P.op("act", lambda E, b=b, k=k: E.activation(out=junk2[:], in_=xc[k][:], func=AF.Square, accum_out=ssf[:, b:b + 1]),
                             r=[f"xc{k}"], w=["junk2", "ssf"])
                        P.op("act", lambda E, b=b: E.activation(out=rrf[:, b:b + 1], in_=ssf[:, b:b + 1], func=AF.Sqrt, bias=epsr[:, 0:1]),
                             r=["ssf", "epsr"], w=["rrf"])
                        P.op("dve", lambda E, b=b: E.reciprocal(out=rrf[:, b:b + 1], in_=rrf[:, b:b + 1]), r=["rrf"], w=["rrf"])
                        P.op("dve", lambda E, b=b, k=k: E.scalar_tensor_tensor(out=xc[k][:], in0=xc[k][:], scalar=rrf[:, b:b + 1], in1=fgb[:],
                                                                              op0=ALU.mult, op1=ALU.mult), r=[f"xc{k}", "rrf", "fgb"], w=[f"xc{k}"])
                        P.dma("act", lambda E, k=k, rows=rows: E.dma_start(out=out_d[rows, :], in_=xc[k][:]), r=[f"xc{k}"], w=[f"out_{b}"])
                for _b in range(NB):
                    cblock(_b)
            if nxt:
                with contextlib.ExitStack() as scq:
                    aw_q, ab_q, gbc_q = pro_bufs(scq, f"q{l + 1}")
                    prologue(l + 1, 20, 24, False, aw_q, ab_q, gbc_q)
        for _l in range(DEPTH_):
            layer(_l)
        P.op("sp", lambda E: E.nop(), r=[f"out_{b}" for b in range(NB)] + [k for k in ("dbg1", "dbg2", "dbg2b", "dbg3", "dbg4", "dbg_d_pos1",
             "dbg_d_pos2", "dbg_d_w1", "dbg_d_w2", "dbg_d_te")], w=["done"])
        P.emit(sems)
    if debug:
        print("UNRESOLVED regions:", sorted(getattr(P, "unres", [])))
    return nc


_CACHE = {}


def _prep(inputs, S):
    f = lambda a: np.ascontiguousarray(np.asarray(a, dtype=np.float32))
    pvec = np.concatenate([f(inputs["conv_w"]), f(inputs["conv_b"])[:, None, :], f(inputs["conv_ln_g"])[:, None, :],
                           f(inputs["conv_ln_b"])[:, None, :], f(inputs["pool_scale"])[:, None, :]], axis=1)
    rw = np.concatenate([f(inputs["router_group_w"]), f(inputs["router_expert_w"])], axis=2)
    rb = np.concatenate([f(inputs["router_group_b"]), f(inputs["router_expert_b"])], axis=1)
    shared = dict(ada_w=f(inputs["ada_w"]), ada_b=f(inputs["ada_b"]), norm1_g=f(inputs["norm1_g"]), w_in=f(inputs["w_in"]),
                  pool_w=f(inputs["pool_w"]), pvec=np.ascontiguousarray(pvec), w_out=f(inputs["w_out"]), norm2_g=f(inputs["norm2_g"]),
                  rw=np.ascontiguousarray(rw), rb=np.ascontiguousarray(rb), final_g=f(inputs["final_g"]))
    for nm, key, nch in (("wg", "expert_w_gate", 8), ("wu", "expert_w_up", 8), ("wd", "expert_w_down", 2)):
        w = f(inputs[key])
        L, E_, K, F_ = w.shape
        wr_ = w.reshape(L, E_, nch, 128, F_).transpose(0, 1, 3, 2, 4).reshape(L, E_ * 128, nch * F_)
        for l in range(L):
            shared[f"{nm}{l}"] = np.ascontiguousarray(wr_[l])
    x = f(inputs["x"])
    c = f(inputs["c"])
    in_maps = []
    for b in range(x.shape[0]):
        m = dict(shared)
        m["x"] = np.ascontiguousarray(x[b])
        m["c"] = np.ascontiguousarray(c[b])
        in_maps.append(m)
    return in_maps


def kernel(**inputs):
    x = np.asarray(inputs["x"])
    B, S, _ = x.shape
    assert B == N_CORES
    if S not in _CACHE:
        _CACHE[S] = build(S)
    nc = _CACHE[S]
    in_maps = _prep(inputs, S)
    res = run_bass_kernel_spmd(nc, in_maps, core_ids=list(range(B)))
    return np.stack([np.asarray(r["out"], dtype=np.float32) for r in res.results], axis=0)
```

```python
import numpy as np
import concourse.bass as bass
import concourse.mybir as mybir
from concourse.bass_utils import run_bass_kernel_spmd

F32 = mybir.dt.float32
BF16 = mybir.dt.bfloat16
I32 = mybir.dt.int32
AF = mybir.ActivationFunctionType
ALU = mybir.AluOpType
AX = mybir.AxisListType

D = 1024
DEPTH = 2
NE = 32
HID = 256
RMS_EPS = 1e-6
LN_EPS = 1e-5
POOL_WINDOWS = (2, 4, 8, 16)
CK = 31
N_CORES = 8


class Prog:
    ENGS = ("pe", "act", "dve", "pool", "sp")

    def __init__(self, nc, n_dma_sems=8):
        self.nc = nc
        self.ops = []
        self.n_dma_sems = n_dma_sems
        self.tens = {}

    def reg(self, short, handle):
        m = self.nc.lookup_mloc(handle)
        self.tens[short] = (m.name, int(m.addr), int(m.addr) + int(m.dims[1]))

    def _resolve(self, name):
        n = name
        while True:
            if n in self.tens:
                return (name,) + self.tens[n]
            n2 = n.rstrip("0123456789")
            if n2.endswith("_"):
                n2 = n2[:-1]
            if n2 == n or not n2:
                self.unres = getattr(self, "unres", set())
                self.unres.add(name.rstrip("0123456789"))
                return (name, None, 0, 0)
            n = n2

    def op(self, eng, fn, r=(), w=()):
        self.ops.append(dict(eng=eng, fn=fn, r=tuple(self._resolve(x) for x in r), w=tuple(self._resolve(x) for x in w), dma=False))

    def dma(self, q, fn, r=(), w=()):
        self.ops.append(dict(eng=q, fn=fn, r=tuple(self._resolve(x) for x in r), w=tuple(self._resolve(x) for x in w), dma=True))

    @staticmethod
    def _key(nm, par, recs, lo, hi, bins):
        key = (nm, par)
        if key not in recs:
            recs[key] = dict(par=par, lo=lo, hi=hi, w=None, r=[])
            if par is not None:
                for b_ in range(lo // 2048, (hi - 1) // 2048 + 1):
                    bins.setdefault(b_, []).append(key)
        return key

    def barrier(self):
        self.ops.append(dict(eng=None, fn=None, r=(), w=(), dma=False, barrier=True))

    def emit(self, sems):
        nc = self.nc
        engobj = dict(pe=nc.tensor, act=nc.scalar, dve=nc.vector, pool=nc.gpsimd, sp=nc.sync)
        ops = self.ops
        n = len(ops)
        recs = {}
        bins = {}
        clock = {e: dict(pe=-1, act=-1, dve=-1, pool=-1, sp=-1, dma=set()) for e in self.ENGS}
        opclock = [None] * n
        dma_hist = {e: [] for e in self.ENGS}
        waits = [None] * n
        sig = [False] * n
        bar_deps = set()
        last_on = {}
        for i, o in enumerate(ops):
            e = o["eng"]
            if e is None:
                bar_deps = set(last_on.values())
                for q in self.ENGS:
                    bar_deps |= set(dma_hist[q][-self.n_dma_sems:])
                waits[i] = []
                opclock[i] = None
                continue
            deps = set(bar_deps)
            for (nm, par, lo, hi) in o["r"]:
                key = self._key(nm, par, recs, lo, hi, bins)
                lw = recs[key]["w"]
                if lw is not None:
                    deps.add(lw)
            for (nm, par, lo, hi) in o["w"]:
                key = self._key(nm, par, recs, lo, hi, bins)
                rc = recs[key]
                if rc["w"] is not None:
                    deps.add(rc["w"])
                deps.update(rc["r"])
                if par is not None:
                    seen = set()
                    for b_ in range(lo // 2048, (hi - 1) // 2048 + 1):
                        for k2_ in bins.get(b_, ()):
                            if k2_ in seen or k2_ == key:
                                continue
                            seen.add(k2_)
                            r2 = recs[k2_]
                            if r2["par"] != par and r2["lo"] < hi and lo < r2["hi"]:
                                if r2["w"] is not None:
                                    deps.add(r2["w"])
                                deps.update(r2["r"])
            if o["dma"]:
                h = dma_hist[e]
                if len(h) >= self.n_dma_sems:
                    deps.add(h[-self.n_dma_sems])
                h.append(i)
            deps.discard(i)
            need = []
            ck = clock[e]
            for j in sorted(deps):
                pj = ops[j]
                if pj["dma"]:
                    if j in ck["dma"]:
                        continue
                else:
                    if pj["eng"] == e and e == "pe" and not o["dma"]:
                        continue
                    if ck[pj["eng"]] >= j:
                        continue
                need.append(j)
                sig[j] = True
                oc = opclock[j]
                for k in ("pe", "act", "dve", "pool", "sp"):
                    if oc[k] > ck[k]:
                        ck[k] = oc[k]
                ck["dma"] |= oc["dma"]
            waits[i] = need
            oc = dict(pe=ck["pe"], act=ck["act"], dve=ck["dve"], pool=ck["pool"], sp=ck["sp"], dma=set(ck["dma"]))
            if o["dma"]:
                oc["dma"].add(i)
            else:
                oc[e] = i
            opclock[i] = oc
            if not o["dma"]:
                last_on[e] = i
            for (nm, par, lo, hi) in o["r"]:
                recs[self._key(nm, par, recs, lo, hi, bins)]["r"].append(i)
            for (nm, par, lo, hi) in o["w"]:
                rc = recs[self._key(nm, par, recs, lo, hi, bins)]
                rc["w"] = i
                rc["r"] = []
        cnt = {e: 0 for e in self.ENGS}
        dcnt = {}
        dma_n = {e: 0 for e in self.ENGS}
        ev = [None] * n
        for i, o in enumerate(ops):
            e = o["eng"]
            if e is None:
                continue
            E = engobj[e]
            for j in waits[i]:
                s, v = ev[j]
                E.wait_ge(s, v)
            inst = o["fn"](E)
            if o["dma"]:
                k = dma_n[e] % self.n_dma_sems
                dma_n[e] += 1
                s = sems["dma_" + e][k]
                dcnt[(e, k)] = dcnt.get((e, k), 0) + 16
                inst.then_inc(s, 16)
                ev[i] = (s, dcnt[(e, k)])
            elif sig[i]:
                cnt[e] += 1
                inst.then_inc(sems[e], 1)
                ev[i] = (sems[e], cnt[e])
        return ev


def build(S, depth=DEPTH, debug=False):
    DEPTH_ = depth
    NB = S // 128
    NT = S // 512
    SL = 256
    NB2 = (S + SL - 1) // SL
    TM = 2 * S // SL + NE
    nc = bass.Bass("TRN2", target_bir_lowering=False)
    P = Prog(nc)

    def dram_in(name, shape, dt=F32):
        return nc.dram_tensor(name, list(shape), dt, kind="ExternalInput")

    x_d = dram_in("x", [S, D])
    c_d = dram_in("c", [D])
    ada_w = dram_in("ada_w", [DEPTH, D, 6 * D])
    ada_b = dram_in("ada_b", [DEPTH, 6 * D])
    norm1_g = dram_in("norm1_g", [DEPTH, D])
    w_in = dram_in("w_in", [DEPTH, D, 1536])
    pool_w = dram_in("pool_w", [DEPTH, 4, 128, 128])
    pvec = dram_in("pvec", [DEPTH, 35, 512])
    w_out = dram_in("w_out", [DEPTH, D, D])
    norm2_g = dram_in("norm2_g", [DEPTH, D])
    rw = dram_in("rw", [DEPTH, D, 36])
    rb = dram_in("rb", [DEPTH, 36])
    wg_d = [dram_in(f"wg{l}", [NE * 128, 8 * HID]) for l in range(DEPTH)]
    wu_d = [dram_in(f"wu{l}", [NE * 128, 8 * HID]) for l in range(DEPTH)]
    wd_d = [dram_in(f"wd{l}", [NE * 128, 2 * D]) for l in range(DEPTH)]
    final_g = dram_in("final_g", [D])
    out_d = nc.dram_tensor("out", [S, D], F32, kind="ExternalOutput")
    sk = "ExternalOutput" if debug else "Internal"
    xs1 = nc.dram_tensor("xs1", [S, D], F32, kind=sk)
    xs2 = nc.dram_tensor("xs2", [S, D], F32, kind=sk)
    h2s = nc.dram_tensor("h2s", [S, D], BF16, kind=sk)
    hsort = nc.dram_tensor("hsort", [TM * SL, D], BF16, kind=sk)
    ysort = nc.dram_tensor("ysort", [TM * SL, D], F32, kind=sk)
    dbg = {}
    if debug:
        for nm, shp, dt in (("d_mod", [128, 6 * D], F32), ("d_hT", [128, 8 * 512], BF16), ("d_ycat", [128, 8 * 512], BF16),
                            ("d_lg", [128, 4 * 36], F32), ("d_pos1", [128, NB], I32), ("d_pos2", [128, NB], I32),
                            ("d_w1", [128, NB], F32), ("d_w2", [128, NB], F32), ("d_te", [128, TM], I32), ("d_pvT", [128, 4 * 35], F32)):
            dbg[nm] = nc.dram_tensor(nm, shp, dt, kind="ExternalOutput")

    import contextlib
    es = contextlib.ExitStack()
    with es:
        def sb(name, shape, dt=F32):
            t_ = es.enter_context(nc.sbuf_tensor(name, list(shape), dt))
            P.reg(name, t_)
            return t_

        def psum(name, shape, dt=F32):
            return es.enter_context(nc.psum_tensor(name, list(shape), dt))

        sems = {}
        for e in Prog.ENGS:
            sems[e] = es.enter_context(nc.semaphore("s_" + e))
            sems["dma_" + e] = [es.enter_context(nc.semaphore(f"d_{e}{k}")) for k in range(P.n_dma_sems)]

        identf = sb("identf", [128, 128])
        identb = sb("identb", [128, 128], BF16)
        onesf = sb("onesf", [128, 128])
        onesb = sb("onesb", [128, 128], BF16)
        trib = sb("trib", [128, 128], BF16)
        trif = sb("trif", [128, 128])
        bavg = sb("bavg", [128, 128], BF16)
        bavgf = sb("bavgf", [128, 128])
        inv16 = sb("inv16", [128, 16])
        iota_i = sb("iota_i", [128, 128], I32)
        iota_f = sb("iota_f", [128, 128])

        P.op("pool", lambda E: E.memset(identf[:], 0.0), w=["identf"])
        P.op("pool", lambda E: E.affine_select(out=identf[:], in_=identf[:], pattern=[[-1, 128]], compare_op=ALU.not_equal,
                                               fill=1.0, base=0, channel_multiplier=1), r=["identf"], w=["identf"])
        P.op("pool", lambda E: E.tensor_copy(out=identb[:], in_=identf[:]), r=["identf"], w=["identb"])
        P.op("pool", lambda E: E.memset(onesf[:], 1.0), w=["onesf"])
        P.op("pool", lambda E: E.memset(onesb[:], 1.0), w=["onesb"])
        P.op("pool", lambda E: E.memset(trif[:], 1.0), w=["trif"])
        P.op("pool", lambda E: E.affine_select(out=trif[:], in_=trif[:], pattern=[[1, 128]], compare_op=ALU.is_gt,
                                               fill=0.0, base=0, channel_multiplier=-1), r=["trif"], w=["trif"])
        P.op("pool", lambda E: E.tensor_copy(out=trib[:], in_=trif[:]), r=["trif"], w=["trib"])
        P.op("pool", lambda E: E.memset(bavgf[:], 0.0), w=["bavgf"])
        P.op("pool", lambda E: E.memset(bavgf[0:64, 0:64], 1.0 / 64), r=["bavgf"], w=["bavgf"])
        P.op("pool", lambda E: E.memset(bavgf[64:128, 64:128], 1.0 / 64), r=["bavgf"], w=["bavgf"])
        P.op("pool", lambda E: E.tensor_copy(out=bavg[:], in_=bavgf[:]), r=["bavgf"], w=["bavg"])
        P.op("pool", lambda E: E.iota(iota_i[:], pattern=[[1, 128]], base=0, channel_multiplier=0), w=["iota_i"])
        P.op("pool", lambda E: E.tensor_copy(out=iota_f[:], in_=iota_i[:]), r=["iota_i"], w=["iota_f"])
        epsr = sb("epsr", [128, 1])
        epsl = sb("epsl", [128, 1])
        P.op("pool", lambda E: E.memset(epsr[:], D * RMS_EPS), w=["epsr"])
        P.op("pool", lambda E: E.memset(epsl[:], LN_EPS), w=["epsl"])
        pidx_i = sb("pidx_i", [128, 1], I32)
        pidx_f = sb("pidx_f", [128, 1])
        P.op("pool", lambda E: E.iota(pidx_i[:], pattern=[[0, 1]], base=0, channel_multiplier=1), w=["pidx_i"])
        P.op("pool", lambda E: E.tensor_copy(out=pidx_f[:], in_=pidx_i[:]), r=["pidx_i"], w=["pidx_f"])
        P.op("dve", lambda E: E.tensor_scalar(out=inv16[:], in0=iota_f[:, 0:16], scalar1=1.0, scalar2=None, op0=ALU.add),
             r=["iota_f"], w=["inv16"])
        P.op("dve", lambda E: E.reciprocal(out=inv16[:], in_=inv16[:]), r=["inv16"], w=["inv16"])

        bcd = {}

        def mk_bc(E):
            reg = E.alloc_register("bcreg")
            inst = E.reg_mov(reg, NE * 128 - 1)
            bcd["v"] = E.snap(reg, donate=True)
            return E.memset(epsl[:], LN_EPS)
        P.op("pool", mk_bc, w=["epsl"])
        NFB = 5
        psf = [psum(f"psf{i}", [128, 512]) for i in range(NFB)]
        psb = [psum(f"psb{i}", [128, 1024], BF16) for i in range(2)]
        psr = psum("psr", [128, 512])
        bank_ctr = [0, 0]

        def fbank():
            i = bank_ctr[0] % NFB
            bank_ctr[0] += 1
            return psf[i], f"psf{i}"

        def bbank():
            i = bank_ctr[1] % 2
            bank_ctr[1] += 1
            return psb[i], f"psb{i}"

        c_sb = sb("c_sb", [128, 8])
        cond = sb("cond", [128, 8])
        condbc = sb("condbc", [128, 8, 128])
        P.dma("sp", lambda E: E.dma_start(out=c_sb[:], in_=c_d.ap().rearrange("(c p) -> p c", p=128), allow_slow_non_contiguous=True),
              w=["c_sb"])
        P.op("act", lambda E: E.activation(out=cond[:], in_=c_sb[:], func=AF.Silu), r=["c_sb"], w=["cond"])
        for c in range(8):
            P.op("act", lambda E, c=c: E.activation(out=condbc[:, c, :], in_=onesf[:], func=AF.Copy, scale=cond[:, c:c + 1]),
                 r=["cond", "onesf"], w=["condbc"])

        mod = sb("mod", [128, 6 * D])
        oh1 = sb("oh1", [128, NB, NE], BF16)
        oh2 = sb("oh2", [128, NB, NE], BF16)
        Aall = sb("Aall", [128, NB, NE], BF16)
        w1 = sb("w1", [128, NB])
        w2 = sb("w2", [128, NB])
        pos1 = sb("pos1", [128, NB], I32)
        pos2 = sb("pos2", [128, NB], I32)
        te_i = sb("te_i", [128, TM], I32)
        lg_all = sb("lg_all", [128, NB, 36])

        B1, A1, G1, B2, A2, G2 = (mod[:, k * D:(k + 1) * D] for k in range(6))

        win = sb("win", [128, 8, 1536], BF16)
        wout = sb("wout", [128, 8, D], BF16)
        pw = sb("pw", [128, 4, 128], BF16)
        wr = sb("wr", [128, 8, 36])
        rbias = sb("rbias", [128, 4, 36])
        pv = sb("pv", [35, 512])
        pvT = sb("pvT", [128, 4, 35])
        diag = sb("diag", [128, CK, 4, 128], BF16)

        def prologue(l, n0, n1, do_scale, aw, ab, gbc):
            for nn in range(n0, n1):
                k = nn % len(aw)
                blk = (nn * 256) // 512
                P.dma("sp", lambda E, nn=nn, k=k: E.dma_start(
                    out=aw[k][:], in_=ada_w[l, :, nn * 256:(nn + 1) * 256].rearrange("(c p) n -> p c n", p=128)), w=[f"aw{k}"])
                P.dma("sp", lambda E, nn=nn, k=k: E.dma_start(
                    out=ab[k][:], in_=ada_b[l, nn * 256:(nn + 1) * 256].partition_broadcast(128)), w=[f"ab{k}"])
                pb, pbn = fbank()
                for c in range(8):
                    P.op("pe", lambda E, c=c, pb=pb, k=k: E.matmul(pb[:, 0:256], lhsT=condbc[:, c, :], rhs=aw[k][:, c, :],
                                                                  start=(c == 0), stop=(c == 7)), r=["condbc", f"aw{k}"], w=[pbn])
                P.op("dve", lambda E, nn=nn, pb=pb, k=k: E.tensor_tensor(out=mod[:, nn * 256:(nn + 1) * 256], in0=pb[:, 0:256], in1=ab[k][:],
                                                                         op=ALU.add), r=[f"ab{k}"], w=[pbn, f"mod{blk}"])
            if do_scale:
                for (gsrc, Aap, mr) in ((norm1_g, A1, ["mod2", "mod3"]), (norm2_g, A2, ["mod8", "mod9"])):
                    P.dma("sp", lambda E, gsrc=gsrc: E.dma_start(out=gbc[:], in_=gsrc[l, :].partition_broadcast(128)), w=["gbc"])
                    P.op("act", lambda E: E.mul(out=gbc[:], in_=gbc[:], mul=32.0), r=["gbc"], w=["gbc"])
                    P.op("dve", lambda E, Aap=Aap: E.scalar_tensor_tensor(out=Aap, in0=Aap, scalar=1.0, in1=gbc[:], op0=ALU.add,
                                                                          op1=ALU.mult), r=["gbc"] + mr, w=mr)

        def load_weights(l):
            for c in range(8):
                P.dma("pool", lambda E, c=c: E.dma_start(out=win[:, c, :], in_=w_in[l, c * 128:(c + 1) * 128, :]), w=["win"])
            for c in range(8):
                P.dma("pool", lambda E, c=c: E.dma_start(out=wout[:, c, :], in_=w_out[l, c * 128:(c + 1) * 128, :]), w=["wout"])
            P.dma("pool", lambda E: E.dma_start(out=pw[:], in_=pool_w[l].rearrange("g c d -> c g d")), w=["pw"])
            P.dma("sp", lambda E: E.dma_start(out=wr[:], in_=rw[l].rearrange("(c p) g -> p c g", p=128)), w=["wr"])
            for j in range(4):
                P.dma("sp", lambda E, j=j: E.dma_start(out=rbias[:, j, :], in_=rb[l, :].partition_broadcast(128)), w=["rbias"])
            P.dma("sp", lambda E: E.dma_start(out=pv[:], in_=pvec[l]), w=["pv"])
            for c in range(4):
                pb, pbn = fbank()
                P.op("pe", lambda E, c=c, pb=pb: E.transpose(out=pb[:, 0:35], in_=pv[:, c * 128:(c + 1) * 128], identity=identf[0:35, 0:35]),
                     r=["pv", "identf"], w=[pbn])
                P.op("act", lambda E, c=c, pb=pb: E.activation(out=pvT[:, c, :], in_=pb[:, 0:35], func=AF.Copy), w=[pbn, "pvT"])
            for k in range(CK):
                for c in range(4):
                    eng = "dve" if (k * 4 + c) % 2 == 0 else "pool"
                    P.op(eng, lambda E, k=k, c=c: E.tensor_scalar(out=diag[:, k, c, :], in0=identf[:], scalar1=pvT[:, c, k:k + 1],
                                                                  scalar2=None, op0=ALU.mult),
                         r=["identf", "pvT"], w=[f"diag{k}_{c}"])

        def pro_bufs(sc, l, nbuf=2):
            def sbl(name, shape, dt=F32):
                t_ = sc.enter_context(nc.sbuf_tensor(f"{name}_{l}", list(shape), dt))
                P.reg(name, t_)
                return t_
            aw = [sbl(f"aw{k}", [128, 8, 256]) for k in range(nbuf)]
            ab = [sbl(f"ab{k}", [128, 256]) for k in range(nbuf)]
            gbc = sbl("gbc", [128, D])
            return aw, ab, gbc

        with contextlib.ExitStack() as sc0:
            aw_, ab_, gbc_ = pro_bufs(sc0, "p0")
            prologue(0, 0, 24, True, aw_, ab_, gbc_)
        load_weights(0)

        def layer(l):
            x_src = x_d if l == 0 else xs2
            last = (l == DEPTH_ - 1)
            with contextlib.ExitStack() as sc:
                def sbl(name, shape, dt=F32, sc=sc):
                    t_ = sc.enter_context(nc.sbuf_tensor(f"{name}_{l}", list(shape), dt))
                    P.reg(name, t_)
                    return t_
                xt = [sbl(f"xt{k}", [128, 4, D]) for k in range(1)]
                ss = sbl("ss", [128, 8])
                rr = sbl("rr", [128, 8])
                tmpA = [sbl(f"tmpA{k}", [128, D]) for k in range(2)]
                hb = sbl("hb", [128, 4, D], BF16)
                hT = sbl("hT", [128, 8, 512], BF16)
                u = sbl("u", [128, 4, 528])
                sA = sbl("sA", [128, 528])
                sB = sbl("sB", [128, 528])
                pooled = sbl("pooled", [128, 4, 512], BF16)
                ycat = sbl("ycat", [128, 8, 512], BF16)
                vbuf = sbl("vbuf", [128, 4, 544], BF16)
                sig = [sbl(f"sig{k}", [128, 512]) for k in range(1)]
                ybf = [sbl(f"ybf{k}", [128, 512], BF16) for k in range(2)]
                dd = [sbl(f"dd{k}", [128, 512]) for k in range(1)]
                sq = [sbl(f"sq{k}", [128, 512], BF16) for k in range(1)]
                rs = [sbl(f"rs{k}", [128, 512]) for k in range(1)]
                yn = dd
                tmpO = [sbl(f"tmpO{k}", [128, 512]) for k in range(2)]
                h2b = hb
                h2T = [sbl(f"h2T{k}", [128, 8, 128]) for k in range(1)]

                P.op("pool", lambda E: E.memset(u[:], 0.0), w=["u0", "u1", "u2", "u3"])
                P.op("pool", lambda E: E.memset(vbuf[:], 0.0), w=["vbuf0", "vbuf1", "vbuf2", "vbuf3"])

                def mtile(i):
                    X = xt[0]
                    Xn = "xt0"
                    rows = slice(i * 512, (i + 1) * 512)
                    P.dma("sp", lambda E, X=X, rows=rows: E.dma_start(out=X[:], in_=x_src[rows, :].rearrange("(j p) d -> p j d", p=128)),
                          r=[f"xs2_{4 * i + q}" for q in range(4)], w=[Xn])
                    P.op("dve", lambda E: E.memset(ss[:], 0.0), w=["ss"])
                    for j in range(4):
                        P.op("act", lambda E, j=j, X=X: E.activation(out=hb[:, j, :], in_=X[:, j, :], func=AF.Square, accum_out=ss[:, j:j + 1]),
                             r=[Xn], w=[f"hb{j}", "ss"])
                    P.op("act", lambda E: E.activation(out=rr[:, 0:4], in_=ss[:, 0:4], func=AF.Sqrt, bias=epsr[:, 0:1]), r=["ss", "epsr"], w=["rr"])
                    P.op("dve", lambda E: E.reciprocal(out=rr[:, 0:4], in_=rr[:, 0:4]), r=["rr"], w=["rr"])
                    for j in range(4):
                        T = tmpA[j % 2]
                        Tn = f"tmpA{j % 2}"
                        P.op("dve", lambda E, j=j, X=X, T=T: E.scalar_tensor_tensor(out=T[:], in0=X[:, j, :], scalar=rr[:, j:j + 1], in1=A1,
                                                                                    op0=ALU.mult, op1=ALU.mult),
                             r=[Xn, "rr", "mod2", "mod3"], w=[Tn])
                        P.op("pool", lambda E, j=j, T=T: E.tensor_tensor(out=hb[:, j, :], in0=T[:], in1=B1, op=ALU.add),
                             r=[Tn, "mod0", "mod1"], w=[f"hb{j}"])
                    for j in range(4):
                        pb, pbn = bbank()
                        for c in range(8):
                            P.op("pe", lambda E, j=j, c=c, pb=pb: E.transpose(out=pb[:, c * 128:(c + 1) * 128], in_=hb[:, j, c * 128:(c + 1) * 128],
                                                                             identity=identb[:]),
                                 r=[f"hb{j}", "identb"], w=[pbn])
                        eng = "act" if j % 2 == 0 else "dve"
                        if eng == "act":
                            P.op("act", lambda E, j=j, pb=pb: E.activation(out=hT[:, :, j * 128:(j + 1) * 128],
                                                                           in_=pb[:].rearrange("p (c t) -> p c t", c=8), func=AF.Copy),
                                 w=[pbn, f"hT{j}"])
                        else:
                            P.op("dve", lambda E, j=j, pb=pb: E.tensor_copy(out=hT[:, :, j * 128:(j + 1) * 128],
                                                                            in_=pb[:].rearrange("p (c t) -> p c t", c=8)),
                                 w=[pbn, f"hT{j}"])
                    hTr = ["hT0", "hT1", "hT2", "hT3"]
                    if debug and l == 0 and i == 0:
                        P.dma("sp", lambda E: E.dma_start(out=dbg["d_hT"].ap(), in_=hT[:].rearrange("p c t -> p (c t)")), r=hTr, w=["dbg1"])
                        P.dma("sp", lambda E: E.dma_start(out=dbg["d_mod"].ap(), in_=mod[:]), r=[f"mod{q}" for q in range(12)], w=["dbg2"])
                        P.dma("sp", lambda E: E.dma_start(out=dbg["d_pvT"].ap(), in_=pvT[:].rearrange("p c t -> p (c t)")), r=["pvT"], w=["dbg2b"])

                    def win_mm(oc):
                        pb, pbn = fbank()
                        for c in range(8):
                            P.op("pe", lambda E, c=c, pb=pb, oc=oc: E.matmul(pb[:], lhsT=win[:, c, oc * 128:(oc + 1) * 128], rhs=hT[:, c, :],
                                                                           start=(c == 0), stop=(c == 7)),
                                 r=["win"] + hTr, w=[pbn])
                        return pb, pbn

                    def pU(g):
                        pb, pbn = win_mm(g)
                        P.op("act", lambda E, pb=pb: E.activation(out=u[:, g, 16:528], in_=pb[:], func=AF.Copy), w=[pbn, f"u{g}"])

                    def pD(g):
                        wdw = POOL_WINDOWS[g]
                        un = f"u{g}"
                        step = 1
                        bufs = [sA, sB]
                        bi = 0
                        cur = None
                        lo = 0
                        while step < wdw:
                            dst = bufs[bi]
                            dn = "sA" if bi == 0 else "sB"
                            nlo = lo + step
                            if cur is None:
                                P.op("pool", lambda E, dst=dst, nlo=nlo, step=step: E.tensor_tensor(
                                    out=dst[:, nlo:528], in0=u[:, g, nlo:528], in1=u[:, g, nlo - step:528 - step], op=ALU.add),
                                    r=[un], w=[dn])
                            else:
                                cs, cn = cur
                                P.op("pool", lambda E, dst=dst, cs=cs, nlo=nlo, step=step: E.tensor_tensor(
                                    out=dst[:, nlo:528], in0=cs[:, nlo:528], in1=cs[:, nlo - step:528 - step], op=ALU.add),
                                    r=[cn], w=[dn])
                            cur = (dst, dn)
                            lo = nlo
                            step *= 2
                            bi ^= 1
                        cs, cn = cur
                        P.op("dve", lambda E, cs=cs, wdw=wdw: E.scalar_tensor_tensor(
                            out=pooled[:, g, :], in0=cs[:, 16:528], scalar=1.0 / wdw, in1=u[:, g, 16:528], op0=ALU.mult, op1=ALU.subtract),
                            r=[cn, un], w=[f"pooled{g}"])
                        if i == 0:
                            nfix = wdw - 1
                            P.op("pool", lambda E, cs=cs, nfix=nfix: E.tensor_tensor(out=cs[:, 16:16 + nfix], in0=cs[:, 16:16 + nfix],
                                                                                    in1=inv16[:, 0:nfix], op=ALU.mult),
                                 r=[cn, "inv16", f"pooled{g}"], w=[cn])
                            P.op("pool", lambda E, cs=cs, nfix=nfix: E.tensor_tensor(out=pooled[:, g, 0:nfix], in0=cs[:, 16:16 + nfix],
                                                                                    in1=u[:, g, 16:16 + nfix], op=ALU.subtract),
                                 r=[cn, un], w=[f"pooled{g}"])
                        P.op("pool", lambda E: E.tensor_copy(out=u[:, g, 0:16], in_=u[:, g, 512:528]), r=[un], w=[un])

                    def pPW(g):
                        pb2, pb2n = fbank()
                        P.op("pe", lambda E, pb2=pb2: E.matmul(pb2[:], lhsT=pw[:, g, :], rhs=pooled[:, g, :], start=True, stop=True),
                             r=["pw", f"pooled{g}"], w=[pb2n])
                        P.op("act", lambda E, pb2=pb2: E.activation(out=ycat[:, g, :], in_=pb2[:], func=AF.Copy, scale=pvT[:, g, 34:35]),
                             r=["pvT"], w=[pb2n, f"ycat{g}"])

                    def cF(c):
                        kb = c % 2
                        vn = f"vbuf{c}"
                        pa, pan = win_mm(4 + c)
                        pg, pgn = win_mm(8 + c)
                        P.op("act", lambda E, pg=pg: E.activation(out=sig[0][:], in_=pg[:], func=AF.Sigmoid), w=[pgn, "sig0"])
                        P.op("dve", lambda E, pa=pa: E.tensor_tensor(out=vbuf[:, c, 32:544], in0=pa[:], in1=sig[0][:], op=ALU.mult),
                             r=["sig0"], w=[pan, vn])
                        py, pyn = fbank()
                        for k in range(CK):
                            P.op("pe", lambda E, k=k, py=py: E.matmul(py[:], lhsT=diag[:, k, c, :], rhs=vbuf[:, c, 2 + k:2 + k + 512],
                                                                    start=(k == 0), stop=(k == CK - 1)),
                                 r=[f"diag{k}_{c}", vn], w=[pyn])
                        P.op("pool", lambda E: E.tensor_copy(out=vbuf[:, c, 2:32], in_=vbuf[:, c, 514:544]), r=[vn], w=[vn])
                        P.op("act", lambda E, py=py: E.activation(out=ybf[kb][:], in_=py[:], func=AF.Identity, bias=pvT[:, c, 31:32]),
                             r=["pvT"], w=[pyn, f"ybf{kb}"])

                    def cB1(c):
                        kb = c % 2
                        pm, pmn = fbank()
                        P.op("pe", lambda E, pm=pm: E.matmul(pm[:], lhsT=bavg[:], rhs=ybf[kb][:], start=True, stop=True),
                             r=["bavg", f"ybf{kb}"], w=[pmn])
                        P.op("dve", lambda E, pm=pm: E.tensor_tensor(out=dd[0][:], in0=ybf[kb][:], in1=pm[:], op=ALU.subtract),
                             r=[f"ybf{kb}"], w=[pmn, "dd0"])
                        P.op("act", lambda E: E.activation(out=sq[0][:], in_=dd[0][:], func=AF.Square), r=["dd0"], w=["sq0"])

                    def cB2(c):
                        pvv, pvn = fbank()
                        P.op("pe", lambda E, pvv=pvv: E.matmul(pvv[:], lhsT=bavg[:], rhs=sq[0][:], start=True, stop=True),
                             r=["bavg", "sq0"], w=[pvn])
                        P.op("act", lambda E, pvv=pvv: E.activation(out=rs[0][:], in_=pvv[:], func=AF.Sqrt, bias=epsl[:, 0:1]),
                             r=["epsl"], w=[pvn, "rs0"])
                        P.op("dve", lambda E: E.reciprocal(out=rs[0][:], in_=rs[0][:]), r=["rs0"], w=["rs0"])
                        P.op("pool", lambda E: E.tensor_tensor(out=dd[0][:], in0=dd[0][:], in1=rs[0][:], op=ALU.mult),
                             r=["dd0", "rs0"], w=["dd0"])
                        P.op("act", lambda E: E.activation(out=ycat[:, 4 + c, :], in_=dd[0][:], func=AF.Silu, bias=pvT[:, c, 33:34],
                                                           scale=pvT[:, c, 32:33]),
                             r=["dd0", "pvT"], w=[f"ycat{4 + c}"])

                    for g_ in range(4):
                        pU(g_)
                    cF(0); pD(0); cF(1); pD(1); cB1(0); cF(2); pD(2); cB2(0); cB1(1); cF(3); pD(3); cB2(1); cB1(2)
                    pPW(0); pPW(1); cB2(2); cB1(3); pPW(2); pPW(3); cB2(3)
                    ycr = [f"ycat{k}" for k in range(8)]
                    if debug and l == 0 and i == 0:
                        P.dma("sp", lambda E: E.dma_start(out=dbg["d_ycat"].ap(), in_=ycat[:].rearrange("p c t -> p (c t)")), r=ycr, w=["dbg3"])
                    for j in range(4):
                        for dh in range(2):
                            po, pon = fbank()
                            for k in range(8):
                                P.op("pe", lambda E, j=j, dh=dh, k=k, po=po: E.matmul(po[:], lhsT=ycat[:, k, j * 128:(j + 1) * 128],
                                                                                  rhs=wout[:, k, dh * 512:(dh + 1) * 512], start=(k == 0), stop=(k == 7)),
                                     r=ycr + ["wout"], w=[pon])
                            kk = (j * 2 + dh) % 2
                            P.op("dve", lambda E, dh=dh, po=po, kk=kk: E.tensor_tensor(out=tmpO[kk][:], in0=po[:], in1=G1[:, dh * 512:(dh + 1) * 512],
                                                                                      op=ALU.mult), r=[f"mod{4 + dh}"], w=[pon, f"tmpO{kk}"])
                            P.op("pool", lambda E, j=j, dh=dh, X=X, kk=kk: E.tensor_tensor(out=X[:, j, dh * 512:(dh + 1) * 512], in0=tmpO[kk][:],
                                                                                         in1=X[:, j, dh * 512:(dh + 1) * 512], op=ALU.add),
                                 r=[f"tmpO{kk}", Xn], w=[Xn])
                    P.dma("act", lambda E, X=X, rows=rows: E.dma_start(out=xs1[rows, :].rearrange("(j p) d -> p j d", p=128), in_=X[:]),
                          r=[Xn], w=[f"xs1_{i}"])
                    for j in range(4):
                        P.op("act", lambda E, j=j, X=X: E.activation(out=hb[:, j, :], in_=X[:, j, :], func=AF.Square, accum_out=ss[:, 4 + j:5 + j]),
                             r=[Xn], w=[f"hb{j}", "ss"])
                    P.op("act", lambda E: E.activation(out=rr[:, 4:8], in_=ss[:, 4:8], func=AF.Sqrt, bias=epsr[:, 0:1]), r=["ss", "epsr"], w=["rr"])
                    P.op("dve", lambda E: E.reciprocal(out=rr[:, 4:8], in_=rr[:, 4:8]), r=["rr"], w=["rr"])
                    pr, prn = psr, "psr"
                    for j in range(4):
                        T = tmpA[j % 2]
                        Tn = f"tmpA{j % 2}"
                        H = T
                        Hn = Tn
                        P.op("dve", lambda E, j=j, X=X, T=T: E.scalar_tensor_tensor(out=T[:], in0=X[:, j, :], scalar=rr[:, 4 + j:5 + j], in1=A2,
                                                                                    op0=ALU.mult, op1=ALU.mult),
                             r=[Xn, "rr", "mod8", "mod9"], w=[Tn])
                        P.op("pool", lambda E, T=T, H=H: E.tensor_tensor(out=H[:], in0=T[:], in1=B2, op=ALU.add), r=[Tn, "mod6", "mod7"], w=[Hn])
                        P.op("act", lambda E, j=j, H=H: E.activation(out=h2b[:, j, :], in_=H[:], func=AF.Copy), r=[Hn], w=[f"hb{j}"])
                        HT = h2T[0]
                        HTn = "h2T0"
                        for half in range(2):
                            pt, ptn = fbank()
                            for cc in range(4):
                                c = half * 4 + cc
                                P.op("pe", lambda E, c=c, cc=cc, H=H, pt=pt: E.transpose(out=pt[:, cc * 128:(cc + 1) * 128],
                                                                                       in_=H[:, c * 128:(c + 1) * 128], identity=identf[:]),
                                     r=[Hn, "identf"], w=[ptn])
                            eng = "act" if half == 0 else "dve"
                            if eng == "act":
                                P.op("act", lambda E, half=half, HT=HT, pt=pt: E.activation(out=HT[:, half * 4:(half + 1) * 4, :],
                                                                                        in_=pt[:].rearrange("p (c t) -> p c t", c=4), func=AF.Copy),
                                     w=[ptn, HTn])
                            else:
                                P.op("dve", lambda E, half=half, HT=HT, pt=pt: E.tensor_copy(out=HT[:, half * 4:(half + 1) * 4, :],
                                                                                         in_=pt[:].rearrange("p (c t) -> p c t", c=4)),
                                     w=[ptn, HTn])
                        for c in range(8):
                            P.op("pe", lambda E, j=j, c=c, HT=HT, pr=pr: E.matmul(pr[:, j * 36:(j + 1) * 36], lhsT=HT[:, c, :], rhs=wr[:, c, :],
                                                                              start=(c == 0), stop=(c == 7)),
                                 r=[HTn, "wr"], w=[prn])
                    P.dma("act", lambda E, rows=rows: E.dma_start(out=h2s[rows, :].rearrange("(j p) d -> p j d", p=128), in_=h2b[:]), r=["hb0", "hb1", "hb2", "hb3"], w=[f"h2s_{i}"])
                    P.op("dve", lambda E, pr=pr: E.tensor_tensor(out=lg_all[:, 4 * i:4 * i + 4, :], in0=pr[:, 0:144].rearrange("p (j e) -> p j e", j=4),
                                                                 in1=rbias[:], op=ALU.add), r=["rbias"], w=[prn, "lg_all"])
                    if debug and l == 0 and i == 0:
                        P.dma("sp", lambda E: E.dma_start(out=dbg["d_lg"].ap(), in_=lg_all[:, 0:4, :].rearrange("p c t -> p (c t)")), r=["lg_all"], w=["dbg4"])
                for _i in range(NT):
                    mtile(_i)

            with contextlib.ExitStack() as sc:
                def sbl(name, shape, dt=F32, sc=sc):
                    t_ = sc.enter_context(nc.sbuf_tensor(f"{name}_{l}", list(shape), dt))
                    P.reg(name, t_)
                    return t_
                NC_ = NB * NE
                within = sbl("within", [128, NB, NE])
                tot = [sbl(f"tot{k}", [128, NB, NE]) for k in range(2)]
                tot0 = sbl("totz", [128, NB, NE])
                cmpb = sbl("cmpb", [128, NE, NB2])
                ntile = [sbl(f"ntile{k}", [128, NE]) for k in range(2)]
                nt0 = sbl("ntz", [128, NE])
                basee = sbl("basee", [128, NE])
                tend = sbl("tend", [128, NE])
                posall = sbl("posall", [128, NB, NE])
                ptmp = sbl("ptmp", [128, NB, NE])
                posf = sbl("posf", [128, NB])
                thr = sbl("thr", [128, NB2])
                cmpt = sbl("cmpt", [128, TM, NE])
                tef = sbl("tef", [128, TM])
                gmax = sbl("gmax", [128, NB])
                gd = sbl("gd", [128, NB, 4])
                gsum = sbl("gsum", [128, NB])
                pgrp = sbl("pgrp", [128, NB])
                ohg = sbl("ohg", [128, NB, 4])
                em = sbl("em", [128, NB, NE])
                em2 = sbl("em2", [128, NB, NE])
                m1 = sbl("m1", [128, NB])
                m2 = sbl("m2", [128, NB])
                d12 = sbl("d12", [128, NB])
                gl = lg_all[:, :, 0:4]
                el = lg_all[:, :, 4:36]
                P.op("dve", lambda E: E.tensor_reduce(out=gmax[:], in_=gl, axis=AX.X, op=ALU.max), r=["lg_all"], w=["gmax"])
                P.op("dve", lambda E: E.tensor_tensor(out=gd[:], in0=gl, in1=gmax[:].unsqueeze(2).to_broadcast([128, NB, 4]), op=ALU.subtract),
                     r=["lg_all", "gmax"], w=["gd"])
                P.op("dve", lambda E: E.tensor_tensor(out=ohg[:], in0=gl, in1=gmax[:].unsqueeze(2).to_broadcast([128, NB, 4]), op=ALU.is_equal),
                     r=["lg_all", "gmax"], w=["ohg"])
                P.op("act", lambda E: E.activation(out=gd[:], in_=gd[:], func=AF.Exp), r=["gd"], w=["gd"])
                P.op("dve", lambda E: E.tensor_reduce(out=gsum[:], in_=gd[:], axis=AX.X, op=ALU.add), r=["gd"], w=["gsum"])
                P.op("dve", lambda E: E.reciprocal(out=pgrp[:], in_=gsum[:]), r=["gsum"], w=["pgrp"])
                P.op("dve", lambda E: E.tensor_scalar(out=ohg[:], in0=ohg[:], scalar1=-1.0, scalar2=1e30, op0=ALU.add, op1=ALU.mult),
                     r=["ohg"], w=["ohg"])
                P.op("dve", lambda E: E.tensor_tensor(out=em[:].rearrange("p j (g e) -> p j g e", g=4),
                                                      in0=el.rearrange("p j (g e) -> p j g e", g=4),
                                                      in1=ohg[:].unsqueeze(3).to_broadcast([128, NB, 4, 8]), op=ALU.add),
                     r=["lg_all", "ohg"], w=["em"])
                P.op("dve", lambda E: E.tensor_reduce(out=m1[:], in_=em[:], axis=AX.X, op=ALU.max), r=["em"], w=["m1"])
                P.op("dve", lambda E: E.tensor_tensor(out=oh1[:], in0=em[:], in1=m1[:].unsqueeze(2).to_broadcast([128, NB, NE]),
                                                      op=ALU.is_equal), r=["em", "m1"], w=["oh1"])
                P.op("dve", lambda E: E.scalar_tensor_tensor(out=em2[:], in0=oh1[:], scalar=-1e30, in1=em[:], op0=ALU.mult,
                                                             op1=ALU.add), r=["oh1", "em"], w=["em2"])
                P.op("dve", lambda E: E.tensor_reduce(out=m2[:], in_=em2[:], axis=AX.X, op=ALU.max), r=["em2"], w=["m2"])
                P.op("dve", lambda E: E.tensor_tensor(out=oh2[:], in0=em2[:], in1=m2[:].unsqueeze(2).to_broadcast([128, NB, NE]),
                                                      op=ALU.is_equal), r=["em2", "m2"], w=["oh2"])
                P.op("dve", lambda E: E.tensor_tensor(out=d12[:], in0=m1[:], in1=m2[:], op=ALU.subtract), r=["m1", "m2"], w=["d12"])
                P.op("act", lambda E: E.activation(out=d12[:], in_=d12[:], func=AF.Sigmoid), r=["d12"], w=["d12"])
                P.op("dve", lambda E: E.tensor_tensor(out=w1[:], in0=d12[:], in1=pgrp[:], op=ALU.mult), r=["d12", "pgrp"], w=["w1"])
                P.op("dve", lambda E: E.tensor_tensor(out=w2[:], in0=pgrp[:], in1=w1[:], op=ALU.subtract), r=["pgrp", "w1"], w=["w2"])
                P.op("dve", lambda E: E.tensor_tensor(out=Aall[:], in0=oh1[:], in1=oh2[:], op=ALU.add), r=["oh1", "oh2"], w=["Aall"])
                Af = Aall[:].rearrange("p b e -> p (b e)")
                for h0 in range(0, NC_, 512):
                    wd_ = min(512, NC_ - h0)
                    pbw, pbwn = fbank()
                    P.op("pe", lambda E, h0=h0, wd_=wd_, pbw=pbw: E.matmul(pbw[:, 0:wd_], lhsT=trib[:], rhs=Af[:, h0:h0 + wd_], start=True, stop=True),
                         r=["trib", "Aall"], w=[pbwn])
                    P.op("act", lambda E, h0=h0, wd_=wd_, pbw=pbw: E.activation(out=within[:].rearrange("p b e -> p (b e)")[:, h0:h0 + wd_],
                                                                           in_=pbw[:, 0:wd_], func=AF.Copy), w=[pbwn, "within"])
                    pbt, pbtn = fbank()
                    P.op("pe", lambda E, h0=h0, wd_=wd_, pbt=pbt: E.matmul(pbt[:, 0:wd_], lhsT=onesb[:], rhs=Af[:, h0:h0 + wd_], start=True, stop=True),
                         r=["onesb", "Aall"], w=[pbtn])
                    P.op("dve", lambda E, h0=h0, wd_=wd_, pbt=pbt: E.tensor_copy(out=tot0[:].rearrange("p b e -> p (b e)")[:, h0:h0 + wd_],
                                                                            in_=pbt[:, 0:wd_]), w=[pbtn, "totz"])
                cur, curn = tot0, "totz"
                step = 1
                bi = 0
                while step < NB:
                    dst, dn = tot[bi], f"tot{bi}"
                    P.op("dve", lambda E, dst=dst, cur=cur, step=step: E.tensor_copy(out=dst[:, 0:step, :], in_=cur[:, 0:step, :]), r=[curn], w=[dn])
                    P.op("dve", lambda E, dst=dst, cur=cur, step=step: E.tensor_tensor(out=dst[:, step:NB, :], in0=cur[:, step:NB, :],
                                                                                   in1=cur[:, 0:NB - step, :], op=ALU.add), r=[curn], w=[dn])
                    cur, curn = dst, dn
                    step *= 2
                    bi ^= 1
                incl, incln = cur, curn
                P.op("dve", lambda E: E.tensor_tensor(out=posall[:], in0=within[:], in1=incl[:], op=ALU.add), r=["within", incln], w=["posall"])
                P.op("dve", lambda E: E.tensor_tensor(out=posall[:], in0=posall[:], in1=tot0[:], op=ALU.subtract), r=["posall", "totz"], w=["posall"])
                cnt = incl[:, NB - 1, :]
                P.op("dve", lambda E: E.tensor_scalar(out=thr[:], in0=iota_f[:, 0:NB2], scalar1=float(SL), scalar2=None, op0=ALU.mult),
                     r=["iota_f"], w=["thr"])
                P.op("dve", lambda E: E.tensor_tensor(out=cmpb[:], in0=cnt.unsqueeze(2).to_broadcast([128, NE, NB2]),
                                                      in1=thr[:].unsqueeze(1).to_broadcast([128, NE, NB2]), op=ALU.is_gt),
                     r=[incln, "thr"], w=["cmpb"])
                P.op("dve", lambda E: E.tensor_reduce(out=nt0[:], in_=cmpb[:], axis=AX.X, op=ALU.add), r=["cmpb"], w=["ntz"])
                cur, curn = nt0, "ntz"
                step = 1
                bi = 0
                while step < NE:
                    dst, dn = ntile[bi], f"ntile{bi}"
                    P.op("dve", lambda E, dst=dst, cur=cur, step=step: E.tensor_copy(out=dst[:, 0:step], in_=cur[:, 0:step]), r=[curn], w=[dn])
                    P.op("dve", lambda E, dst=dst, cur=cur, step=step: E.tensor_tensor(out=dst[:, step:NE], in0=cur[:, step:NE],
                                                                                   in1=cur[:, 0:NE - step], op=ALU.add), r=[curn], w=[dn])
                    cur, curn = dst, dn
                    step *= 2
                    bi ^= 1
                P.op("dve", lambda E, cur=cur: E.tensor_copy(out=tend[:], in_=cur[:]), r=[curn], w=["tend"])
                P.op("dve", lambda E: E.tensor_tensor(out=basee[:], in0=tend[:], in1=nt0[:], op=ALU.subtract), r=["tend", "ntz"], w=["basee"])
                P.op("dve", lambda E: E.tensor_scalar(out=basee[:], in0=basee[:], scalar1=float(SL), scalar2=None, op0=ALU.mult), r=["basee"], w=["basee"])
                P.op("dve", lambda E: E.tensor_tensor(out=posall[:], in0=posall[:], in1=basee[:].unsqueeze(1).to_broadcast([128, NB, NE]), op=ALU.add),
                     r=["posall", "basee"], w=["posall"])
                for (ohk, posk, nm) in ((oh1, pos1, "pos1"), (oh2, pos2, "pos2")):
                    P.op("dve", lambda E, ohk=ohk: E.tensor_tensor(out=ptmp[:], in0=ohk[:], in1=posall[:], op=ALU.mult),
                         r=["oh1", "oh2", "posall"], w=["ptmp"])
                    P.op("dve", lambda E: E.tensor_reduce(out=posf[:], in_=ptmp[:], axis=AX.X, op=ALU.add), r=["ptmp"], w=["posf"])
                    P.op("dve", lambda E, posk=posk: E.tensor_copy(out=posk[:], in_=posf[:]), r=["posf"], w=[nm])
                for j0 in range(0, TM, 128):
                    jn = min(128, TM - j0)
                    P.op("dve", lambda E, j0=j0, jn=jn: E.tensor_scalar(out=tef[:, j0:j0 + jn], in0=iota_f[:, 0:jn], scalar1=float(j0), scalar2=None,
                                                                       op0=ALU.add), r=["iota_f"], w=["tef"])
                P.op("dve", lambda E: E.tensor_tensor(out=cmpt[:], in0=tend[:].unsqueeze(1).to_broadcast([128, TM, NE]),
                                                      in1=tef[:].unsqueeze(2).to_broadcast([128, TM, NE]), op=ALU.is_le),
                     r=["tend", "tef"], w=["cmpt"])
                P.op("dve", lambda E: E.tensor_reduce(out=tef[:], in_=cmpt[:], axis=AX.X, op=ALU.add), r=["cmpt"], w=["tef"])
                P.op("dve", lambda E: E.tensor_scalar(out=tef[:], in0=tef[:], scalar1=128.0, scalar2=pidx_f[:, 0:1], op0=ALU.mult, op1=ALU.add),
                     r=["tef", "pidx_f"], w=["tef"])
                P.op("dve", lambda E: E.tensor_copy(out=te_i[:], in_=tef[:]), r=["tef"], w=["te_i"])

            if debug and l == 0:
                for nm, t_, rn in (("d_pos1", pos1, "pos1"), ("d_pos2", pos2, "pos2"), ("d_w1", w1, "w1"), ("d_w2", w2, "w2"), ("d_te", te_i, "te_i")):
                    P.dma("sp", lambda E, nm=nm, t_=t_: E.dma_start(out=dbg[nm].ap(), in_=t_[:]), r=[rn], w=["dbg_" + nm])
            with contextlib.ExitStack() as sc:
                def sbl(name, shape, dt=F32, sc=sc):
                    t_ = sc.enter_context(nc.sbuf_tensor(f"{name}_{l}", list(shape), dt))
                    P.reg(name, t_)
                    return t_
                hrow = [sbl(f"hrow{k}", [128, D], BF16) for k in range(6)]
                def sblock(b):
                    k = b % 6
                    P.dma("sp", lambda E, b=b, k=k: E.dma_start(out=hrow[k][:], in_=h2s[b * 128:(b + 1) * 128, :]), r=[f"h2s_{b // 4}"], w=[f"hrow{k}"])
                    for (posk, nm, tg) in ((pos1, "pos1", "a"), (pos2, "pos2", "b")):
                        P.dma("pool", lambda E, b=b, k=k, posk=posk: E.indirect_dma_start(
                            out=hsort[:, :], out_offset=bass.IndirectOffsetOnAxis(ap=posk[:, b:b + 1], axis=0),
                            in_=hrow[k][:], in_offset=None), r=[f"hrow{k}", nm], w=[f"hsortw{tg}{b}"])
                for _b in range(NB):
                    sblock(_b)
            nxt = (l + 1 < DEPTH_)
            scP = contextlib.ExitStack()
            if nxt:
                load_weights(l + 1)
                aw_n, ab_n, gbc_n = pro_bufs(scP, f"p{l + 1}", nbuf=1)
                prologue(l + 1, 0, 20, True, aw_n, ab_n, gbc_n)
            with contextlib.ExitStack() as sc:
                def sbl(name, shape, dt=F32, sc=sc):
                    t_ = sc.enter_context(nc.sbuf_tensor(f"{name}_{l}", list(shape), dt))
                    P.reg(name, t_)
                    return t_
                NW = 3
                wgb = [sbl(f"wgb{k}", [128, 8, HID], BF16) for k in range(NW)]
                wub = [sbl(f"wub{k}", [128, 8, HID], BF16) for k in range(NW)]
                wdb = [sbl(f"wdb{k}", [128, 2, D], BF16) for k in range(NW)]
                stt = [sbl(f"stt{k}", [128, 2, D], BF16) for k in range(2)]
                hsT = [sbl(f"hsT{k}", [128, 8, SL], BF16) for k in range(2)]
                sgl = [sbl(f"sgl{k}", [128, 2 * SL]) for k in range(2)]
                hid = [sbl(f"hid{k}", [128, 2, SL], BF16) for k in range(2)]
                yo = [sbl(f"yo{k}", [128, 2, D]) for k in range(2)]
                allsc = [f"hsortw{tg}{b}" for b in range(NB) for tg in "ab"]

                def etile(j):
                    k = j % NW
                    k2 = j % 2
                    for (wb, wsrc, nm) in ((wgb, wg_d, "wgb"), (wub, wu_d, "wub"), (wdb, wd_d, "wdb")):
                        P.dma("pool", lambda E, wb=wb, wsrc=wsrc: E.indirect_dma_start(
                            out=wb[k][:].rearrange("p c h -> p (c h)"), out_offset=None, in_=wsrc[l][:, :],
                            in_offset=bass.IndirectOffsetOnAxis(ap=te_i[:, j:j + 1], axis=0), bounds_check=bcd["v"], oob_is_err=False),
                            r=["te_i"], w=[f"{nm}{k}"])
                    P.dma("sp", lambda E: E.dma_start(out=stt[k2][:], in_=hsort[j * SL:(j + 1) * SL, :].rearrange("(q p) d -> p q d", p=128)),
                          r=allsc, w=[f"stt{k2}"])
                    for q in range(2):
                        pb, pbn = bbank()
                        for c in range(8):
                            P.op("pe", lambda E, c=c, q=q, pb=pb: E.transpose(out=pb[:, c * 128:(c + 1) * 128], in_=stt[k2][:, q, c * 128:(c + 1) * 128],
                                                                           identity=identb[:]), r=[f"stt{k2}", "identb"], w=[pbn])
                        if q == 0:
                            P.op("act", lambda E, q=q, pb=pb: E.activation(out=hsT[k2][:, :, q * 128:(q + 1) * 128],
                                                                         in_=pb[:].rearrange("p (c t) -> p c t", c=8), func=AF.Copy), w=[pbn, f"hsT{k2}_{q}"])
                        else:
                            P.op("dve", lambda E, q=q, pb=pb: E.tensor_copy(out=hsT[k2][:, :, q * 128:(q + 1) * 128],
                                                                          in_=pb[:].rearrange("p (c t) -> p c t", c=8)), w=[pbn, f"hsT{k2}_{q}"])
                    hsr = [f"hsT{k2}_0", f"hsT{k2}_1"]
                    pg, pgn = fbank()
                    pu, pun = fbank()
                    for (W, Wn, pz, pzn) in ((wgb[k], f"wgb{k}", pg, pgn), (wub[k], f"wub{k}", pu, pun)):
                        for hc in range(2):
                            for c in range(8):
                                P.op("pe", lambda E, c=c, W=W, hc=hc, pz=pz: E.matmul(
                                    pz[:, hc * SL:(hc + 1) * SL], lhsT=W[:, c, hc * 128:(hc + 1) * 128], rhs=hsT[k2][:, c, :],
                                    start=(c == 0), stop=(c == 7)), r=[Wn] + hsr, w=[pzn])
                    P.op("act", lambda E: E.activation(out=sgl[k2][:], in_=pg[:, 0:2 * SL], func=AF.Silu), w=[pgn, f"sgl{k2}"])
                    P.op("dve", lambda E: E.tensor_tensor(out=hid[k2][:].rearrange("p c t -> p (c t)"), in0=pu[:, 0:2 * SL],
                                                          in1=sgl[k2][:], op=ALU.mult), r=[f"sgl{k2}"], w=[pun, f"hid{k2}"])
                    for q in range(2):
                        for dh in range(2):
                            pd, pdn = fbank()
                            for kc in range(2):
                                P.op("pe", lambda E, q=q, dh=dh, kc=kc, pd=pd: E.matmul(pd[:], lhsT=hid[k2][:, kc, q * 128:(q + 1) * 128],
                                                                                     rhs=wdb[k][:, kc, dh * 512:(dh + 1) * 512],
                                                                                     start=(kc == 0), stop=(kc == 1)),
                                     r=[f"hid{k2}", f"wdb{k}"], w=[pdn])
                            if dh == 0:
                                P.op("act", lambda E, q=q, pd=pd: E.activation(out=yo[k2][:, q, 0:512], in_=pd[:], func=AF.Copy), w=[pdn, f"yo{k2}_{q}0"])
                            else:
                                P.op("dve", lambda E, q=q, pd=pd: E.tensor_copy(out=yo[k2][:, q, 512:1024], in_=pd[:]), w=[pdn, f"yo{k2}_{q}1"])
                    P.dma("act", lambda E: E.dma_start(out=ysort[j * SL:(j + 1) * SL, :].rearrange("(q p) d -> p q d", p=128), in_=yo[k2][:]),
                          r=[f"yo{k2}_{q}{h}" for q in range(2) for h in "01"], w=[f"ysortw{j}"])
                for _j in range(TM):
                    etile(_j)

            scP.close()
            with contextlib.ExitStack() as sc:
                def sbl(name, shape, dt=F32, sc=sc):
                    t_ = sc.enter_context(nc.sbuf_tensor(f"{name}_{l}", list(shape), dt))
                    P.reg(name, t_)
                    return t_
                r1 = [sbl(f"r1{k}", [128, D]) for k in range(4)]
                r2 = [sbl(f"r2{k}", [128, D]) for k in range(4)]
                xc = [sbl(f"xc{k}", [128, D]) for k in range(4)]
                fgb = sbl("fgb", [128, D])
                ssf = sbl("ssf", [128, NB])
                rrf = sbl("rrf", [128, NB])
                junk2 = sbl("junk2", [128, D], BF16)
                ally = [f"ysortw{j}" for j in range(TM)]
                if last:
                    P.dma("sp", lambda E: E.dma_start(out=fgb[:], in_=final_g.ap().partition_broadcast(128)), w=["fgb"])
                    P.op("act", lambda E: E.mul(out=fgb[:], in_=fgb[:], mul=32.0), r=["fgb"], w=["fgb"])
                    P.op("dve", lambda E: E.memset(ssf[:], 0.0), w=["ssf"])
                def cblock(b):
                    k = b % 4
                    rows = slice(b * 128, (b + 1) * 128)
                    P.dma("pool", lambda E, b=b, k=k: E.indirect_dma_start(out=r1[k][:], out_offset=None, in_=ysort[:, :],
                                                                          in_offset=bass.IndirectOffsetOnAxis(ap=pos1[:, b:b + 1], axis=0)),
                          r=ally + ["pos1"], w=[f"r1{k}"])
                    P.dma("pool", lambda E, b=b, k=k: E.indirect_dma_start(out=r2[k][:], out_offset=None, in_=ysort[:, :],
                                                                          in_offset=bass.IndirectOffsetOnAxis(ap=pos2[:, b:b + 1], axis=0)),
                          r=ally + ["pos2"], w=[f"r2{k}"])
                    P.dma("sp", lambda E, k=k, rows=rows: E.dma_start(out=xc[k][:], in_=xs1[rows, :]), r=[f"xs1_{b // 4}"], w=[f"xc{k}"])
                    P.op("act", lambda E, b=b, k=k: E.activation(out=r1[k][:], in_=r1[k][:], func=AF.Copy, scale=w1[:, b:b + 1]),
                         r=[f"r1{k}", "w1"], w=[f"r1{k}"])
                    P.op("dve", lambda E, b=b, k=k: E.scalar_tensor_tensor(out=r2[k][:], in0=r2[k][:], scalar=w2[:, b:b + 1], in1=r1[k][:],
                                                                          op0=ALU.mult, op1=ALU.add), r=[f"r2{k}", f"r1{k}", "w2"], w=[f"r2{k}"])
                    P.op("pool", lambda E, k=k: E.tensor_tensor(out=r2[k][:], in0=r2[k][:], in1=G2, op=ALU.mult), r=[f"r2{k}", "mod10", "mod11"], w=[f"r2{k}"])
                    P.op("dve", lambda E, k=k: E.tensor_tensor(out=xc[k][:], in0=xc[k][:], in1=r2[k][:], op=ALU.add), r=[f"xc{k}", f"r2{k}"],
                         w=[f"xc{k}"])
                    if not last:
                        P.dma("act", lambda E, k=k, rows=rows: E.dma_start(out=xs2[rows, :], in_=xc[k][:]), r=[f"xc{k}"], w=[f"xs2_{b}"])
                    else:
                        P.op("act", lambda E, b=b, k=k: E.activation(out=junk2[:], in_=xc[k][:], func=AF.Square, accum_out=ssf[:, b:b + 1]),
                             r=[f"xc{k}"], w=["junk2", "ssf"])
                        P.op("act", lambda E, b=b: E.activation(out=rrf[:, b:b + 1], in_=ssf[:, b:b + 1], func=AF.Sqrt, bias=epsr[:, 0:1]),
                             r=["ssf", "epsr"], w=["rrf"])
                        P.op("dve", lambda E, b=b: E.reciprocal(out=rrf[:, b:b + 1], in_=rrf[:, b:b + 1]), r=["rrf"], w=["rrf"])
                        P.op("dve", lambda E, b=b, k=k: E.scalar_tensor_tensor(out=xc[k][:], in0=xc[k][:], scalar=rrf[:, b:b + 1], in1=fgb[:],
                                                                              op0=ALU.mult, op1=ALU.mult), r=[f"xc{k}", "rrf", "fgb"], w=[f"xc{k}"])
                        P.dma("act", lambda E, k=k, rows=rows: E.dma_start(out=out_d[rows, :], in_=xc[k][:]), r=[f"xc{k}"], w=[f"out_{b}"])
                for _b in range(NB):
                    cblock(_b)
            if nxt:
                with contextlib.ExitStack() as scq:
                    aw_q, ab_q, gbc_q = pro_bufs(scq, f"q{l + 1}")
                    prologue(l + 1, 20, 24, False, aw_q, ab_q, gbc_q)
        for _l in range(DEPTH_):
            layer(_l)
        P.op("sp", lambda E: E.nop(), r=[f"out_{b}" for b in range(NB)] + [k for k in ("dbg1", "dbg2", "dbg2b", "dbg3", "dbg4", "dbg_d_pos1",
             "dbg_d_pos2", "dbg_d_w1", "dbg_d_w2", "dbg_d_te")], w=["done"])
        P.emit(sems)
    if debug:
        print("UNRESOLVED regions:", sorted(getattr(P, "unres", [])))
    return nc


_CACHE = {}


def _prep(inputs, S):
    f = lambda a: np.ascontiguousarray(np.asarray(a, dtype=np.float32))
    pvec = np.concatenate([f(inputs["conv_w"]), f(inputs["conv_b"])[:, None, :], f(inputs["conv_ln_g"])[:, None, :],
                           f(inputs["conv_ln_b"])[:, None, :], f(inputs["pool_scale"])[:, None, :]], axis=1)
    rw = np.concatenate([f(inputs["router_group_w"]), f(inputs["router_expert_w"])], axis=2)
    rb = np.concatenate([f(inputs["router_group_b"]), f(inputs["router_expert_b"])], axis=1)
    shared = dict(ada_w=f(inputs["ada_w"]), ada_b=f(inputs["ada_b"]), norm1_g=f(inputs["norm1_g"]), w_in=f(inputs["w_in"]),
                  pool_w=f(inputs["pool_w"]), pvec=np.ascontiguousarray(pvec), w_out=f(inputs["w_out"]), norm2_g=f(inputs["norm2_g"]),
                  rw=np.ascontiguousarray(rw), rb=np.ascontiguousarray(rb), final_g=f(inputs["final_g"]))
    for nm, key, nch in (("wg", "expert_w_gate", 8), ("wu", "expert_w_up", 8), ("wd", "expert_w_down", 2)):
        w = f(inputs[key])
        L, E_, K, F_ = w.shape
        wr_ = w.reshape(L, E_, nch, 128, F_).transpose(0, 1, 3, 2, 4).reshape(L, E_ * 128, nch * F_)
        for l in range(L):
            shared[f"{nm}{l}"] = np.ascontiguousarray(wr_[l])
    x = f(inputs["x"])
    c = f(inputs["c"])
    in_maps = []
    for b in range(x.shape[0]):
        m = dict(shared)
        m["x"] = np.ascontiguousarray(x[b])
        m["c"] = np.ascontiguousarray(c[b])
        in_maps.append(m)
    return in_maps


def kernel(**inputs):
    x = np.asarray(inputs["x"])
    B, S, _ = x.shape
    assert B == N_CORES
    if S not in _CACHE:
        _CACHE[S] = build(S)
    nc = _CACHE[S]
    in_maps = _prep(inputs, S)
    res = run_bass_kernel_spmd(nc, in_maps, core_ids=list(range(B)))
    return np.stack([np.asarray(r["out"], dtype=np.float32) for r in res.results], axis=0)
```

```python
import numpy as np
import concourse.bass as bass
import concourse.mybir as mybir
from concourse.bass_utils import run_bass_kernel_spmd

F32 = mybir.dt.float32
BF16 = mybir.dt.bfloat16
I32 = mybir.dt.int32
AF = mybir.ActivationFunctionType
ALU = mybir.AluOpType
AX = mybir.AxisListType

D = 1024
DEPTH = 2
NE = 32
HID = 256
RMS_EPS = 1e-6
LN_EPS = 1e-5
POOL_WINDOWS = (2, 4, 8, 16)
CK = 31
N_CORES = 8


class Prog:
    ENGS = ("pe", "act", "dve", "pool", "sp")

    def __init__(self, nc, n_dma_sems=8):
        self.nc = nc
        self.ops = []
        self.n_dma_sems = n_dma_sems
        self.tens = {}

    def reg(self, short, handle):
        m = self.nc.lookup_mloc(handle)
        self.tens[short] = (m.name, int(m.addr), int(m.addr) + int(m.dims[1]))

    def _resolve(self, name):
        n = name
        while True:
            if n in self.tens:
                return (name,) + self.tens[n]
            n2 = n.rstrip("0123456789")
            if n2.endswith("_"):
                n2 = n2[:-1]
            if n2 == n or not n2:
                self.unres = getattr(self, "unres", set())
                self.unres.add(name.rstrip("0123456789"))
                return (name, None, 0, 0)
            n = n2

    def op(self, eng, fn, r=(), w=()):
        self.ops.append(dict(eng=eng, fn=fn, r=tuple(self._resolve(x) for x in r), w=tuple(self._resolve(x) for x in w), dma=False))

    def dma(self, q, fn, r=(), w=()):
        self.ops.append(dict(eng=q, fn=fn, r=tuple(self._resolve(x) for x in r), w=tuple(self._resolve(x) for x in w), dma=True))

    @staticmethod
    def _key(nm, par, recs, lo, hi, bins):
        key = (nm, par)
        if key not in recs:
            recs[key] = dict(par=par, lo=lo, hi=hi, w=None, r=[])
            if par is not None:
                for b_ in range(lo // 2048, (hi - 1) // 2048 + 1):
                    bins.setdefault(b_, []).append(key)
        return key

    def barrier(self):
        self.ops.append(dict(eng=None, fn=None, r=(), w=(), dma=False, barrier=True))

    def emit(self, sems):
        nc = self.nc
        engobj = dict(pe=nc.tensor, act=nc.scalar, dve=nc.vector, pool=nc.gpsimd, sp=nc.sync)
        ops = self.ops
        n = len(ops)
        recs = {}
        bins = {}
        clock = {e: dict(pe=-1, act=-1, dve=-1, pool=-1, sp=-1, dma=set()) for e in self.ENGS}
        opclock = [None] * n
        dma_hist = {e: [] for e in self.ENGS}
        waits = [None] * n
        sig = [False] * n
        bar_deps = set()
        last_on = {}
        for i, o in enumerate(ops):
            e = o["eng"]
            if e is None:
                bar_deps = set(last_on.values())
                for q in self.ENGS:
                    bar_deps |= set(dma_hist[q][-self.n_dma_sems:])
                waits[i] = []
                opclock[i] = None
                continue
            deps = set(bar_deps)
            for (nm, par, lo, hi) in o["r"]:
                key = self._key(nm, par, recs, lo, hi, bins)
                lw = recs[key]["w"]
                if lw is not None:
                    deps.add(lw)
            for (nm, par, lo, hi) in o["w"]:
                key = self._key(nm, par, recs, lo, hi, bins)
                rc = recs[key]
                if rc["w"] is not None:
                    deps.add(rc["w"])
                deps.update(rc["r"])
                if par is not None:
                    seen = set()
                    for b_ in range(lo // 2048, (hi - 1) // 2048 + 1):
                        for k2_ in bins.get(b_, ()):
                            if k2_ in seen or k2_ == key:
                                continue
                            seen.add(k2_)
                            r2 = recs[k2_]
                            if r2["par"] != par and r2["lo"] < hi and lo < r2["hi"]:
                                if r2["w"] is not None:
                                    deps.add(r2["w"])
                                deps.update(r2["r"])
            if o["dma"]:
                h = dma_hist[e]
                if len(h) >= self.n_dma_sems:
                    deps.add(h[-self.n_dma_sems])
                h.append(i)
            deps.discard(i)
            need = []
            ck = clock[e]
            for j in sorted(deps):
                pj = ops[j]
                if pj["dma"]:
                    if j in ck["dma"]:
                        continue
                else:
                    if pj["eng"] == e and e == "pe" and not o["dma"]:
                        continue
                    if ck[pj["eng"]] >= j:
                        continue
                need.append(j)
                sig[j] = True
                oc = opclock[j]
                for k in ("pe", "act", "dve", "pool", "sp"):
                    if oc[k] > ck[k]:
                        ck[k] = oc[k]
                ck["dma"] |= oc["dma"]
            waits[i] = need
            oc = dict(pe=ck["pe"], act=ck["act"], dve=ck["dve"], pool=ck["pool"], sp=ck["sp"], dma=set(ck["dma"]))
            if o["dma"]:
                oc["dma"].add(i)
            else:
                oc[e] = i
            opclock[i] = oc
            if not o["dma"]:
                last_on[e] = i
            for (nm, par, lo, hi) in o["r"]:
                recs[self._key(nm, par, recs, lo, hi, bins)]["r"].append(i)
            for (nm, par, lo, hi) in o["w"]:
                rc = recs[self._key(nm, par, recs, lo, hi, bins)]
                rc["w"] = i
                rc["r"] = []
        cnt = {e: 0 for e in self.ENGS}
        dcnt = {}
        dma_n = {e: 0 for e in self.ENGS}
        ev = [None] * n
        for i, o in enumerate(ops):
            e = o["eng"]
            if e is None:
                continue
            E = engobj[e]
            for j in waits[i]:
                s, v = ev[j]
                E.wait_ge(s, v)
            inst = o["fn"](E)
            if o["dma"]:
                k = dma_n[e] % self.n_dma_sems
                dma_n[e] += 1
                s = sems["dma_" + e][k]
                dcnt[(e, k)] = dcnt.get((e, k), 0) + 16
                inst.then_inc(s, 16)
                ev[i] = (s, dcnt[(e, k)])
            elif sig[i]:
                cnt[e] += 1
                inst.then_inc(sems[e], 1)
                ev[i] = (sems[e], cnt[e])
        return ev


def build(S, depth=DEPTH, debug=False):
    DEPTH_ = depth
    NB = S // 128
    NT = S // 512
    SL = 256
    NB2 = (S + SL - 1) // SL
    TM = 2 * S // SL + NE
    nc = bass.Bass("TRN2", target_bir_lowering=False)
    P = Prog(nc)

    def dram_in(name, shape, dt=F32):
        return nc.dram_tensor(name, list(shape), dt, kind="ExternalInput")

    x_d = dram_in("x", [S, D])
    c_d = dram_in("c", [D])
    ada_w = dram_in("ada_w", [DEPTH, D, 6 * D])
    ada_b = dram_in("ada_b", [DEPTH, 6 * D])
    norm1_g = dram_in("norm1_g", [DEPTH, D])
    w_in = dram_in("w_in", [DEPTH, D, 1536])
    pool_w = dram_in("pool_w", [DEPTH, 4, 128, 128])
    pvec = dram_in("pvec", [DEPTH, 35, 512])
    w_out = dram_in("w_out", [DEPTH, D, D])
    norm2_g = dram_in("norm2_g", [DEPTH, D])
    rw = dram_in("rw", [DEPTH, D, 36])
    rb = dram_in("rb", [DEPTH, 36])
    wg_d = [dram_in(f"wg{l}", [NE * 128, 8 * HID]) for l in range(DEPTH)]
    wu_d = [dram_in(f"wu{l}", [NE * 128, 8 * HID]) for l in range(DEPTH)]
    wd_d = [dram_in(f"wd{l}", [NE * 128, 2 * D]) for l in range(DEPTH)]
    final_g = dram_in("final_g", [D])
    out_d = nc.dram_tensor("out", [S, D], F32, kind="ExternalOutput")
    sk = "ExternalOutput" if debug else "Internal"
    xs1 = nc.dram_tensor("xs1", [S, D], F32, kind=sk)
    xs2 = nc.dram_tensor("xs2", [S, D], F32, kind=sk)
    h2s = nc.dram_tensor("h2s", [S, D], BF16, kind=sk)
    hsort = nc.dram_tensor("hsort", [TM * SL, D], BF16, kind=sk)
    ysort = nc.dram_tensor("ysort", [TM * SL, D], BF16, kind=sk)
    dbg = {}
    if debug:
        for nm, shp, dt in (("d_mod", [128, 6 * D], F32), ("d_hT", [128, 8 * 512], BF16), ("d_ycat", [128, 8 * 512], BF16),
                            ("d_lg", [128, 4 * 36], F32), ("d_pos1", [128, NB], I32), ("d_pos2", [128, NB], I32),
                            ("d_w1", [128, NB], F32), ("d_w2", [128, NB], F32), ("d_te", [128, TM], I32), ("d_pvT", [128, 4 * 35], F32)):
            dbg[nm] = nc.dram_tensor(nm, shp, dt, kind="ExternalOutput")

    import contextlib
    es = contextlib.ExitStack()
    with es:
        def sb(name, shape, dt=F32):
            t_ = es.enter_context(nc.sbuf_tensor(name, list(shape), dt))
            P.reg(name, t_)
            return t_

        def psum(name, shape, dt=F32):
            return es.enter_context(nc.psum_tensor(name, list(shape), dt))

        sems = {}
        for e in Prog.ENGS:
            sems[e] = es.enter_context(nc.semaphore("s_" + e))
            sems["dma_" + e] = [es.enter_context(nc.semaphore(f"d_{e}{k}")) for k in range(P.n_dma_sems)]

        identf = sb("identf", [128, 128])
        identb = sb("identb", [128, 128], BF16)
        onesf = sb("onesf", [128, 128])
        onesb = sb("onesb", [128, 128], BF16)
        trib = sb("trib", [128, 128], BF16)
        trif = sb("trif", [128, 128])
        bavg = sb("bavg", [128, 128], BF16)
        bavgf = sb("bavgf", [128, 128])
        inv16 = sb("inv16", [128, 16])
        iota_i = sb("iota_i", [128, 128], I32)
        iota_f = sb("iota_f", [128, 128])

        P.op("pool", lambda E: E.memset(identf[:], 0.0), w=["identf"])
        P.op("pool", lambda E: E.affine_select(out=identf[:], in_=identf[:], pattern=[[-1, 128]], compare_op=ALU.not_equal,
                                               fill=1.0, base=0, channel_multiplier=1), r=["identf"], w=["identf"])
        P.op("pool", lambda E: E.tensor_copy(out=identb[:], in_=identf[:]), r=["identf"], w=["identb"])
        P.op("pool", lambda E: E.memset(onesf[:], 1.0), w=["onesf"])
        P.op("pool", lambda E: E.memset(onesb[:], 1.0), w=["onesb"])
        P.op("pool", lambda E: E.memset(trif[:], 1.0), w=["trif"])
        P.op("pool", lambda E: E.affine_select(out=trif[:], in_=trif[:], pattern=[[1, 128]], compare_op=ALU.is_gt,
                                               fill=0.0, base=0, channel_multiplier=-1), r=["trif"], w=["trif"])
        P.op("pool", lambda E: E.tensor_copy(out=trib[:], in_=trif[:]), r=["trif"], w=["trib"])
        P.op("pool", lambda E: E.memset(bavgf[:], 0.0), w=["bavgf"])
        P.op("pool", lambda E: E.memset(bavgf[0:64, 0:64], 1.0 / 64), r=["bavgf"], w=["bavgf"])
        P.op("pool", lambda E: E.memset(bavgf[64:128, 64:128], 1.0 / 64), r=["bavgf"], w=["bavgf"])
        P.op("pool", lambda E: E.tensor_copy(out=bavg[:], in_=bavgf[:]), r=["bavgf"], w=["bavg"])
        P.op("pool", lambda E: E.iota(iota_i[:], pattern=[[1, 128]], base=0, channel_multiplier=0), w=["iota_i"])
        P.op("pool", lambda E: E.tensor_copy(out=iota_f[:], in_=iota_i[:]), r=["iota_i"], w=["iota_f"])
        epsr = sb("epsr", [128, 1])
        epsl = sb("epsl", [128, 1])
        P.op("pool", lambda E: E.memset(epsr[:], D * RMS_EPS), w=["epsr"])
        P.op("pool", lambda E: E.memset(epsl[:], LN_EPS), w=["epsl"])
        pidx_i = sb("pidx_i", [128, 1], I32)
        pidx_f = sb("pidx_f", [128, 1])
        P.op("pool", lambda E: E.iota(pidx_i[:], pattern=[[0, 1]], base=0, channel_multiplier=1), w=["pidx_i"])
        P.op("pool", lambda E: E.tensor_copy(out=pidx_f[:], in_=pidx_i[:]), r=["pidx_i"], w=["pidx_f"])
        P.op("dve", lambda E: E.tensor_scalar(out=inv16[:], in0=iota_f[:, 0:16], scalar1=1.0, scalar2=None, op0=ALU.add),
             r=["iota_f"], w=["inv16"])
        P.op("dve", lambda E: E.reciprocal(out=inv16[:], in_=inv16[:]), r=["inv16"], w=["inv16"])

        bcd = {}

        def mk_bc(E):
            reg = E.alloc_register("bcreg")
            inst = E.reg_mov(reg, NE * 128 - 1)
            bcd["v"] = E.snap(reg, donate=True)
            return E.memset(epsl[:], LN_EPS)
        P.op("pool", mk_bc, w=["epsl"])
        NFB = 5
        psf = [psum(f"psf{i}", [128, 512]) for i in range(NFB)]
        psb = [psum(f"psb{i}", [128, 1024], BF16) for i in range(2)]
        psr = psum("psr", [128, 512])
        bank_ctr = [0, 0]

        def fbank():
            i = bank_ctr[0] % NFB
            bank_ctr[0] += 1
            return psf[i], f"psf{i}"

        def bbank():
            i = bank_ctr[1] % 2
            bank_ctr[1] += 1
            return psb[i], f"psb{i}"

        c_sb = sb("c_sb", [128, 8])
        cond = sb("cond", [128, 8])
        condbc = sb("condbc", [128, 8, 128])
        P.dma("sp", lambda E: E.dma_start(out=c_sb[:], in_=c_d.ap().rearrange("(c p) -> p c", p=128), allow_slow_non_contiguous=True),
              w=["c_sb"])
        P.op("act", lambda E: E.activation(out=cond[:], in_=c_sb[:], func=AF.Silu), r=["c_sb"], w=["cond"])
        for c in range(8):
            P.op("act", lambda E, c=c: E.activation(out=condbc[:, c, :], in_=onesf[:], func=AF.Copy, scale=cond[:, c:c + 1]),
                 r=["cond", "onesf"], w=["condbc"])

        mod = sb("mod", [128, 6 * D])
        oh1 = sb("oh1", [128, NB, NE], BF16)
        oh2 = sb("oh2", [128, NB, NE], BF16)
        Aall = sb("Aall", [128, NB, NE], BF16)
        w1 = sb("w1", [128, NB])
        w2 = sb("w2", [128, NB])
        pos1 = sb("pos1", [128, NB], I32)
        pos2 = sb("pos2", [128, NB], I32)
        te_i = sb("te_i", [128, TM], I32)
        lg_all = sb("lg_all", [128, NB, 36])

        B1, A1, G1, B2, A2, G2 = (mod[:, k * D:(k + 1) * D] for k in range(6))

        win = sb("win", [128, 8, 1536], BF16)
        wout = sb("wout", [128, 8, D], BF16)
        pw = sb("pw", [128, 4, 128], BF16)
        wr = sb("wr", [128, 8, 36])
        rbias = sb("rbias", [128, 4, 36])
        pv = sb("pv", [35, 512])
        pvT = sb("pvT", [128, 4, 35])
        diag = sb("diag", [128, CK, 4, 128], BF16)

        def prologue(l, n0, n1, do_scale, aw, ab, gbc):
            for nn in range(n0, n1):
                k = nn % len(aw)
                blk = (nn * 256) // 512
                P.dma("sp", lambda E, nn=nn, k=k: E.dma_start(
                    out=aw[k][:], in_=ada_w[l, :, nn * 256:(nn + 1) * 256].rearrange("(c p) n -> p c n", p=128)), w=[f"aw{k}"])
                P.dma("sp", lambda E, nn=nn, k=k: E.dma_start(
                    out=ab[k][:], in_=ada_b[l, nn * 256:(nn + 1) * 256].partition_broadcast(128)), w=[f"ab{k}"])
                pb, pbn = fbank()
                for c in range(8):
                    P.op("pe", lambda E, c=c, pb=pb, k=k: E.matmul(pb[:, 0:256], lhsT=condbc[:, c, :], rhs=aw[k][:, c, :],
                                                                  start=(c == 0), stop=(c == 7)), r=["condbc", f"aw{k}"], w=[pbn])
                P.op("dve", lambda E, nn=nn, pb=pb, k=k: E.tensor_tensor(out=mod[:, nn * 256:(nn + 1) * 256], in0=pb[:, 0:256], in1=ab[k][:],
                                                                         op=ALU.add), r=[f"ab{k}"], w=[pbn, f"mod{blk}"])
            if do_scale:
                for (gsrc, Aap, mr) in ((norm1_g, A1, ["mod2", "mod3"]), (norm2_g, A2, ["mod8", "mod9"])):
                    P.dma("sp", lambda E, gsrc=gsrc: E.dma_start(out=gbc[:], in_=gsrc[l, :].partition_broadcast(128)), w=["gbc"])
                    P.op("act", lambda E: E.mul(out=gbc[:], in_=gbc[:], mul=32.0), r=["gbc"], w=["gbc"])
                    P.op("dve", lambda E, Aap=Aap: E.scalar_tensor_tensor(out=Aap, in0=Aap, scalar=1.0, in1=gbc[:], op0=ALU.add,
                                                                          op1=ALU.mult), r=["gbc"] + mr, w=mr)

        def load_weights(l):
            for c in range(8):
                P.dma("pool", lambda E, c=c: E.dma_start(out=win[:, c, :], in_=w_in[l, c * 128:(c + 1) * 128, :]), w=["win"])
            for c in range(8):
                P.dma("pool", lambda E, c=c: E.dma_start(out=wout[:, c, :], in_=w_out[l, c * 128:(c + 1) * 128, :]), w=["wout"])
            P.dma("pool", lambda E: E.dma_start(out=pw[:], in_=pool_w[l].rearrange("g c d -> c g d")), w=["pw"])
            P.dma("sp", lambda E: E.dma_start(out=wr[:], in_=rw[l].rearrange("(c p) g -> p c g", p=128)), w=["wr"])
            for j in range(4):
                P.dma("sp", lambda E, j=j: E.dma_start(out=rbias[:, j, :], in_=rb[l, :].partition_broadcast(128)), w=["rbias"])
            P.dma("sp", lambda E: E.dma_start(out=pv[:], in_=pvec[l]), w=["pv"])
            for c in range(4):
                pb, pbn = fbank()
                P.op("pe", lambda E, c=c, pb=pb: E.transpose(out=pb[:, 0:35], in_=pv[:, c * 128:(c + 1) * 128], identity=identf[0:35, 0:35]),
                     r=["pv", "identf"], w=[pbn])
                P.op("act", lambda E, c=c, pb=pb: E.activation(out=pvT[:, c, :], in_=pb[:, 0:35], func=AF.Copy), w=[pbn, "pvT"])
            for k in range(CK):
                for c in range(4):
                    eng = "dve" if (k * 4 + c) % 2 == 0 else "pool"
                    P.op(eng, lambda E, k=k, c=c: E.tensor_scalar(out=diag[:, k, c, :], in0=identf[:], scalar1=pvT[:, c, k:k + 1],
                                                                  scalar2=None, op0=ALU.mult),
                         r=["identf", "pvT"], w=[f"diag{k}_{c}"])

        def pro_bufs(sc, l, nbuf=2):
            def sbl(name, shape, dt=F32):
                t_ = sc.enter_context(nc.sbuf_tensor(f"{name}_{l}", list(shape), dt))
                P.reg(name, t_)
                return t_
            aw = [sbl(f"aw{k}", [128, 8, 256]) for k in range(nbuf)]
            ab = [sbl(f"ab{k}", [128, 256]) for k in range(nbuf)]
            gbc = sbl("gbc", [128, D])
            return aw, ab, gbc

        with contextlib.ExitStack() as sc0:
            aw_, ab_, gbc_ = pro_bufs(sc0, "p0")
            prologue(0, 0, 24, True, aw_, ab_, gbc_)
        load_weights(0)

        def layer(l):
            x_src = x_d if l == 0 else xs2
            last = (l == DEPTH_ - 1)
            with contextlib.ExitStack() as sc:
                def sbl(name, shape, dt=F32, sc=sc):
                    t_ = sc.enter_context(nc.sbuf_tensor(f"{name}_{l}", list(shape), dt))
                    P.reg(name, t_)
                    return t_
                xt = [sbl(f"xt{k}", [128, 4, D]) for k in range(1)]
                ss = sbl("ss", [128, 8])
                rr = sbl("rr", [128, 8])
                tmpA = [sbl(f"tmpA{k}", [128, D]) for k in range(2)]
                hb = sbl("hb", [128, 4, D], BF16)
                hT = sbl("hT", [128, 8, 512], BF16)
                u = sbl("u", [128, 4, 528])
                sA = sbl("sA", [128, 528])
                sB = sbl("sB", [128, 528])
                pooled = sbl("pooled", [128, 4, 512], BF16)
                ycat = sbl("ycat", [128, 8, 512], BF16)
                vbuf = sbl("vbuf", [128, 4, 544], BF16)
                sig = [sbl(f"sig{k}", [128, 512]) for k in range(1)]
                ybf = [sbl(f"ybf{k}", [128, 512], BF16) for k in range(2)]
                dd = [sbl(f"dd{k}", [128, 512]) for k in range(1)]
                sq = [sbl(f"sq{k}", [128, 512], BF16) for k in range(1)]
                rs = [sbl(f"rs{k}", [128, 512]) for k in range(1)]
                yn = dd
                tmpO = [sbl(f"tmpO{k}", [128, 512]) for k in range(2)]
                h2b = hb
                h2T = [sbl(f"h2T{k}", [128, 8, 128]) for k in range(1)]

                P.op("pool", lambda E: E.memset(u[:], 0.0), w=["u0", "u1", "u2", "u3"])
                P.op("pool", lambda E: E.memset(vbuf[:], 0.0), w=["vbuf0", "vbuf1", "vbuf2", "vbuf3"])

                def mtile(i):
                    X = xt[0]
                    Xn = "xt0"
                    rows = slice(i * 512, (i + 1) * 512)
                    P.dma("sp", lambda E, X=X, rows=rows: E.dma_start(out=X[:], in_=x_src[rows, :].rearrange("(j p) d -> p j d", p=128)),
                          r=[f"xs2_{4 * i + q}" for q in range(4)], w=[Xn])
                    P.op("dve", lambda E: E.memset(ss[:], 0.0), w=["ss"])
                    for j in range(4):
                        P.op("act", lambda E, j=j, X=X: E.activation(out=hb[:, j, :], in_=X[:, j, :], func=AF.Square, accum_out=ss[:, j:j + 1]),
                             r=[Xn], w=[f"hb{j}", "ss"])
                    P.op("act", lambda E: E.activation(out=rr[:, 0:4], in_=ss[:, 0:4], func=AF.Sqrt, bias=epsr[:, 0:1]), r=["ss", "epsr"], w=["rr"])
                    P.op("dve", lambda E: E.reciprocal(out=rr[:, 0:4], in_=rr[:, 0:4]), r=["rr"], w=["rr"])
                    for j in range(4):
                        T = tmpA[j % 2]
                        Tn = f"tmpA{j % 2}"
                        P.op("dve", lambda E, j=j, X=X, T=T: E.scalar_tensor_tensor(out=T[:], in0=X[:, j, :], scalar=rr[:, j:j + 1], in1=A1,
                                                                                    op0=ALU.mult, op1=ALU.mult),
                             r=[Xn, "rr", "mod2", "mod3"], w=[Tn])
                        P.op("pool", lambda E, j=j, T=T: E.tensor_tensor(out=hb[:, j, :], in0=T[:], in1=B1, op=ALU.add),
                             r=[Tn, "mod0", "mod1"], w=[f"hb{j}"])
                    for j in range(4):
                        pb, pbn = bbank()
                        for c in range(8):
                            P.op("pe", lambda E, j=j, c=c, pb=pb: E.transpose(out=pb[:, c * 128:(c + 1) * 128], in_=hb[:, j, c * 128:(c + 1) * 128],
                                                                             identity=identb[:]),
                                 r=[f"hb{j}", "identb"], w=[pbn])
                        eng = "act" if j % 2 == 0 else "dve"
                        if eng == "act":
                            P.op("act", lambda E, j=j, pb=pb: E.activation(out=hT[:, :, j * 128:(j + 1) * 128],
                                                                           in_=pb[:].rearrange("p (c t) -> p c t", c=8), func=AF.Copy),
                                 w=[pbn, f"hT{j}"])
                        else:
                            P.op("dve", lambda E, j=j, pb=pb: E.tensor_copy(out=hT[:, :, j * 128:(j + 1) * 128],
                                                                            in_=pb[:].rearrange("p (c t) -> p c t", c=8)),
                                 w=[pbn, f"hT{j}"])
                    hTr = ["hT0", "hT1", "hT2", "hT3"]
                    if debug and l == 0 and i == 0:
                        P.dma("sp", lambda E: E.dma_start(out=dbg["d_hT"].ap(), in_=hT[:].rearrange("p c t -> p (c t)")), r=hTr, w=["dbg1"])
                        P.dma("sp", lambda E: E.dma_start(out=dbg["d_mod"].ap(), in_=mod[:]), r=[f"mod{q}" for q in range(12)], w=["dbg2"])
                        P.dma("sp", lambda E: E.dma_start(out=dbg["d_pvT"].ap(), in_=pvT[:].rearrange("p c t -> p (c t)")), r=["pvT"], w=["dbg2b"])

                    def win_mm(oc):
                        pb, pbn = fbank()
                        for c in range(8):
                            P.op("pe", lambda E, c=c, pb=pb, oc=oc: E.matmul(pb[:], lhsT=win[:, c, oc * 128:(oc + 1) * 128], rhs=hT[:, c, :],
                                                                           start=(c == 0), stop=(c == 7)),
                                 r=["win"] + hTr, w=[pbn])
                        return pb, pbn

                    def pU(g):
                        pb, pbn = win_mm(g)
                        P.op("act", lambda E, pb=pb: E.activation(out=u[:, g, 16:528], in_=pb[:], func=AF.Copy), w=[pbn, f"u{g}"])

                    def pD(g):
                        wdw = POOL_WINDOWS[g]
                        un = f"u{g}"
                        step = 1
                        bufs = [sA, sB]
                        bi = 0
                        cur = None
                        lo = 0
                        while step < wdw:
                            dst = bufs[bi]
                            dn = "sA" if bi == 0 else "sB"
                            nlo = lo + step
                            if cur is None:
                                P.op("pool", lambda E, dst=dst, nlo=nlo, step=step: E.tensor_tensor(
                                    out=dst[:, nlo:528], in0=u[:, g, nlo:528], in1=u[:, g, nlo - step:528 - step], op=ALU.add),
                                    r=[un], w=[dn])
                            else:
                                cs, cn = cur
                                P.op("pool", lambda E, dst=dst, cs=cs, nlo=nlo, step=step: E.tensor_tensor(
                                    out=dst[:, nlo:528], in0=cs[:, nlo:528], in1=cs[:, nlo - step:528 - step], op=ALU.add),
                                    r=[cn], w=[dn])
                            cur = (dst, dn)
                            lo = nlo
                            step *= 2
                            bi ^= 1
                        cs, cn = cur
                        P.op("dve", lambda E, cs=cs, wdw=wdw: E.scalar_tensor_tensor(
                            out=pooled[:, g, :], in0=cs[:, 16:528], scalar=1.0 / wdw, in1=u[:, g, 16:528], op0=ALU.mult, op1=ALU.subtract),
                            r=[cn, un], w=[f"pooled{g}"])
                        if i == 0:
                            nfix = wdw - 1
                            P.op("pool", lambda E, cs=cs, nfix=nfix: E.tensor_tensor(out=cs[:, 16:16 + nfix], in0=cs[:, 16:16 + nfix],
                                                                                    in1=inv16[:, 0:nfix], op=ALU.mult),
                                 r=[cn, "inv16", f"pooled{g}"], w=[cn])
                            P.op("pool", lambda E, cs=cs, nfix=nfix: E.tensor_tensor(out=pooled[:, g, 0:nfix], in0=cs[:, 16:16 + nfix],
                                                                                    in1=u[:, g, 16:16 + nfix], op=ALU.subtract),
                                 r=[cn, un], w=[f"pooled{g}"])
                        P.op("pool", lambda E: E.tensor_copy(out=u[:, g, 0:16], in_=u[:, g, 512:528]), r=[un], w=[un])

                    def pPW(g):
                        pb2, pb2n = fbank()
                        P.op("pe", lambda E, pb2=pb2: E.matmul(pb2[:], lhsT=pw[:, g, :], rhs=pooled[:, g, :], start=True, stop=True),
                             r=["pw", f"pooled{g}"], w=[pb2n])
                        P.op("act", lambda E, pb2=pb2: E.activation(out=ycat[:, g, :], in_=pb2[:], func=AF.Copy, scale=pvT[:, g, 34:35]),
                             r=["pvT"], w=[pb2n, f"ycat{g}"])

                    def cF(c):
                        kb = c % 2
                        vn = f"vbuf{c}"
                        pa, pan = win_mm(4 + c)
                        pg, pgn = win_mm(8 + c)
                        P.op("act", lambda E, pg=pg: E.activation(out=sig[0][:], in_=pg[:], func=AF.Sigmoid), w=[pgn, "sig0"])
                        P.op("dve", lambda E, pa=pa: E.tensor_tensor(out=vbuf[:, c, 32:544], in0=pa[:], in1=sig[0][:], op=ALU.mult),
                             r=["sig0"], w=[pan, vn])
                        py, pyn = fbank()
                        for k in range(CK):
                            P.op("pe", lambda E, k=k, py=py: E.matmul(py[:], lhsT=diag[:, k, c, :], rhs=vbuf[:, c, 2 + k:2 + k + 512],
                                                                    start=(k == 0), stop=(k == CK - 1)),
                                 r=[f"diag{k}_{c}", vn], w=[pyn])
                        P.op("pool", lambda E: E.tensor_copy(out=vbuf[:, c, 2:32], in_=vbuf[:, c, 514:544]), r=[vn], w=[vn])
                        P.op("act", lambda E, py=py: E.activation(out=ybf[kb][:], in_=py[:], func=AF.Identity, bias=pvT[:, c, 31:32]),
                             r=["pvT"], w=[pyn, f"ybf{kb}"])

                    def cB1(c):
                        kb = c % 2
                        pm, pmn = fbank()
                        P.op("pe", lambda E, pm=pm: E.matmul(pm[:], lhsT=bavg[:], rhs=ybf[kb][:], start=True, stop=True),
                             r=["bavg", f"ybf{kb}"], w=[pmn])
                        P.op("dve", lambda E, pm=pm: E.tensor_tensor(out=dd[0][:], in0=ybf[kb][:], in1=pm[:], op=ALU.subtract),
                             r=[f"ybf{kb}"], w=[pmn, "dd0"])
                        P.op("act", lambda E: E.activation(out=sq[0][:], in_=dd[0][:], func=AF.Square), r=["dd0"], w=["sq0"])

                    def cB2(c):
                        pvv, pvn = fbank()
                        P.op("pe", lambda E, pvv=pvv: E.matmul(pvv[:], lhsT=bavg[:], rhs=sq[0][:], start=True, stop=True),
                             r=["bavg", "sq0"], w=[pvn])
                        P.op("act", lambda E, pvv=pvv: E.activation(out=rs[0][:], in_=pvv[:], func=AF.Sqrt, bias=epsl[:, 0:1]),
                             r=["epsl"], w=[pvn, "rs0"])
                        P.op("dve", lambda E: E.reciprocal(out=rs[0][:], in_=rs[0][:]), r=["rs0"], w=["rs0"])
                        P.op("pool", lambda E: E.tensor_tensor(out=dd[0][:], in0=dd[0][:], in1=rs[0][:], op=ALU.mult),
                             r=["dd0", "rs0"], w=["dd0"])
                        P.op("act", lambda E: E.activation(out=ycat[:, 4 + c, :], in_=dd[0][:], func=AF.Silu, bias=pvT[:, c, 33:34],
                                                           scale=pvT[:, c, 32:33]),
                             r=["dd0", "pvT"], w=[f"ycat{4 + c}"])

                    for g_ in range(4):
                        pU(g_)
                    cF(0); pD(0); cF(1); pD(1); cB1(0); cF(2); pD(2); cB2(0); cB1(1); cF(3); pD(3); cB2(1); cB1(2)
                    pPW(0); pPW(1); cB2(2); cB1(3); pPW(2); pPW(3); cB2(3)
                    ycr = [f"ycat{k}" for k in range(8)]
                    if debug and l == 0 and i == 0:
                        P.dma("sp", lambda E: E.dma_start(out=dbg["d_ycat"].ap(), in_=ycat[:].rearrange("p c t -> p (c t)")), r=ycr, w=["dbg3"])
                    for j in range(4):
                        for dh in range(2):
                            po, pon = fbank()
                            for k in range(8):
                                P.op("pe", lambda E, j=j, dh=dh, k=k, po=po: E.matmul(po[:], lhsT=ycat[:, k, j * 128:(j + 1) * 128],
                                                                                  rhs=wout[:, k, dh * 512:(dh + 1) * 512], start=(k == 0), stop=(k == 7)),
                                     r=ycr + ["wout"], w=[pon])
                            kk = (j * 2 + dh) % 2
                            P.op("dve", lambda E, dh=dh, po=po, kk=kk: E.tensor_tensor(out=tmpO[kk][:], in0=po[:], in1=G1[:, dh * 512:(dh + 1) * 512],
                                                                                      op=ALU.mult), r=[f"mod{4 + dh}"], w=[pon, f"tmpO{kk}"])
                            P.op("pool", lambda E, j=j, dh=dh, X=X, kk=kk: E.tensor_tensor(out=X[:, j, dh * 512:(dh + 1) * 512], in0=tmpO[kk][:],
                                                                                         in1=X[:, j, dh * 512:(dh + 1) * 512], op=ALU.add),
                                 r=[f"tmpO{kk}", Xn], w=[Xn])
                    P.dma("act", lambda E, X=X, rows=rows: E.dma_start(out=xs1[rows, :].rearrange("(j p) d -> p j d", p=128), in_=X[:]),
                          r=[Xn], w=[f"xs1_{i}"])
                    for j in range(4):
                        P.op("act", lambda E, j=j, X=X: E.activation(out=hb[:, j, :], in_=X[:, j, :], func=AF.Square, accum_out=ss[:, 4 + j:5 + j]),
                             r=[Xn], w=[f"hb{j}", "ss"])
                    P.op("act", lambda E: E.activation(out=rr[:, 4:8], in_=ss[:, 4:8], func=AF.Sqrt, bias=epsr[:, 0:1]), r=["ss", "epsr"], w=["rr"])
                    P.op("dve", lambda E: E.reciprocal(out=rr[:, 4:8], in_=rr[:, 4:8]), r=["rr"], w=["rr"])
                    pr, prn = psr, "psr"
                    for j in range(4):
                        T = tmpA[j % 2]
                        Tn = f"tmpA{j % 2}"
                        H = T
                        Hn = Tn
                        P.op("dve", lambda E, j=j, X=X, T=T: E.scalar_tensor_tensor(out=T[:], in0=X[:, j, :], scalar=rr[:, 4 + j:5 + j], in1=A2,
                                                                                    op0=ALU.mult, op1=ALU.mult),
                             r=[Xn, "rr", "mod8", "mod9"], w=[Tn])
                        P.op("pool", lambda E, T=T, H=H: E.tensor_tensor(out=H[:], in0=T[:], in1=B2, op=ALU.add), r=[Tn, "mod6", "mod7"], w=[Hn])
                        P.op("act", lambda E, j=j, H=H: E.activation(out=h2b[:, j, :], in_=H[:], func=AF.Copy), r=[Hn], w=[f"hb{j}"])
                        HT = h2T[0]
                        HTn = "h2T0"
                        for half in range(2):
                            pt, ptn = fbank()
                            for cc in range(4):
                                c = half * 4 + cc
                                P.op("pe", lambda E, c=c, cc=cc, H=H, pt=pt: E.transpose(out=pt[:, cc * 128:(cc + 1) * 128],
                                                                                       in_=H[:, c * 128:(c + 1) * 128], identity=identf[:]),
                                     r=[Hn, "identf"], w=[ptn])
                            eng = "act" if half == 0 else "dve"
                            if eng == "act":
                                P.op("act", lambda E, half=half, HT=HT, pt=pt: E.activation(out=HT[:, half * 4:(half + 1) * 4, :],
                                                                                        in_=pt[:].rearrange("p (c t) -> p c t", c=4), func=AF.Copy),
                                     w=[ptn, HTn])
                            else:
                                P.op("dve", lambda E, half=half, HT=HT, pt=pt: E.tensor_copy(out=HT[:, half * 4:(half + 1) * 4, :],
                                                                                         in_=pt[:].rearrange("p (c t) -> p c t", c=4)),
                                     w=[ptn, HTn])
                        for c in range(8):
                            P.op("pe", lambda E, j=j, c=c, HT=HT, pr=pr: E.matmul(pr[:, j * 36:(j + 1) * 36], lhsT=HT[:, c, :], rhs=wr[:, c, :],
                                                                              start=(c == 0), stop=(c == 7)),
                                 r=[HTn, "wr"], w=[prn])
                    P.dma("act", lambda E, rows=rows: E.dma_start(out=h2s[rows, :].rearrange("(j p) d -> p j d", p=128), in_=h2b[:]), r=["hb0", "hb1", "hb2", "hb3"], w=[f"h2s_{i}"])
                    P.op("dve", lambda E, pr=pr: E.tensor_tensor(out=lg_all[:, 4 * i:4 * i + 4, :], in0=pr[:, 0:144].rearrange("p (j e) -> p j e", j=4),
                                                                 in1=rbias[:], op=ALU.add), r=["rbias"], w=[prn, "lg_all"])
                    if debug and l == 0 and i == 0:
                        P.dma("sp", lambda E: E.dma_start(out=dbg["d_lg"].ap(), in_=lg_all[:, 0:4, :].rearrange("p c t -> p (c t)")), r=["lg_all"], w=["dbg4"])
                for _i in range(NT):
                    mtile(_i)

            with contextlib.ExitStack() as sc:
                def sbl(name, shape, dt=F32, sc=sc):
                    t_ = sc.enter_context(nc.sbuf_tensor(f"{name}_{l}", list(shape), dt))
                    P.reg(name, t_)
                    return t_
                NC_ = NB * NE
                within = sbl("within", [128, NB, NE])
                tot = [sbl(f"tot{k}", [128, NB, NE]) for k in range(2)]
                tot0 = sbl("totz", [128, NB, NE])
                cmpb = sbl("cmpb", [128, NE, NB2])
                ntile = [sbl(f"ntile{k}", [128, NE]) for k in range(2)]
                nt0 = sbl("ntz", [128, NE])
                basee = sbl("basee", [128, NE])
                tend = sbl("tend", [128, NE])
                posall = sbl("posall", [128, NB, NE])
                ptmp = sbl("ptmp", [128, NB, NE])
                posf = sbl("posf", [128, NB])
                thr = sbl("thr", [128, NB2])
                cmpt = sbl("cmpt", [128, TM, NE])
                tef = sbl("tef", [128, TM])
                gmax = sbl("gmax", [128, NB])
                gd = sbl("gd", [128, NB, 4])
                gsum = sbl("gsum", [128, NB])
                pgrp = sbl("pgrp", [128, NB])
                ohg = sbl("ohg", [128, NB, 4])
                em = sbl("em", [128, NB, NE])
                em2 = sbl("em2", [128, NB, NE])
                m1 = sbl("m1", [128, NB])
                m2 = sbl("m2", [128, NB])
                d12 = sbl("d12", [128, NB])
                gl = lg_all[:, :, 0:4]
                el = lg_all[:, :, 4:36]
                P.op("dve", lambda E: E.tensor_reduce(out=gmax[:], in_=gl, axis=AX.X, op=ALU.max), r=["lg_all"], w=["gmax"])
                P.op("dve", lambda E: E.tensor_tensor(out=gd[:], in0=gl, in1=gmax[:].unsqueeze(2).to_broadcast([128, NB, 4]), op=ALU.subtract),
                     r=["lg_all", "gmax"], w=["gd"])
                P.op("dve", lambda E: E.tensor_tensor(out=ohg[:], in0=gl, in1=gmax[:].unsqueeze(2).to_broadcast([128, NB, 4]), op=ALU.is_equal),
                     r=["lg_all", "gmax"], w=["ohg"])
                P.op("act", lambda E: E.activation(out=gd[:], in_=gd[:], func=AF.Exp), r=["gd"], w=["gd"])
                P.op("dve", lambda E: E.tensor_reduce(out=gsum[:], in_=gd[:], axis=AX.X, op=ALU.add), r=["gd"], w=["gsum"])
                P.op("dve", lambda E: E.reciprocal(out=pgrp[:], in_=gsum[:]), r=["gsum"], w=["pgrp"])
                P.op("dve", lambda E: E.tensor_scalar(out=ohg[:], in0=ohg[:], scalar1=-1.0, scalar2=1e30, op0=ALU.add, op1=ALU.mult),
                     r=["ohg"], w=["ohg"])
                P.op("dve", lambda E: E.tensor_tensor(out=em[:].rearrange("p j (g e) -> p j g e", g=4),
                                                      in0=el.rearrange("p j (g e) -> p j g e", g=4),
                                                      in1=ohg[:].unsqueeze(3).to_broadcast([128, NB, 4, 8]), op=ALU.add),
                     r=["lg_all", "ohg"], w=["em"])
                P.op("dve", lambda E: E.tensor_reduce(out=m1[:], in_=em[:], axis=AX.X, op=ALU.max), r=["em"], w=["m1"])
                P.op("dve", lambda E: E.tensor_tensor(out=oh1[:], in0=em[:], in1=m1[:].unsqueeze(2).to_broadcast([128, NB, NE]),
                                                      op=ALU.is_equal), r=["em", "m1"], w=["oh1"])
                P.op("dve", lambda E: E.scalar_tensor_tensor(out=em2[:], in0=oh1[:], scalar=-1e30, in1=em[:], op0=ALU.mult,
                                                             op1=ALU.add), r=["oh1", "em"], w=["em2"])
                P.op("dve", lambda E: E.tensor_reduce(out=m2[:], in_=em2[:], axis=AX.X, op=ALU.max), r=["em2"], w=["m2"])
                P.op("dve", lambda E: E.tensor_tensor(out=oh2[:], in0=em2[:], in1=m2[:].unsqueeze(2).to_broadcast([128, NB, NE]),
                                                      op=ALU.is_equal), r=["em2", "m2"], w=["oh2"])
                P.op("dve", lambda E: E.tensor_tensor(out=d12[:], in0=m1[:], in1=m2[:], op=ALU.subtract), r=["m1", "m2"], w=["d12"])
                P.op("act", lambda E: E.activation(out=d12[:], in_=d12[:], func=AF.Sigmoid), r=["d12"], w=["d12"])
                P.op("dve", lambda E: E.tensor_tensor(out=w1[:], in0=d12[:], in1=pgrp[:], op=ALU.mult), r=["d12", "pgrp"], w=["w1"])
                P.op("dve", lambda E: E.tensor_tensor(out=w2[:], in0=pgrp[:], in1=w1[:], op=ALU.subtract), r=["pgrp", "w1"], w=["w2"])
                P.op("dve", lambda E: E.tensor_tensor(out=Aall[:], in0=oh1[:], in1=oh2[:], op=ALU.add), r=["oh1", "oh2"], w=["Aall"])
                Af = Aall[:].rearrange("p b e -> p (b e)")
                for h0 in range(0, NC_, 512):
                    wd_ = min(512, NC_ - h0)
                    pbw, pbwn = fbank()
                    P.op("pe", lambda E, h0=h0, wd_=wd_, pbw=pbw: E.matmul(pbw[:, 0:wd_], lhsT=trib[:], rhs=Af[:, h0:h0 + wd_], start=True, stop=True),
                         r=["trib", "Aall"], w=[pbwn])
                    P.op("act", lambda E, h0=h0, wd_=wd_, pbw=pbw: E.activation(out=within[:].rearrange("p b e -> p (b e)")[:, h0:h0 + wd_],
                                                                           in_=pbw[:, 0:wd_], func=AF.Copy), w=[pbwn, "within"])
                    pbt, pbtn = fbank()
                    P.op("pe", lambda E, h0=h0, wd_=wd_, pbt=pbt: E.matmul(pbt[:, 0:wd_], lhsT=onesb[:], rhs=Af[:, h0:h0 + wd_], start=True, stop=True),
                         r=["onesb", "Aall"], w=[pbtn])
                    P.op("dve", lambda E, h0=h0, wd_=wd_, pbt=pbt: E.tensor_copy(out=tot0[:].rearrange("p b e -> p (b e)")[:, h0:h0 + wd_],
                                                                            in_=pbt[:, 0:wd_]), w=[pbtn, "totz"])
                cur, curn = tot0, "totz"
                step = 1
                bi = 0
                while step < NB:
                    dst, dn = tot[bi], f"tot{bi}"
                    P.op("dve", lambda E, dst=dst, cur=cur, step=step: E.tensor_copy(out=dst[:, 0:step, :], in_=cur[:, 0:step, :]), r=[curn], w=[dn])
                    P.op("dve", lambda E, dst=dst, cur=cur, step=step: E.tensor_tensor(out=dst[:, step:NB, :], in0=cur[:, step:NB, :],
                                                                                   in1=cur[:, 0:NB - step, :], op=ALU.add), r=[curn], w=[dn])
                    cur, curn = dst, dn
                    step *= 2
                    bi ^= 1
                incl, incln = cur, curn
                P.op("dve", lambda E: E.tensor_tensor(out=posall[:], in0=within[:], in1=incl[:], op=ALU.add), r=["within", incln], w=["posall"])
                P.op("dve", lambda E: E.tensor_tensor(out=posall[:], in0=posall[:], in1=tot0[:], op=ALU.subtract), r=["posall", "totz"], w=["posall"])
                cnt = incl[:, NB - 1, :]
                P.op("dve", lambda E: E.tensor_scalar(out=thr[:], in0=iota_f[:, 0:NB2], scalar1=float(SL), scalar2=None, op0=ALU.mult),
                     r=["iota_f"], w=["thr"])
                P.op("dve", lambda E: E.tensor_tensor(out=cmpb[:], in0=cnt.unsqueeze(2).to_broadcast([128, NE, NB2]),
                                                      in1=thr[:].unsqueeze(1).to_broadcast([128, NE, NB2]), op=ALU.is_gt),
                     r=[incln, "thr"], w=["cmpb"])
                P.op("dve", lambda E: E.tensor_reduce(out=nt0[:], in_=cmpb[:], axis=AX.X, op=ALU.add), r=["cmpb"], w=["ntz"])
                cur, curn = nt0, "ntz"
                step = 1
                bi = 0
                while step < NE:
                    dst, dn = ntile[bi], f"ntile{bi}"
                    P.op("dve", lambda E, dst=dst, cur=cur, step=step: E.tensor_copy(out=dst[:, 0:step], in_=cur[:, 0:step]), r=[curn], w=[dn])
                    P.op("dve", lambda E, dst=dst, cur=cur, step=step: E.tensor_tensor(out=dst[:, step:NE], in0=cur[:, step:NE],
                                                                                   in1=cur[:, 0:NE - step], op=ALU.add), r=[curn], w=[dn])
                    cur, curn = dst, dn
                    step *= 2
                    bi ^= 1
                P.op("dve", lambda E, cur=cur: E.tensor_copy(out=tend[:], in_=cur[:]), r=[curn], w=["tend"])
                P.op("dve", lambda E: E.tensor_tensor(out=basee[:], in0=tend[:], in1=nt0[:], op=ALU.subtract), r=["tend", "ntz"], w=["basee"])
                P.op("dve", lambda E: E.tensor_scalar(out=basee[:], in0=basee[:], scalar1=float(SL), scalar2=None, op0=ALU.mult), r=["basee"], w=["basee"])
                P.op("dve", lambda E: E.tensor_tensor(out=posall[:], in0=posall[:], in1=basee[:].unsqueeze(1).to_broadcast([128, NB, NE]), op=ALU.add),
                     r=["posall", "basee"], w=["posall"])
                for (ohk, posk, nm) in ((oh1, pos1, "pos1"), (oh2, pos2, "pos2")):
                    P.op("dve", lambda E, ohk=ohk: E.tensor_tensor(out=ptmp[:], in0=ohk[:], in1=posall[:], op=ALU.mult),
                         r=["oh1", "oh2", "posall"], w=["ptmp"])
                    P.op("dve", lambda E: E.tensor_reduce(out=posf[:], in_=ptmp[:], axis=AX.X, op=ALU.add), r=["ptmp"], w=["posf"])
                    P.op("dve", lambda E, posk=posk: E.tensor_copy(out=posk[:], in_=posf[:]), r=["posf"], w=[nm])
                for j0 in range(0, TM, 128):
                    jn = min(128, TM - j0)
                    P.op("dve", lambda E, j0=j0, jn=jn: E.tensor_scalar(out=tef[:, j0:j0 + jn], in0=iota_f[:, 0:jn], scalar1=float(j0), scalar2=None,
                                                                       op0=ALU.add), r=["iota_f"], w=["tef"])
                P.op("dve", lambda E: E.tensor_tensor(out=cmpt[:], in0=tend[:].unsqueeze(1).to_broadcast([128, TM, NE]),
                                                      in1=tef[:].unsqueeze(2).to_broadcast([128, TM, NE]), op=ALU.is_le),
                     r=["tend", "tef"], w=["cmpt"])
                P.op("dve", lambda E: E.tensor_reduce(out=tef[:], in_=cmpt[:], axis=AX.X, op=ALU.add), r=["cmpt"], w=["tef"])
                P.op("dve", lambda E: E.tensor_scalar(out=tef[:], in0=tef[:], scalar1=128.0, scalar2=pidx_f[:, 0:1], op0=ALU.mult, op1=ALU.add),
                     r=["tef", "pidx_f"], w=["tef"])
                P.op("dve", lambda E: E.tensor_copy(out=te_i[:], in_=tef[:]), r=["tef"], w=["te_i"])

            if debug and l == 0:
                for nm, t_, rn in (("d_pos1", pos1, "pos1"), ("d_pos2", pos2, "pos2"), ("d_w1", w1, "w1"), ("d_w2", w2, "w2"), ("d_te", te_i, "te_i")):
                    P.dma("sp", lambda E, nm=nm, t_=t_: E.dma_start(out=dbg[nm].ap(), in_=t_[:]), r=[rn], w=["dbg_" + nm])
            with contextlib.ExitStack() as sc:
                def sbl(name, shape, dt=F32, sc=sc):
                    t_ = sc.enter_context(nc.sbuf_tensor(f"{name}_{l}", list(shape), dt))
                    P.reg(name, t_)
                    return t_
                hrow = [sbl(f"hrow{k}", [128, D], BF16) for k in range(6)]
                def sblock(b):
                    k = b % 6
                    P.dma("sp", lambda E, b=b, k=k: E.dma_start(out=hrow[k][:], in_=h2s[b * 128:(b + 1) * 128, :]), r=[f"h2s_{b // 4}"], w=[f"hrow{k}"])
                    for (posk, nm, tg) in ((pos1, "pos1", "a"), (pos2, "pos2", "b")):
                        P.dma("pool", lambda E, b=b, k=k, posk=posk: E.indirect_dma_start(
                            out=hsort[:, :], out_offset=bass.IndirectOffsetOnAxis(ap=posk[:, b:b + 1], axis=0),
                            in_=hrow[k][:], in_offset=None), r=[f"hrow{k}", nm], w=[f"hsortw{tg}{b}"])
                for _b in range(NB):
                    sblock(_b)
            nxt = (l + 1 < DEPTH_)
            scP = contextlib.ExitStack()
            if nxt:
                load_weights(l + 1)
                aw_n, ab_n, gbc_n = pro_bufs(scP, f"p{l + 1}", nbuf=1)
                prologue(l + 1, 0, 20, True, aw_n, ab_n, gbc_n)
            with contextlib.ExitStack() as sc:
                def sbl(name, shape, dt=F32, sc=sc):
                    t_ = sc.enter_context(nc.sbuf_tensor(f"{name}_{l}", list(shape), dt))
                    P.reg(name, t_)
                    return t_
                NW = 3
                wgb = [sbl(f"wgb{k}", [128, 8, HID], BF16) for k in range(NW)]
                wub = [sbl(f"wub{k}", [128, 8, HID], BF16) for k in range(NW)]
                wdb = [sbl(f"wdb{k}", [128, 2, D], BF16) for k in range(NW)]
                stt = [sbl(f"stt{k}", [128, 2, D], BF16) for k in range(2)]
                hsT = [sbl(f"hsT{k}", [128, 8, SL], BF16) for k in range(2)]
                sgl = [sbl(f"sgl{k}", [128, 2 * SL]) for k in range(2)]
                hid = [sbl(f"hid{k}", [128, 2, SL], BF16) for k in range(2)]
                yo = [sbl(f"yo{k}", [128, 2, D], BF16) for k in range(2)]
                allsc = [f"hsortw{tg}{b}" for b in range(NB) for tg in "ab"]

                def etile(j):
                    k = j % NW
                    k2 = j % 2
                    for (wb, wsrc, nm) in ((wgb, wg_d, "wgb"), (wub, wu_d, "wub"), (wdb, wd_d, "wdb")):
                        P.dma("pool", lambda E, wb=wb, wsrc=wsrc: E.indirect_dma_start(
                            out=wb[k][:].rearrange("p c h -> p (c h)"), out_offset=None, in_=wsrc[l][:, :],
                            in_offset=bass.IndirectOffsetOnAxis(ap=te_i[:, j:j + 1], axis=0), bounds_check=bcd["v"], oob_is_err=False),
                            r=["te_i"], w=[f"{nm}{k}"])
                    P.dma("sp", lambda E: E.dma_start(out=stt[k2][:], in_=hsort[j * SL:(j + 1) * SL, :].rearrange("(q p) d -> p q d", p=128)),
                          r=allsc, w=[f"stt{k2}"])
                    for q in range(2):
                        pb, pbn = bbank()
                        for c in range(8):
                            P.op("pe", lambda E, c=c, q=q, pb=pb: E.transpose(out=pb[:, c * 128:(c + 1) * 128], in_=stt[k2][:, q, c * 128:(c + 1) * 128],
                                                                           identity=identb[:]), r=[f"stt{k2}", "identb"], w=[pbn])
                        if q == 0:
                            P.op("act", lambda E, q=q, pb=pb: E.activation(out=hsT[k2][:, :, q * 128:(q + 1) * 128],
                                                                         in_=pb[:].rearrange("p (c t) -> p c t", c=8), func=AF.Copy), w=[pbn, f"hsT{k2}_{q}"])
                        else:
                            P.op("dve", lambda E, q=q, pb=pb: E.tensor_copy(out=hsT[k2][:, :, q * 128:(q + 1) * 128],
                                                                          in_=pb[:].rearrange("p (c t) -> p c t", c=8)), w=[pbn, f"hsT{k2}_{q}"])
                    hsr = [f"hsT{k2}_0", f"hsT{k2}_1"]
                    pg, pgn = fbank()
                    pu, pun = fbank()
                    for (W, Wn, pz, pzn) in ((wgb[k], f"wgb{k}", pg, pgn), (wub[k], f"wub{k}", pu, pun)):
                        for hc in range(2):
                            for c in range(8):
                                P.op("pe", lambda E, c=c, W=W, hc=hc, pz=pz: E.matmul(
                                    pz[:, hc * SL:(hc + 1) * SL], lhsT=W[:, c, hc * 128:(hc + 1) * 128], rhs=hsT[k2][:, c, :],
                                    start=(c == 0), stop=(c == 7)), r=[Wn] + hsr, w=[pzn])
                    P.op("act", lambda E: E.activation(out=sgl[k2][:], in_=pg[:, 0:2 * SL], func=AF.Silu), w=[pgn, f"sgl{k2}"])
                    P.op("dve", lambda E: E.tensor_tensor(out=hid[k2][:].rearrange("p c t -> p (c t)"), in0=pu[:, 0:2 * SL],
                                                          in1=sgl[k2][:], op=ALU.mult), r=[f"sgl{k2}"], w=[pun, f"hid{k2}"])
                    for q in range(2):
                        for dh in range(2):
                            pd, pdn = fbank()
                            for kc in range(2):
                                P.op("pe", lambda E, q=q, dh=dh, kc=kc, pd=pd: E.matmul(pd[:], lhsT=hid[k2][:, kc, q * 128:(q + 1) * 128],
                                                                                     rhs=wdb[k][:, kc, dh * 512:(dh + 1) * 512],
                                                                                     start=(kc == 0), stop=(kc == 1)),
                                     r=[f"hid{k2}", f"wdb{k}"], w=[pdn])
                            if dh == 0:
                                P.op("act", lambda E, q=q, pd=pd: E.activation(out=yo[k2][:, q, 0:512], in_=pd[:], func=AF.Copy), w=[pdn, f"yo{k2}_{q}0"])
                            else:
                                P.op("dve", lambda E, q=q, pd=pd: E.tensor_copy(out=yo[k2][:, q, 512:1024], in_=pd[:]), w=[pdn, f"yo{k2}_{q}1"])
                    P.dma("act", lambda E: E.dma_start(out=ysort[j * SL:(j + 1) * SL, :].rearrange("(q p) d -> p q d", p=128), in_=yo[k2][:]),
                          r=[f"yo{k2}_{q}{h}" for q in range(2) for h in "01"], w=[f"ysortw{j}"])
                for _j in range(TM):
                    etile(_j)

            scP.close()
            with contextlib.ExitStack() as sc:
                def sbl(name, shape, dt=F32, sc=sc):
                    t_ = sc.enter_context(nc.sbuf_tensor(f"{name}_{l}", list(shape), dt))
                    P.reg(name, t_)
                    return t_
                r1 = [sbl(f"r1{k}", [128, D], BF16) for k in range(4)]
                r2 = [sbl(f"r2{k}", [128, D], BF16) for k in range(4)]
                rf = [sbl(f"rf{k}", [128, D]) for k in range(4)]
                xc = [sbl(f"xc{k}", [128, D]) for k in range(4)]
                fgb = sbl("fgb", [128, D])
                ssf = sbl("ssf", [128, NB])
                rrf = sbl("rrf", [128, NB])
                junk2 = sbl("junk2", [128, D], BF16)
                ally = [f"ysortw{j}" for j in range(TM)]
                if last:
                    P.dma("sp", lambda E: E.dma_start(out=fgb[:], in_=final_g.ap().partition_broadcast(128)), w=["fgb"])
                    P.op("act", lambda E: E.mul(out=fgb[:], in_=fgb[:], mul=32.0), r=["fgb"], w=["fgb"])
                    P.op("dve", lambda E: E.memset(ssf[:], 0.0), w=["ssf"])
                def cload(b):
                    k = b % 4
                    rows = slice(b * 128, (b + 1) * 128)
                    P.dma("pool", lambda E: E.indirect_dma_start(out=r1[k][:], out_offset=None, in_=ysort[:, :],
                                                                 in_offset=bass.IndirectOffsetOnAxis(ap=pos1[:, b:b + 1], axis=0)),
                          r=ally + ["pos1"], w=[f"r1{k}"])
                    P.dma("pool", lambda E: E.indirect_dma_start(out=r2[k][:], out_offset=None, in_=ysort[:, :],
                                                                 in_offset=bass.IndirectOffsetOnAxis(ap=pos2[:, b:b + 1], axis=0)),
                          r=ally + ["pos2"], w=[f"r2{k}"])
                    P.dma("sp", lambda E: E.dma_start(out=xc[k][:], in_=xs1[rows, :]), r=[f"xs1_{b // 4}"], w=[f"xc{k}"])

                def ccomp(b):
                    k = b % 4
                    rows = slice(b * 128, (b + 1) * 128)
                    P.op("act", lambda E: E.activation(out=rf[k][:], in_=r1[k][:], func=AF.Copy, scale=w1[:, b:b + 1]),
                         r=[f"r1{k}", "w1"], w=[f"rf{k}"])
                    P.op("dve", lambda E: E.scalar_tensor_tensor(out=rf[k][:], in0=r2[k][:], scalar=w2[:, b:b + 1], in1=rf[k][:],
                                                                 op0=ALU.mult, op1=ALU.add), r=[f"r2{k}", f"rf{k}", "w2"], w=[f"rf{k}"])
                    P.op("pool", lambda E: E.tensor_tensor(out=rf[k][:], in0=rf[k][:], in1=G2, op=ALU.mult), r=[f"rf{k}", "mod10", "mod11"], w=[f"rf{k}"])
                    P.op("dve", lambda E: E.tensor_tensor(out=xc[k][:], in0=xc[k][:], in1=rf[k][:], op=ALU.add), r=[f"xc{k}", f"rf{k}"],
                         w=[f"xc{k}"])
                    if not last:
                        P.dma("sp", lambda E: E.dma_start(out=xs2[rows, :], in_=xc[k][:]), r=[f"xc{k}"], w=[f"xs2_{b}"])
                    else:
                        P.op("act", lambda E: E.activation(out=junk2[:], in_=xc[k][:], func=AF.Square, accum_out=ssf[:, b:b + 1]),
                             r=[f"xc{k}"], w=["junk2", "ssf"])
                        P.op("act", lambda E: E.activation(out=rrf[:, b:b + 1], in_=ssf[:, b:b + 1], func=AF.Sqrt, bias=epsr[:, 0:1]),
                             r=["ssf", "epsr"], w=["rrf"])
                        P.op("dve", lambda E: E.reciprocal(out=rrf[:, b:b + 1], in_=rrf[:, b:b + 1]), r=["rrf"], w=["rrf"])
                        P.op("dve", lambda E: E.scalar_tensor_tensor(out=xc[k][:], in0=xc[k][:], scalar=rrf[:, b:b + 1], in1=fgb[:],
                                                                     op0=ALU.mult, op1=ALU.mult), r=[f"xc{k}", "rrf", "fgb"], w=[f"xc{k}"])
                        P.dma("sp", lambda E: E.dma_start(out=out_d[rows, :], in_=xc[k][:]), r=[f"xc{k}"], w=[f"out_{b}"])
                for _b in range(min(3, NB)):
                    cload(_b)
                for _b in range(NB):
                    if _b + 3 < NB:
                        cload(_b + 3)
                    ccomp(_b)
            if nxt:
                with contextlib.ExitStack() as scq:
                    aw_q, ab_q, gbc_q = pro_bufs(scq, f"q{l + 1}")
                    prologue(l + 1, 20, 24, False, aw_q, ab_q, gbc_q)
        for _l in range(DEPTH_):
            layer(_l)
        P.op("sp", lambda E: E.nop(), r=[f"out_{b}" for b in range(NB)] + [k for k in ("dbg1", "dbg2", "dbg2b", "dbg3", "dbg4", "dbg_d_pos1",
             "dbg_d_pos2", "dbg_d_w1", "dbg_d_w2", "dbg_d_te")], w=["done"])
        P.emit(sems)
    if debug:
        print("UNRESOLVED regions:", sorted(getattr(P, "unres", [])))
    return nc


_CACHE = {}


def _prep(inputs, S):
    f = lambda a: np.ascontiguousarray(np.asarray(a, dtype=np.float32))
    pvec = np.concatenate([f(inputs["conv_w"]), f(inputs["conv_b"])[:, None, :], f(inputs["conv_ln_g"])[:, None, :],
                           f(inputs["conv_ln_b"])[:, None, :], f(inputs["pool_scale"])[:, None, :]], axis=1)
    rw = np.concatenate([f(inputs["router_group_w"]), f(inputs["router_expert_w"])], axis=2)
    rb = np.concatenate([f(inputs["router_group_b"]), f(inputs["router_expert_b"])], axis=1)
    shared = dict(ada_w=f(inputs["ada_w"]), ada_b=f(inputs["ada_b"]), norm1_g=f(inputs["norm1_g"]), w_in=f(inputs["w_in"]),
                  pool_w=f(inputs["pool_w"]), pvec=np.ascontiguousarray(pvec), w_out=f(inputs["w_out"]), norm2_g=f(inputs["norm2_g"]),
                  rw=np.ascontiguousarray(rw), rb=np.ascontiguousarray(rb), final_g=f(inputs["final_g"]))
    for nm, key, nch in (("wg", "expert_w_gate", 8), ("wu", "expert_w_up", 8), ("wd", "expert_w_down", 2)):
        w = f(inputs[key])
        L, E_, K, F_ = w.shape
        wr_ = w.reshape(L, E_, nch, 128, F_).transpose(0, 1, 3, 2, 4).reshape(L, E_ * 128, nch * F_)
        for l in range(L):
            shared[f"{nm}{l}"] = np.ascontiguousarray(wr_[l])
    x = f(inputs["x"])
    c = f(inputs["c"])
    in_maps = []
    for b in range(x.shape[0]):
        m = dict(shared)
        m["x"] = np.ascontiguousarray(x[b])
        m["c"] = np.ascontiguousarray(c[b])
        in_maps.append(m)
    return in_maps


def kernel(**inputs):
    x = np.asarray(inputs["x"])
    B, S, _ = x.shape
    assert B == N_CORES
    if S not in _CACHE:
        _CACHE[S] = build(S)
    nc = _CACHE[S]
    in_maps = _prep(inputs, S)
    res = run_bass_kernel_spmd(nc, in_maps, core_ids=list(range(B)))
    return np.stack([np.asarray(r["out"], dtype=np.float32) for r in res.results], axis=0)
```
